# Optimizing a Trainium2 kernel written in Bass

```python
import jax, jax.numpy as jnp
from jax import lax
import numpy as np

D_MODEL = 2048
BATCH = 1
SEQ = 8192
DEPTH = 2

CHUNK = 64
EPS = 1e-6
GDN_HEADS = 8
GDN_DK = 128
GDN_DV = 128
GDN_QK_W = GDN_HEADS * GDN_DK
GDN_V_W = GDN_HEADS * GDN_DV
SHORT_CONV = 4
CONF_CH = 1024
CONF_KERNEL = 31
SSM_D_INNER = 2 * D_MODEL
SSM_HEADDIM = 64
SSM_HEADS = SSM_D_INNER // SSM_HEADDIM
SSM_STATE = 128
SSM_GROUPS = 8
SSM_CONV = 4
MOE_GROUPS = 4
MOE_EXPERTS_PER_GROUP = 8
MOE_EXPERTS = MOE_GROUPS * MOE_EXPERTS_PER_GROUP
MOE_TOP_K = 2
MOE_D_FF = 512
MOE_BLOCK = 128

IN0_SPLITS = [GDN_QK_W, 2 * GDN_QK_W, 2 * GDN_QK_W + GDN_V_W, 2 * GDN_QK_W + 2 * GDN_V_W,
              2 * GDN_QK_W + 2 * GDN_V_W + GDN_HEADS, 2 * GDN_QK_W + 2 * GDN_V_W + 2 * GDN_HEADS]
IN0_WIDTH = IN0_SPLITS[-1] + 2 * CONF_CH
MIX0_WIDTH = GDN_V_W + CONF_CH
SSM_XBC = SSM_D_INNER + 2 * SSM_GROUPS * SSM_STATE
IN1_SPLITS = [SSM_D_INNER, SSM_D_INNER + SSM_XBC]
IN1_WIDTH = SSM_D_INNER + SSM_XBC + SSM_HEADS
N_EVEN = (DEPTH + 1) // 2
N_ODD = DEPTH // 2

kernel_name = "hybrid_gdn_conformer_ssd_hmoe_adaln"


def _rmsnorm(x, g):
    xf = x.astype(jnp.float32)
    y = xf * lax.rsqrt(jnp.mean(xf * xf, axis=-1, keepdims=True) + EPS)
    return (y * g.astype(jnp.float32)).astype(x.dtype)


def _layernorm(x, g, b):
    xf = x.astype(jnp.float32)
    mu = jnp.mean(xf, axis=-1, keepdims=True)
    xc = xf - mu
    var = jnp.mean(xc * xc, axis=-1, keepdims=True)
    return (xc * lax.rsqrt(var + EPS) * g.astype(jnp.float32) + b.astype(jnp.float32)).astype(x.dtype)


def _l2norm(x):
    xf = x.astype(jnp.float32)
    return (xf * lax.rsqrt(jnp.sum(xf * xf, axis=-1, keepdims=True) + EPS)).astype(x.dtype)


def _causal_depthwise_conv(x, w):
    k = w.shape[0]
    return lax.conv_general_dilated(
        x, w[:, None, :].astype(x.dtype), window_strides=(1,), padding=((k - 1, 0),),
        dimension_numbers=('NWC', 'WIO', 'NWC'), feature_group_count=x.shape[-1])


def _gated_delta_rule(q, k, v, g, beta):
    out_dtype = v.dtype
    bsz, t_len, h, dk = q.shape
    dv = v.shape[-1]
    nc = t_len // CHUNK

    def chunks(t):
        t = t.astype(jnp.float32).reshape(bsz, nc, CHUNK, h, *t.shape[3:])
        return jnp.moveaxis(t, 3, 2)

    q = chunks(q) * (dk ** -0.5)
    k, v, g, beta = chunks(k), chunks(v), chunks(g), chunks(beta)
    g_cs = jnp.cumsum(g, axis=-1)
    tri_incl = jnp.tril(jnp.ones((CHUNK, CHUNK), bool))
    tri_strict = jnp.tril(jnp.ones((CHUNK, CHUNK), bool), -1)
    decay = jnp.exp(jnp.where(tri_incl, g_cs[..., :, None] - g_cs[..., None, :], -jnp.inf))
    kb = k * beta[..., None]
    a_mat = jnp.where(tri_strict, jnp.einsum('bnhik,bnhjk->bnhij', kb, k) * decay, 0.0)
    eye = jnp.eye(CHUNK, dtype=jnp.float32)
    rhs = jnp.concatenate([v * beta[..., None], kb * jnp.exp(g_cs)[..., None]], axis=-1)
    sol = lax.linalg.triangular_solve(eye + a_mat, rhs, left_side=True, lower=True)
    u_base, k_cumdecay = sol[..., :dv], sol[..., dv:]
    qk = jnp.einsum('bnhik,bnhjk->bnhij', q, k) * decay
    q_dec = q * jnp.exp(g_cs)[..., None]
    k_dec = k * jnp.exp(g_cs[..., -1:] - g_cs)[..., None]
    chunk_decay = jnp.exp(g_cs[..., -1])

    def step(state, inp):
        qk_c, qd_c, kd_c, ub_c, kcd_c, cd_c = inp
        u = ub_c - jnp.einsum('bhik,bhkv->bhiv', kcd_c, state)
        o = jnp.einsum('bhik,bhkv->bhiv', qd_c, state) + jnp.einsum('bhij,bhjv->bhiv', qk_c, u)
        state = state * cd_c[..., None, None] + jnp.einsum('bhjk,bhjv->bhkv', kd_c, u)
        return state, o

    xs = tuple(jnp.moveaxis(t, 1, 0) for t in (qk, q_dec, k_dec, u_base, k_cumdecay, chunk_decay))
    _, o = lax.scan(step, jnp.zeros((bsz, h, dk, dv), jnp.float32), xs)
    o = jnp.transpose(o, (1, 0, 3, 2, 4)).reshape(bsz, t_len, h, dv)
    return o.astype(out_dtype)


def _ssd(x, dt, a, b_mat, c_mat, d_skip):
    out_dtype = x.dtype
    bsz, t_len, h, p = x.shape
    grp, n = b_mat.shape[2], b_mat.shape[3]
    hg = h // grp
    nc = t_len // CHUNK
    xf = x.astype(jnp.float32).reshape(bsz, nc, CHUNK, grp, hg, p)
    dt = dt.astype(jnp.float32).reshape(bsz, nc, CHUNK, grp, hg)
    bm = b_mat.astype(jnp.float32).reshape(bsz, nc, CHUNK, grp, n)
    cm = c_mat.astype(jnp.float32).reshape(bsz, nc, CHUNK, grp, n)
    a_cs = jnp.cumsum(dt * a.astype(jnp.float32).reshape(grp, hg), axis=2)
    tri = jnp.tril(jnp.ones((CHUNK, CHUNK), bool))
    seg = a_cs[:, :, :, None] - a_cs[:, :, None, :]
    l_mat = jnp.exp(jnp.where(tri[:, :, None, None], seg, -jnp.inf))
    xdt = xf * dt[..., None]
    cb = jnp.einsum('bcigs,bcjgs->bcijg', cm, bm)
    y_diag = jnp.einsum('bcijgh,bcjghp->bcighp', cb[..., None] * l_mat, xdt)
    decay_to_end = jnp.exp(a_cs[:, :, -1:] - a_cs)
    chunk_states = jnp.einsum('bcjgs,bcjghp->bcghps', bm, xdt * decay_to_end[..., None])
    chunk_decay = jnp.exp(a_cs[:, :, -1])
    decay_from_start = jnp.exp(a_cs)

    def step(state, inp):
        st_c, dec_c, c_c, dfs_c = inp
        y_off = jnp.einsum('bigs,bghps->bighp', c_c, state) * dfs_c[..., None]
        state = state * dec_c[..., None, None] + st_c
        return state, y_off

    xs = tuple(jnp.moveaxis(t, 1, 0) for t in (chunk_states, chunk_decay, cm, decay_from_start))
    _, y_off = lax.scan(step, jnp.zeros((bsz, grp, hg, p, n), jnp.float32), xs)
    y = y_diag + jnp.moveaxis(y_off, 0, 1) + d_skip.astype(jnp.float32).reshape(grp, hg)[..., None] * xf
    return y.reshape(bsz, t_len, h, p).astype(out_dtype)


def _even_mixer(h, w_in, conv_qkv, a_log, dt_bias, head_norm_g, conf_dw, conf_dw_b, conf_ln_g, conf_ln_b, w_out):
    bsz, t_len, _ = h.shape
    proj = h @ w_in
    qkv, z, b_logit, a_logit, glu = jnp.split(proj, IN0_SPLITS[2:], axis=-1)
    qkv = jax.nn.silu(_causal_depthwise_conv(qkv, conv_qkv))
    q, k, v = jnp.split(qkv, [GDN_QK_W, 2 * GDN_QK_W], axis=-1)
    q = _l2norm(q.reshape(bsz, t_len, GDN_HEADS, GDN_DK))
    k = _l2norm(k.reshape(bsz, t_len, GDN_HEADS, GDN_DK))
    v = v.reshape(bsz, t_len, GDN_HEADS, GDN_DV)
    beta = jax.nn.sigmoid(b_logit.astype(jnp.float32))
    g = -jnp.exp(a_log.astype(jnp.float32)) * jax.nn.softplus(a_logit.astype(jnp.float32) + dt_bias.astype(jnp.float32))
    o = _gated_delta_rule(q, k, v, g, beta)
    o = _rmsnorm(o, head_norm_g) * jax.nn.silu(z.reshape(bsz, t_len, GDN_HEADS, GDN_DV))
    a_out = o.reshape(bsz, t_len, GDN_V_W)
    u = glu[..., :CONF_CH] * jax.nn.sigmoid(glu[..., CONF_CH:])
    u = _causal_depthwise_conv(u, conf_dw) + conf_dw_b
    b_out = jax.nn.silu(_layernorm(u, conf_ln_g, conf_ln_b))
    return jnp.concatenate([a_out, b_out], axis=-1) @ w_out


def _odd_mixer(h, w_in, conv_w, conv_b, dt_bias, a_log, d_skip, norm_g, w_out):
    bsz, t_len, _ = h.shape
    proj = h @ w_in
    z, xbc, dt_raw = jnp.split(proj, IN1_SPLITS, axis=-1)
    xbc = jax.nn.silu(_causal_depthwise_conv(xbc, conv_w) + conv_b)
    xs, bm, cm = jnp.split(xbc, [SSM_D_INNER, SSM_D_INNER + SSM_GROUPS * SSM_STATE], axis=-1)
    dt = jax.nn.softplus(dt_raw.astype(jnp.float32) + dt_bias.astype(jnp.float32))
    a = -jnp.exp(a_log.astype(jnp.float32))
    y = _ssd(xs.reshape(bsz, t_len, SSM_HEADS, SSM_HEADDIM), dt, a,
             bm.reshape(bsz, t_len, SSM_GROUPS, SSM_STATE), cm.reshape(bsz, t_len, SSM_GROUPS, SSM_STATE), d_skip)
    y = _rmsnorm(y.reshape(bsz, t_len, SSM_D_INNER) * jax.nn.silu(z), norm_g)
    return y @ w_out


def _hier_moe(h, w_group, b_group, w_expert, b_expert, w1, w3, w2):
    bsz, t_len, d = h.shape
    m = bsz * t_len
    hf = h.reshape(m, d)
    grp_prob = jax.nn.softmax((hf @ w_group + b_group).astype(jnp.float32), axis=-1)
    grp_p, grp_idx = lax.top_k(grp_prob, 1)
    exp_logits = (hf @ w_expert + b_expert).astype(jnp.float32).reshape(m, MOE_GROUPS, MOE_EXPERTS_PER_GROUP)
    sel = jnp.broadcast_to(grp_idx[:, :, None], (m, 1, MOE_EXPERTS_PER_GROUP))
    in_grp = jnp.take_along_axis(exp_logits, sel, axis=1)[:, 0]
    top_v, top_i = lax.top_k(in_grp, MOE_TOP_K)
    gate = jax.nn.softmax(top_v, axis=-1) * grp_p
    expert_id = grp_idx * MOE_EXPERTS_PER_GROUP + top_i
    n_assign = m * MOE_TOP_K
    flat_e = expert_id.reshape(n_assign)
    flat_gate = gate.reshape(n_assign)
    flat_tok = jnp.repeat(jnp.arange(m, dtype=jnp.int32), MOE_TOP_K)
    order = jnp.argsort(flat_e)
    e_sorted = flat_e[order]
    counts = jnp.bincount(flat_e, length=MOE_EXPERTS)
    padded = (counts + MOE_BLOCK - 1) // MOE_BLOCK * MOE_BLOCK
    pad_end = jnp.cumsum(padded)
    pad_start = pad_end - padded
    raw_start = jnp.cumsum(counts) - counts
    dest = pad_start[e_sorted] + jnp.arange(n_assign) - raw_start[e_sorted]
    n_rows = (n_assign + MOE_EXPERTS * (MOE_BLOCK - 1) + MOE_BLOCK - 1) // MOE_BLOCK * MOE_BLOCK
    n_blocks = n_rows // MOE_BLOCK
    tok_rows = jnp.zeros((n_rows,), jnp.int32).at[dest].set(flat_tok[order])
    gate_rows = jnp.zeros((n_rows,), jnp.float32).at[dest].set(flat_gate[order])
    x_rows = hf[tok_rows].reshape(n_blocks, MOE_BLOCK, d)
    block_expert = jnp.minimum(jnp.searchsorted(pad_end, jnp.arange(n_blocks) * MOE_BLOCK, side='right'), MOE_EXPERTS - 1)

    def expert_block(args):
        xb, e = args
        return (jax.nn.silu(xb @ w1[e]) * (xb @ w3[e])) @ w2[e]

    y_rows = lax.map(expert_block, (x_rows, block_expert)).reshape(n_rows, d)
    y_rows = y_rows * gate_rows[:, None].astype(y_rows.dtype)
    out = jax.ops.segment_sum(y_rows, tok_rows, num_segments=m)
    return out.reshape(bsz, t_len, d)


def _adaln(x, g, shift, scale):
    return _rmsnorm(x, g) * (1 + scale[:, None, :]) + shift[:, None, :]


def setup_inputs(seed: int = 0) -> dict:
    key = jax.random.key(seed)
    ks = jax.random.split(key, 32)
    f32 = jnp.float32
    d = D_MODEL

    def nrm(k, shape, scale):
        return jax.random.normal(k, shape, f32) * scale

    def gain(k, shape):
        return 1.0 + 0.02 * jax.random.normal(k, shape, f32)

    def a_log_init(k, shape):
        return jnp.log(jax.random.uniform(k, shape, f32, 1.0, 16.0))

    def dt_bias_init(k, shape):
        dt = jnp.exp(jax.random.uniform(k, shape, f32, math_log(1e-3), math_log(1e-1)))
        return dt + jnp.log(-jnp.expm1(-dt))

    return {
        'x': nrm(ks[0], (BATCH, SEQ, d), 1.0),
        'c': nrm(ks[1], (BATCH, d), 1.0),
        'w_mod': nrm(ks[2], (2 * DEPTH, d, 3 * d), 0.5 * d ** -0.5),
        'b_mod': nrm(ks[3], (2 * DEPTH, 3 * d), 0.01),
        'norm_g': gain(ks[4], (DEPTH, 2, d)),
        'final_norm_g': gain(ks[5], (d,)),
        'e_w_in': nrm(ks[6], (N_EVEN, d, IN0_WIDTH), d ** -0.5),
        'e_conv_qkv': nrm(ks[7], (N_EVEN, SHORT_CONV, 2 * GDN_QK_W + GDN_V_W), SHORT_CONV ** -0.5),
        'e_a_log': a_log_init(ks[8], (N_EVEN, GDN_HEADS)),
        'e_dt_bias': dt_bias_init(ks[9], (N_EVEN, GDN_HEADS)),
        'e_head_norm_g': gain(ks[10], (N_EVEN, GDN_DV)),
        'e_conf_dw': nrm(ks[11], (N_EVEN, CONF_KERNEL, CONF_CH), CONF_KERNEL ** -0.5),
        'e_conf_dw_b': nrm(ks[12], (N_EVEN, CONF_CH), 0.01),
        'e_conf_ln_g': gain(ks[13], (N_EVEN, CONF_CH)),
        'e_conf_ln_b': nrm(ks[14], (N_EVEN, CONF_CH), 0.01),
        'e_w_out': nrm(ks[15], (N_EVEN, MIX0_WIDTH, d), MIX0_WIDTH ** -0.5),
        'o_w_in': nrm(ks[16], (N_ODD, d, IN1_WIDTH), d ** -0.5),
        'o_conv_w': nrm(ks[17], (N_ODD, SSM_CONV, SSM_XBC), SSM_CONV ** -0.5),
        'o_conv_b': nrm(ks[18], (N_ODD, SSM_XBC), 0.01),
        'o_dt_bias': dt_bias_init(ks[19], (N_ODD, SSM_HEADS)),
        'o_a_log': a_log_init(ks[20], (N_ODD, SSM_HEADS)),
        'o_d_skip': gain(ks[21], (N_ODD, SSM_HEADS)),
        'o_norm_g': gain(ks[22], (N_ODD, SSM_D_INNER)),
        'o_w_out': nrm(ks[23], (N_ODD, SSM_D_INNER, d), SSM_D_INNER ** -0.5),
        'moe_w_group': nrm(ks[24], (DEPTH, d, MOE_GROUPS), d ** -0.5),
        'moe_b_group': nrm(ks[25], (DEPTH, MOE_GROUPS), 0.01),
        'moe_w_expert': nrm(ks[26], (DEPTH, d, MOE_EXPERTS), d ** -0.5),
        'moe_b_expert': nrm(ks[27], (DEPTH, MOE_EXPERTS), 0.01),
        'moe_w1': nrm(ks[28], (DEPTH, MOE_EXPERTS, d, MOE_D_FF), d ** -0.5),
        'moe_w3': nrm(ks[29], (DEPTH, MOE_EXPERTS, d, MOE_D_FF), d ** -0.5),
        'moe_w2': nrm(ks[30], (DEPTH, MOE_EXPERTS, MOE_D_FF, d), MOE_D_FF ** -0.5),
    }


def math_log(v):
    return float(np.log(v))


def reference(x, c, w_mod, b_mod, norm_g, final_norm_g,
              e_w_in, e_conv_qkv, e_a_log, e_dt_bias, e_head_norm_g, e_conf_dw, e_conf_dw_b,
              e_conf_ln_g, e_conf_ln_b, e_w_out,
              o_w_in, o_conv_w, o_conv_b, o_dt_bias, o_a_log, o_d_skip, o_norm_g, o_w_out,
              moe_w_group, moe_b_group, moe_w_expert, moe_b_expert, moe_w1, moe_w3, moe_w2):
    mod = jnp.einsum('bd,lde->lbe', jax.nn.silu(c), w_mod) + b_mod[:, None, :]
    for layer in range(DEPTH):
        li = layer // 2
        shift, scale, gate = jnp.split(mod[2 * layer], 3, axis=-1)
        h = _adaln(x, norm_g[layer, 0], shift, scale)
        if layer % 2 == 0:
            mix = _even_mixer(h, e_w_in[li], e_conv_qkv[li], e_a_log[li], e_dt_bias[li], e_head_norm_g[li],
                              e_conf_dw[li], e_conf_dw_b[li], e_conf_ln_g[li], e_conf_ln_b[li], e_w_out[li])
        else:
            mix = _odd_mixer(h, o_w_in[li], o_conv_w[li], o_conv_b[li], o_dt_bias[li], o_a_log[li],
                             o_d_skip[li], o_norm_g[li], o_w_out[li])
        x = x + gate[:, None, :] * mix
        shift, scale, gate = jnp.split(mod[2 * layer + 1], 3, axis=-1)
        h = _adaln(x, norm_g[layer, 1], shift, scale)
        ffn = _hier_moe(h, moe_w_group[layer], moe_b_group[layer], moe_w_expert[layer], moe_b_expert[layer],
                        moe_w1[layer], moe_w3[layer], moe_w2[layer])
        x = x + gate[:, None, :] * ffn
    return _rmsnorm(x, final_norm_g)
```

```python
import numpy as np

from contextlib import ExitStack
import concourse.bass as bass
import concourse.mybir as mybir

F32 = mybir.dt.float32
BF16 = mybir.dt.bfloat16
I32 = mybir.dt.int32
AF = mybir.ActivationFunctionType
ALU = mybir.AluOpType
AX = mybir.AxisListType


class Buf:
    __slots__ = ("t", "name", "w", "r", "dsem")

    def __init__(self, t, name):
        self.t = t
        self.name = name
        self.w = None
        self.r = {}
        self.dsem = None

    def __getitem__(self, idx):
        return self.t[idx]


class View:
    __slots__ = ("buf", "ap")

    def __init__(self, buf, ap):
        self.buf = buf
        self.ap = ap

    def __getitem__(self, idx):
        return self.ap[idx]


class Prog:
    def __init__(self, nc):
        self.nc = nc
        self.st = ExitStack()
        self.eng = {"pe": nc.tensor, "act": nc.scalar, "dve": nc.vector,
                    "pool": nc.gpsimd, "sp": nc.sync}
        self.sems = {}
        self.cnt = {}
        self.waited = {e: {} for e in self.eng}
        for e in ("pe", "act", "dve", "pool"):
            self.sems[e] = self.st.enter_context(nc.semaphore("s_" + e))
            self.cnt[e] = 0
        self.nbuf = 0
        self.pending = {}
        self.dma_sem_free = []
        self.ndsem = 0

    def sbuf(self, shape, dt, name=None):
        self.nbuf += 1
        name = name or f"sb{self.nbuf}"
        t = self.st.enter_context(self.nc.sbuf_tensor("S_" + name, list(shape), dt))
        return Buf(t, name)

    def psum(self, shape, dt, name=None):
        self.nbuf += 1
        name = name or f"ps{self.nbuf}"
        t = self.st.enter_context(self.nc.psum_tensor("P_" + name, list(shape), dt))
        return Buf(t, name)

    def dram(self, name, shape, dt, kind):
        t = self.nc.dram_tensor(name, list(shape), dt, kind=kind)
        return Buf(t.ap(), name)

    def new_dma_sem(self):
        self.ndsem += 1
        k = f"d{self.ndsem}"
        self.sems[k] = self.st.enter_context(self.nc.semaphore("s_" + k))
        self.cnt[k] = 0
        return k

    def _wait(self, e, deps):
        eng = self.eng[e]
        need = {}
        for d in deps:
            if d is None:
                continue
            k, v = d
            if e == "pe" and k == "pe":
                continue
            if v > need.get(k, 0):
                need[k] = v
        for k, v in need.items():
            if self.waited[e].get(k, 0) < v:
                eng.wait_ge(self.sems[k], v)
                self.waited[e][k] = v

    def _deps(self, reads, writes):
        reads = [getattr(b, "buf", b) for b in reads]
        writes = [getattr(b, "buf", b) for b in writes]
        deps = []
        for b in reads:
            deps.append(b.w)
        for b in writes:
            deps.append(b.w)
            deps.extend(b.r.items())
        return deps

    def _mark(self, key, val, reads, writes):
        reads = [getattr(b, "buf", b) for b in reads]
        writes = [getattr(b, "buf", b) for b in writes]
        for b in reads:
            if b.r.get(key, 0) < val:
                b.r[key] = val
        for b in writes:
            b.w = (key, val)
            b.r = {}

    def op(self, e, fn, reads=(), writes=(), defer=False):
        self._wait(e, self._deps(reads, writes))
        inst = fn(self.eng[e])
        if defer:
            pr, pw = self.pending.setdefault(e, ([], []))
            pr.extend(reads)
            pw.extend(writes)
            self._mark(e, self.cnt[e] + 1, reads, writes)
            return inst
        self.cnt[e] += 1
        inst.then_inc(self.sems[e], 1)
        if e in self.pending:
            self.pending.pop(e)
        self._mark(e, self.cnt[e], reads, writes)
        return inst

    def dma(self, q, out, in_, reads=(), writes=(), sb=None, **kw):
        if sb.dsem is None:
            sb.dsem = self.new_dma_sem()
        sem = sb.dsem
        self._wait(q, self._deps(reads, writes))
        inst = self.eng[q].dma_start(out=out, in_=in_, **kw)
        self.cnt[sem] += 16
        inst.then_inc(self.sems[sem], 16)
        self._mark(sem, self.cnt[sem], reads, writes)
        return inst

    def wait_all(self, e, bufs):
        self._wait(e, self._deps(bufs, bufs))

    def close(self):
        self.st.close()

EPS = 1e-6


def fm(v):
    return np.ascontiguousarray(np.asarray(v, np.float32).reshape(-1, 128).T)

def fmk(w):
    K, n = w.shape[0], w.shape[1] // 128
    return np.ascontiguousarray(np.asarray(w, np.float32).reshape(K, n, 128).transpose(2, 0, 1).reshape(128, K * n))

def pre_inputs(layer, xfull, mod, inp):
    m = mod[2 * layer]
    vec = np.concatenate([fm(m[:2048]), fm(m[2048:4096]), fm(inp["norm_g"][layer, 0])], axis=1)
    if layer == 0:
        w = inp["e_w_in"][0]
        glu_a = w[:, 4112:5136].reshape(2048, 8, 128); glu_b = w[:, 5136:6160].reshape(2048, 8, 128)
        glu = np.stack([glu_a, glu_b], axis=2).reshape(2048, 2048)
        win = np.concatenate([w[:, 0:3072], glu, w[:, 3072:4096], w[:, 4096:4112]], axis=1)
        convw = fmk(inp["e_conv_qkv"][0]); convb = np.zeros((128, 24), np.float32)
        extra = {"dw": fmk(inp["e_conf_dw"][0]),
                 "dvec": np.concatenate([fm(inp["e_conf_dw_b"][0]), fm(inp["e_conf_ln_g"][0]), fm(inp["e_conf_ln_b"][0])], axis=1)}
    else:
        w = inp["o_w_in"][0]
        win = np.concatenate([w[:, 4096:10240], w[:, 0:4096], w[:, 10240:10304]], axis=1)
        convw = fmk(inp["o_conv_w"][0]); convb = fm(inp["o_conv_b"][0])
        extra = {}
    win = np.ascontiguousarray(win)
    xT = np.ascontiguousarray(xfull.T)
    maps = []
    for i in range(8):
        xs = np.zeros((2048, 1056), np.float32)
        lo = i * 1024 - 32
        if i == 0:
            xs[:, 32:] = xT[:, 0:1024]
        else:
            xs[:] = xT[:, lo:lo + 1056]
        hmask = np.full((128, 1), 0.0 if i == 0 else 1.0, np.float32)
        maps.append({"xT": xs, "vec": vec, "win": win, "convw": convw, "convb": convb, "hmask": hmask, **extra})
    return maps

def cmat():
    j = np.arange(128)[:, None]; i = np.arange(128)[None, :]
    same = (j // 64) == (i // 64)
    tri = ((j <= i) & same).astype(np.float32)
    blk = same.astype(np.float32)
    sel0 = np.broadcast_to(j < 64, (128, 128)).astype(np.float32)
    sel1 = np.broadcast_to(j >= 64, (128, 128)).astype(np.float32)
    return np.ascontiguousarray(np.concatenate([tri, blk, sel0, sel1], axis=1))

def ssd_inputs(pout, inp):
    maps = []
    cm = cmat()
    for g in range(8):
        xT = pout[g * 512:(g + 1) * 512]; zT = pout[6144 + g * 512:6144 + (g + 1) * 512]
        BT = np.ascontiguousarray(pout[4096 + g * 128:4096 + (g + 1) * 128]); CT = np.ascontiguousarray(pout[5120 + g * 128:5120 + (g + 1) * 128])
        dtr = pout[10240 + g * 8:10240 + (g + 1) * 8]
        dtr = np.ascontiguousarray(dtr.reshape(8, 64, 128).transpose(2, 1, 0).reshape(128, 512))
        hs = slice(g * 8, (g + 1) * 8)
        rv = np.concatenate([inp["o_dt_bias"][0][hs], inp["o_a_log"][0][hs], inp["o_d_skip"][0][hs]])[None, :]
        rv = np.ascontiguousarray(np.tile(rv, (128, 1)).astype(np.float32))
        maps.append({"x_tok": np.ascontiguousarray(xT.T), "z_tok": np.ascontiguousarray(zT.T), "BT": BT, "CT": CT,
                     "B_tok": np.ascontiguousarray(BT.T), "dtr": dtr, "rowvec": rv, "cmat": cm})
    return maps

def cmat_gdn():
    c = cmat()
    j = np.arange(128)[:, None]; i = np.arange(128)[None, :]
    masks = ((j > i) & ((j // 64) == (i // 64))).astype(np.float32)
    return np.ascontiguousarray(np.concatenate([c, np.eye(128, dtype=np.float32), masks], axis=1))

def gdn_inputs(pout, inp):
    cm = cmat_gdn()
    gn = np.ascontiguousarray(np.tile(inp["e_head_norm_g"][0][None, :], (128, 1)).astype(np.float32))
    maps = []
    for hd in range(8):
        r = slice(hd * 128, (hd + 1) * 128)
        qT = np.ascontiguousarray(pout[0:1024][r]); kT = np.ascontiguousarray(pout[1024:2048][r])
        vT = pout[2048:3072][r]; zT = pout[4096:5120][r]
        bl = np.ascontiguousarray(pout[5120 + hd].reshape(64, 128).T); al = np.ascontiguousarray(pout[5128 + hd].reshape(64, 128).T)
        rv = np.tile(np.array([[inp["e_a_log"][0][hd], inp["e_dt_bias"][0][hd]]], np.float32), (128, 1))
        maps.append({"qT": qT, "kT": kT, "k_tok": np.ascontiguousarray(kT.T), "v_tok": np.ascontiguousarray(vT.T),
                     "z_tok": np.ascontiguousarray(zT.T), "bl": bl, "al": al, "rowvec": np.ascontiguousarray(rv),
                     "gn": gn, "cmat": cm})
    return maps


def build_mod():
    nc = bass.Bass("TRN2", target_bir_lowering=False)
    P = Prog(nc)
    NCOL = 768
    c_d = P.dram("cT", [128, 16], F32, "ExternalInput")
    w_d = P.dram("wmod", [4, 2048, NCOL], F32, "ExternalInput")
    b_d = P.dram("bmod", [4, NCOL], F32, "ExternalInput")
    o_d = P.dram("mod", [4, NCOL], F32, "ExternalOutput")
    cs = P.sbuf([128, 16], F32, "cs")
    sc = P.sbuf([128, 16], F32, "sc")
    wb = [P.sbuf([128, 16, NCOL], F32, f"w{i}") for i in range(2)]
    bs = P.sbuf([1, 4 * NCOL], F32, "bs")
    os_ = P.sbuf([1, 4 * NCOL], F32, "os")
    bank = [P.psum([128, 512], F32, f"bank{i}") for i in range(2)]
    P.dma("sp", cs[:], c_d[:], writes=[cs], sb=cs)
    P.dma("sp", bs[:], b_d.t.rearrange("(o l) n -> o (l n)", o=1), writes=[bs], sb=bs)
    P.op("act", lambda e: e.activation(sc[:], cs[:], AF.Silu), reads=[cs], writes=[sc])
    k = 0
    for l in range(4):
        w = wb[l % 2]
        P.dma("sp" if l % 2 else "act", w[:], w_d[l].rearrange("(j p) n -> p j n", p=128), writes=[w], sb=w)
        for nh in range(2):
            bk = bank[k % 2]
            k += 1
            for j in range(16):
                P.op("pe", lambda e: e.matmul(bk[0:1, 0:384], sc[:, j:j + 1], w[:, j, nh * 384:(nh + 1) * 384],
                                              start=(j == 0), stop=(j == 15)),
                     reads=[sc, w], writes=[bk], defer=(j != 15))
            o = l * NCOL + nh * 384
            P.op("dve", lambda e: e.tensor_tensor(os_[0:1, o:o + 384], bk[0:1, 0:384], bs[0:1, o:o + 384], ALU.add),
                 reads=[bk, bs], writes=[os_])
    P.dma("sp", o_d.t.rearrange("(o l) n -> o (l n)", o=1), os_[:], reads=[os_], sb=os_)
    P.wait_all("sp", [os_])
    P.close()
    return nc


def build_pre(layer):
    NTK, HALO, NT = 1056, 32, 1024
    pieces = [(0, 32), (32, 544), (544, 1056)]
    if layer == 0:
        plan = [("conv", i) for i in range(24)] + [("glu", i) for i in range(16)] + \
               [("pass", i) for i in range(8)] + [("passp", 0)]
        NW, NOUT, NCONV, PW = 6160, 5136, 24, 16
        out_row = {"conv": 0, "glu": 3072, "pass": 4096, "passp": 5120}
    else:
        plan = [("conv", i) for i in range(48)] + [("pass", i) for i in range(32)] + [("passp", 0)]
        NW, NOUT, NCONV, PW = 10304, 10304, 48, 64
        out_row = {"conv": 0, "pass": 6144, "passp": 10240}
    nc = bass.Bass("TRN2", target_bir_lowering=False)
    P = Prog(nc)
    xT_d = P.dram("xT", [2048, NTK], F32, "ExternalInput")
    vec_d = P.dram("vec", [128, 48], F32, "ExternalInput")
    w_d = P.dram("win", [2048, NW], F32, "ExternalInput")
    cw_d = P.dram("convw", [128, 4 * NCONV], F32, "ExternalInput")
    cb_d = P.dram("convb", [128, NCONV], F32, "ExternalInput")
    hm_d = P.dram("hmask", [128, 1], F32, "ExternalInput")
    if layer == 0:
        dw_d = P.dram("dw", [128, 31 * 8], F32, "ExternalInput")
        dv_d = P.dram("dvec", [128, 24], F32, "ExternalInput")
    out_d = P.dram("pout", [NOUT, NT], F32, "ExternalOutput")

    big = [P.sbuf([128, NTK], F32, f"big{i}") for i in range(16)]
    hT = [P.sbuf([128, NTK], BF16, f"h{j}") for j in range(16)]
    wsl = [P.sbuf([128, 16, 512], BF16, f"wsl{i}") for i in range(3)]
    rstd = P.sbuf([128, NTK], F32, "rstd")
    sqb = [P.sbuf([128, 512], F32, f"sq{i}") for i in range(2)]
    tsm = [P.sbuf([128, 512], F32, f"tsm{i}") for i in range(2)]
    ones = P.sbuf([128, 128], F32, "ones")
    vec = P.sbuf([128, 48], F32, "vec")
    gs = P.sbuf([128, 16], F32, "gs")
    cw = P.sbuf([128, 4 * NCONV], F32, "cw")
    cb = P.sbuf([128, NCONV], F32, "cb")
    hm = P.sbuf([128, 1], F32, "hm")
    bank = [P.psum([128, 512], F32, f"bank{i}") for i in range(8)]
    if layer == 0:
        dw = P.sbuf([128, 31 * 8], F32, "dw")
        dv = P.sbuf([128, 24], F32, "dv")
        P.dma("sp", dw[:], dw_d[:], writes=[dw], sb=dw)
        P.dma("sp", dv[:], dv_d[:], writes=[dv], sb=dv)
    P.dma("sp", vec[:], vec_d[:], writes=[vec], sb=vec)
    P.dma("sp", cw[:], cw_d[:], writes=[cw], sb=cw)
    P.dma("sp", cb[:], cb_d[:], writes=[cb], sb=cb)
    P.dma("sp", hm[:], hm_d[:], writes=[hm], sb=hm)
    P.op("dve", lambda e: e.memset(ones[:], 1.0), writes=[ones])
    for j in range(16):
        P.dma("act" if j % 2 else "sp", big[j][:], xT_d[j * 128:(j + 1) * 128, :], writes=[big[j]], sb=big[j])

    P.op("dve", lambda e: e.tensor_scalar(gs[:], vec[:, 16:32], 1.0, None, ALU.add), reads=[vec], writes=[gs])
    P.op("dve", lambda e: e.tensor_tensor(gs[:], gs[:], vec[:, 32:48], ALU.mult), reads=[gs, vec], writes=[gs])
    for (lo, hi) in pieces:
        n = hi - lo
        for j in range(16):
            q = sqb[j % 2]
            P.op("act", lambda e: e.activation(q[:, 0:n], big[j][:, lo:hi], AF.Square), reads=[big[j]], writes=[q])
            P.op("pe", lambda e: e.matmul(bank[6][:, 0:n], ones[:], q[:, 0:n], start=(j == 0), stop=(j == 15)),
                 reads=[ones, q], writes=[bank[6]])
        t = tsm[0]
        P.op("act", lambda e: e.activation(t[:, 0:n], bank[6][:, 0:n], AF.Sqrt, bias=EPS, scale=1.0 / 2048),
             reads=[bank[6]], writes=[t])
        P.op("dve", lambda e: e.reciprocal(rstd[:, lo:hi], t[:, 0:n]), reads=[t], writes=[rstd])
    for j in range(16):
        P.op("dve", lambda e: e.tensor_tensor(big[j][:], big[j][:], rstd[:], ALU.mult),
             reads=[big[j], rstd], writes=[big[j]])
        P.op("dve", lambda e: e.tensor_scalar(hT[j][:], big[j][:], gs[:, j:j + 1], vec[:, j:j + 1], ALU.mult, ALU.add),
             reads=[big[j], gs, vec], writes=[hT[j]])

    rot = {"n": 0}
    if layer == 0:
        cv = big[0:8]
        pa = big[8]
        pool = big[9:16]
    else:
        pool = big

    def nextbuf():
        b = pool[rot["n"] % len(pool)]
        rot["n"] += 1
        return b

    nb = {"n": 0}
    wv = None
    for ci, (kind, idx) in enumerate(plan):
        blk = ci // 4
        if ci % 4 == 0:
            width = min(512, NW - blk * 512)
            ws = wsl[blk % 3]
            P.dma("pool", ws[:, :, 0:width], w_d[:, blk * 512:blk * 512 + width].rearrange("(j p) n -> p j n", p=128),
                  writes=[ws], sb=ws)
        off = (ci % 4) * 128
        wc = PW if kind == "passp" else 128
        halo = kind in ("conv", "glu")
        pr = pa if (kind == "glu" and idx % 2 == 0) else nextbuf()
        for pi, (lo, hi) in enumerate(pieces):
            if pi == 0 and not halo:
                continue
            n = hi - lo
            bk = bank[nb["n"] % 4]
            nb["n"] += 1
            for j in range(16):
                P.op("pe", lambda e: e.matmul(bk[0:wc, 0:n], ws[:, j, off:off + wc], hT[j][:, lo:hi],
                                              start=(j == 0), stop=(j == 15)),
                     reads=[ws, hT[j]], writes=[bk], defer=(j != 15))
            if pi == 0:
                P.op("act", lambda e: e.activation(pr[0:wc, lo:hi], bk[0:wc, 0:n], AF.Copy, scale=hm[0:wc, 0:1]),
                     reads=[bk, hm], writes=[pr])
            else:
                P.op("act", lambda e: e.activation(pr[0:wc, lo:hi], bk[0:wc, 0:n], AF.Copy), reads=[bk], writes=[pr])
        if kind == "conv":
            acc = nextbuf()
            P.op("dve", lambda e: e.tensor_scalar(acc[:, 0:NT], pr[:, 29:29 + NT], cw[:, idx:idx + 1], None, ALU.mult),
                 reads=[pr, cw], writes=[acc])
            for k in range(1, 4):
                P.op("dve", lambda e: e.scalar_tensor_tensor(acc[:, 0:NT], pr[:, 29 + k:29 + k + NT],
                                                             cw[:, k * NCONV + idx:k * NCONV + idx + 1], acc[:, 0:NT],
                                                             ALU.mult, ALU.add),
                     reads=[pr, cw, acc], writes=[acc])
            P.op("act", lambda e: e.activation(acc[:, 0:NT], acc[:, 0:NT], AF.Silu, bias=cb[:, idx:idx + 1], scale=1.0),
                 reads=[acc, cb], writes=[acc])
            if layer == 0 and idx < 16:
                qs = 128.0 ** -0.5 if idx < 8 else 1.0
                for hh in range(2):
                    sl = slice(hh * 512, (hh + 1) * 512)
                    q = sqb[hh]
                    P.op("act", lambda e: e.activation(q[:], acc[:, sl], AF.Square), reads=[acc], writes=[q])
                    P.op("pe", lambda e: e.matmul(bank[6][:], ones[:], q[:], start=True, stop=True),
                         reads=[ones, q], writes=[bank[6]])
                    t = tsm[hh]
                    P.op("act", lambda e: e.activation(t[:], bank[6][:], AF.Sqrt, bias=EPS, scale=1.0),
                         reads=[bank[6]], writes=[t])
                    P.op("dve", lambda e: e.reciprocal(t[:], t[:]), reads=[t], writes=[t])
                    P.op("dve", lambda e: e.scalar_tensor_tensor(acc[:, sl], acc[:, sl], qs, t[:], ALU.mult, ALU.mult),
                         reads=[acc, t], writes=[acc])
            r0 = out_row["conv"] + idx * 128
            P.dma("sp" if ci % 2 else "act", out_d[r0:r0 + 128, :], acc[:, 0:NT], reads=[acc], sb=acc)
        elif kind == "glu":
            if idx % 2 == 0:
                continue
            c = idx // 2
            P.op("act", lambda e: e.activation(pr[:], pr[:], AF.Sigmoid), reads=[pr], writes=[pr])
            P.op("dve", lambda e: e.tensor_tensor(pr[:], pr[:], pa[:], ALU.mult), reads=[pr, pa], writes=[pr])
            acc = cv[c]
            P.op("dve", lambda e: e.tensor_scalar(acc[:, 0:NT], pr[:, 2:2 + NT], dw[:, c:c + 1], None, ALU.mult),
                 reads=[pr, dw], writes=[acc])
            for k in range(1, 31):
                P.op("dve", lambda e: e.scalar_tensor_tensor(acc[:, 0:NT], pr[:, 2 + k:2 + k + NT],
                                                             dw[:, k * 8 + c:k * 8 + c + 1], acc[:, 0:NT],
                                                             ALU.mult, ALU.add),
                     reads=[pr, dw, acc], writes=[acc])
            P.op("dve", lambda e: e.tensor_scalar(acc[:, 0:NT], acc[:, 0:NT], dv[:, c:c + 1], None, ALU.add),
                 reads=[acc, dv], writes=[acc])
            if c == 7:
                for hh in range(2):
                    sl = slice(hh * 512, (hh + 1) * 512)
                    for c2 in range(8):
                        P.op("pe", lambda e: e.matmul(bank[6][:], ones[:], cv[c2][:, sl], start=(c2 == 0), stop=(c2 == 7)),
                             reads=[ones, cv[c2]], writes=[bank[6]], defer=(c2 != 7))
                    mb = tsm[0]
                    P.op("act", lambda e: e.activation(mb[:], bank[6][:], AF.Copy, scale=1.0 / 1024),
                         reads=[bank[6]], writes=[mb])
                    for c2 in range(8):
                        P.op("dve", lambda e: e.tensor_tensor(cv[c2][:, sl], cv[c2][:, sl], mb[:], ALU.subtract),
                             reads=[cv[c2], mb], writes=[cv[c2]])
                        q = sqb[c2 % 2]
                        P.op("act", lambda e: e.activation(q[:], cv[c2][:, sl], AF.Square), reads=[cv[c2]], writes=[q])
                        P.op("pe", lambda e: e.matmul(bank[7][:], ones[:], q[:], start=(c2 == 0), stop=(c2 == 7)),
                             reads=[ones, q], writes=[bank[7]])
                    t = tsm[1]
                    P.op("act", lambda e: e.activation(t[:], bank[7][:], AF.Sqrt, bias=EPS, scale=1.0 / 1024),
                         reads=[bank[7]], writes=[t])
                    P.op("dve", lambda e: e.reciprocal(t[:], t[:]), reads=[t], writes=[t])
                    for c2 in range(8):
                        P.op("dve", lambda e: e.tensor_tensor(cv[c2][:, sl], cv[c2][:, sl], t[:], ALU.mult),
                             reads=[cv[c2], t], writes=[cv[c2]])
                        P.op("act", lambda e: e.activation(cv[c2][:, sl], cv[c2][:, sl], AF.Silu,
                                                           bias=dv[:, 16 + c2:17 + c2], scale=dv[:, 8 + c2:9 + c2]),
                             reads=[cv[c2], dv], writes=[cv[c2]])
                for c2 in range(8):
                    r0 = out_row["glu"] + c2 * 128
                    P.dma("sp" if c2 % 2 else "act", out_d[r0:r0 + 128, :], cv[c2][:, 0:NT], reads=[cv[c2]], sb=cv[c2])
        else:
            r0 = out_row[kind] + idx * 128
            P.dma("sp" if ci % 2 else "act", out_d[r0:r0 + wc, :], pr[0:wc, HALO:NTK], reads=[pr], sb=pr)
    allb = big
    P.wait_all("sp", allb)
    P.wait_all("act", allb)
    P.close()
    return nc


def build_gdn():
    T, NTILE, GT = 8192, 64, 4
    nc = bass.Bass("TRN2", target_bir_lowering=False)
    P = Prog(nc)
    qT_d = P.dram("qT", [128, T], F32, "ExternalInput")
    kT_d = P.dram("kT", [128, T], F32, "ExternalInput")
    kk_d = P.dram("k_tok", [T, 128], F32, "ExternalInput")
    vk_d = P.dram("v_tok", [T, 128], F32, "ExternalInput")
    zk_d = P.dram("z_tok", [T, 128], F32, "ExternalInput")
    bl_d = P.dram("bl", [128, 64], F32, "ExternalInput")
    al_d = P.dram("al", [128, 64], F32, "ExternalInput")
    rv_d = P.dram("rowvec", [128, 2], F32, "ExternalInput")
    gn_d = P.dram("gn", [128, 128], F32, "ExternalInput")
    cm_d = P.dram("cmat", [128, 768], F32, "ExternalInput")
    out_d = P.dram("o_tok", [T, 128], F32, "ExternalOutput")

    cm = P.sbuf([128, 768], F32, "cm")
    TRI, BLK, SEL0, SEL1, IDENT, MASKS = (cm[:, i * 128:(i + 1) * 128] for i in range(6))
    rv = P.sbuf([128, 2], F32, "rv")
    gn = P.sbuf([128, 128], F32, "gn")
    names = ("bl", "al", "beta", "xb", "ax", "ex", "ln", "sp", "g", "gcs", "gtot", "eg", "kdsc", "beg",
             "dec0", "dec1", "aneg")
    S = {k: P.sbuf([128, 64], F32, "s_" + k) for k in names}
    St = [P.sbuf([128, 128], F32, f"St{i}") for i in range(2)]
    qTg = [P.sbuf([128, 512], F32, f"qTg{i}") for i in range(2)]
    kTg = [P.sbuf([128, 512], F32, f"kTg{i}") for i in range(2)]
    kkg = [P.sbuf([128, GT, 128], F32, f"kkg{i}") for i in range(2)]
    vkg = [P.sbuf([128, GT, 128], F32, f"vkg{i}") for i in range(2)]
    zkg = [P.sbuf([128, GT, 128], F32, f"zkg{i}") for i in range(2)]
    og = [P.sbuf([128, GT, 128], F32, f"og{i}") for i in range(2)]
    W = {}
    for gi in range(GT):
        for k in ("Y", "YT", "decn", "dect", "erb", "t", "N", "M", "Na", "Nb", "Ma", "Mb", "QKm", "qeg", "kd",
                  "kcdT", "u", "o", "sz", "rbs"):
            W[k, gi] = P.sbuf([128, 128], F32, f"w_{k}{gi}")
        for k in ("R0", "R1"):
            W[k, gi] = P.sbuf([128, 256], F32, f"w_{k}{gi}")
        for k in ("ss", "rs"):
            W[k, gi] = P.sbuf([128, 1], F32, f"w_{k}{gi}")
    bkA = [P.psum([128, 512], F32, f"bkA{i}") for i in range(GT)]
    bkB = [P.psum([128, 512], F32, f"bkB{i}") for i in range(GT)]
    def vw(b, lo, n):
        return View(b, b[:, lo:lo + n])
    A_q = [[vw(bkA[gi], q * 128, 128) for q in range(4)] for gi in range(GT)]
    A_h = [[vw(bkA[gi], h * 256, 256) for h in range(2)] for gi in range(GT)]
    B_q = [[vw(bkB[gi], q * 128, 128) for q in range(4)] for gi in range(GT)]
    pq = [B_q[0][0]]

    P.dma("sp", cm[:], cm_d[:], writes=[cm], sb=cm)
    P.dma("sp", rv[:], rv_d[:], writes=[rv], sb=rv)
    P.dma("sp", gn[:], gn_d[:], writes=[gn], sb=gn)
    P.dma("sp", S["bl"][:], bl_d[:], writes=[S["bl"]], sb=S["bl"])
    P.dma("sp", S["al"][:], al_d[:], writes=[S["al"]], sb=S["al"])

    D = lambda fn, r, w: P.op("dve", fn, reads=r, writes=w)
    A = lambda fn, r, w: P.op("act", fn, reads=r, writes=w)
    G = lambda fn, r, w: P.op("dve", fn, reads=r, writes=w)
    PE = lambda fn, r, w: P.op("pe", fn, reads=r, writes=w)

    A(lambda e: e.activation(S["beta"][:], S["bl"][:], AF.Sigmoid), [S["bl"]], [S["beta"]])
    D(lambda e: e.tensor_scalar(S["xb"][:], S["al"][:], rv[:, 1:2], None, ALU.add), [S["al"], rv], [S["xb"]])
    A(lambda e: e.activation(S["ax"][:], S["xb"][:], AF.Abs), [S["xb"]], [S["ax"]])
    A(lambda e: e.activation(S["ex"][:], S["ax"][:], AF.Exp, scale=-1.0), [S["ax"]], [S["ex"]])
    A(lambda e: e.activation(S["ln"][:], S["ex"][:], AF.Ln, bias=1.0, scale=1.0), [S["ex"]], [S["ln"]])
    D(lambda e: e.scalar_tensor_tensor(S["sp"][:], S["xb"][:], 0.0, S["ln"][:], ALU.max, ALU.add),
      [S["xb"], S["ln"]], [S["sp"]])
    A(lambda e: e.activation(S["aneg"][:, 0:1], rv[:, 0:1], AF.Exp), [rv], [S["aneg"]])
    D(lambda e: e.tensor_scalar(S["g"][:], S["sp"][:], S["aneg"][:, 0:1], -1.0, ALU.mult, ALU.mult),
      [S["sp"], S["aneg"]], [S["g"]])
    for (dst, mat, fn) in (("gcs", TRI, AF.Copy), ("gtot", BLK, AF.Copy), ("dec0", SEL0, AF.Exp), ("dec1", SEL1, AF.Exp)):
        PE(lambda e: e.matmul(pq[0][:, 0:64], mat, S["g"][:], start=True, stop=True), [cm, S["g"]], [pq[0]])
        A(lambda e: e.activation(S[dst][:], pq[0][:, 0:64], fn), [pq[0]], [S[dst]])
    A(lambda e: e.activation(S["eg"][:], S["gcs"][:], AF.Exp), [S["gcs"]], [S["eg"]])
    D(lambda e: e.tensor_tensor(S["kdsc"][:], S["gtot"][:], S["gcs"][:], ALU.subtract), [S["gtot"], S["gcs"]], [S["kdsc"]])
    A(lambda e: e.activation(S["kdsc"][:], S["kdsc"][:], AF.Exp), [S["kdsc"]], [S["kdsc"]])
    D(lambda e: e.tensor_tensor(S["beg"][:], S["beta"][:], S["eg"][:], ALU.mult), [S["beta"], S["eg"]], [S["beg"]])
    D(lambda e: e.memset(St[0][:], 0.0), [], [St[0]])

    def load_group(g):
        i = g % 2
        cs = slice(g * 512, (g + 1) * 512)
        P.dma("sp", qTg[i][:], qT_d[:, cs], writes=[qTg[i]], sb=qTg[i])
        P.dma("act", kTg[i][:], kT_d[:, cs], writes=[kTg[i]], sb=kTg[i])
        P.dma("sp", kkg[i][:], kk_d[cs, :].rearrange("(n p) d -> p n d", p=128), writes=[kkg[i]], sb=kkg[i])
        P.dma("act", vkg[i][:], vk_d[cs, :].rearrange("(n p) d -> p n d", p=128), writes=[vkg[i]], sb=vkg[i])
        P.dma("sp", zkg[i][:], zk_d[cs, :].rearrange("(n p) d -> p n d", p=128), writes=[zkg[i]], sb=zkg[i])

    NG = NTILE // GT
    load_group(0)
    cur = 0
    for g in range(NG):
        if g + 1 < NG:
            load_group(g + 1)
        i = g % 2
        tiles = range(GT)
        col = lambda gi: slice(g * GT + gi, g * GT + gi + 1)
        kT = lambda gi: kTg[i][:, gi * 128:(gi + 1) * 128]
        qT = lambda gi: qTg[i][:, gi * 128:(gi + 1) * 128]
        qa = lambda gi: A_q[gi][0]
        qb = lambda gi: A_q[gi][1]
        qc = lambda gi: A_q[gi][2]
        qd = lambda gi: B_q[gi][3]
        for gi in tiles:
            PE(lambda e: e.matmul(qa(gi)[:], S["g"][:, col(gi)].broadcast_to([128, 128]), TRI, start=True, stop=True),
               [S["g"], cm], [qa(gi)])
            PE(lambda e: e.matmul(qb(gi)[:], kT(gi), kT(gi), start=True, stop=True), [kTg[i]], [qb(gi)])
            PE(lambda e: e.matmul(qc(gi)[:], kT(gi), qT(gi), start=True, stop=True), [kTg[i], qTg[i]], [qc(gi)])
        for gi in tiles:
            w = lambda k: W[k, gi]
            gc_ = S["gcs"][:, col(gi)]
            D(lambda e: e.tensor_copy(w("rbs")[:], qa(gi)[:]), [qa(gi)], [w("rbs")])
            D(lambda e: e.tensor_scalar(w("Y")[:], w("rbs")[:], gc_, 0.0, ALU.subtract, ALU.max), [w("rbs"), S["gcs"]], [w("Y")])
            D(lambda e: e.tensor_scalar(w("YT")[:], w("rbs")[:], gc_, 0.0, ALU.subtract, ALU.min), [w("rbs"), S["gcs"]], [w("YT")])
            A(lambda e: e.activation(w("erb")[:], w("rbs")[:], AF.Exp), [w("rbs")], [w("erb")])
            A(lambda e: e.activation(w("decn")[:], w("Y")[:], AF.Exp, scale=-1.0), [w("Y")], [w("decn")])
            A(lambda e: e.activation(w("dect")[:], w("YT")[:], AF.Exp), [w("YT")], [w("dect")])
            D(lambda e: e.tensor_tensor(w("t")[:], qb(gi)[:], w("decn")[:], ALU.mult), [qb(gi), w("decn")], [w("t")])
            D(lambda e: e.scalar_tensor_tensor(w("N")[:], w("t")[:], S["beta"][:, col(gi)], MASKS, ALU.mult, ALU.mult),
              [w("t"), S["beta"], cm], [w("N")])
            G(lambda e: e.tensor_tensor(w("dect")[:], w("dect")[:], TRI, ALU.mult), [w("dect"), cm], [w("dect")])
            D(lambda e: e.tensor_tensor(w("QKm")[:], qc(gi)[:], w("dect")[:], ALU.mult), [qc(gi), w("dect")], [w("QKm")])
            G(lambda e: e.tensor_tensor(w("qeg")[:], qT(gi), w("erb")[:], ALU.mult), [qTg[i], w("erb")], [w("qeg")])
            G(lambda e: e.tensor_scalar(w("R0")[:, 0:128], vkg[i][:, gi, :], S["beta"][:, col(gi)], None, ALU.mult),
              [vkg[i], S["beta"]], [w("R0")])
            G(lambda e: e.tensor_scalar(w("R0")[:, 128:256], kkg[i][:, gi, :], S["beg"][:, col(gi)], None, ALU.mult),
              [kkg[i], S["beg"]], [w("R0")])
            G(lambda e: e.tensor_scalar(w("kd")[:], kkg[i][:, gi, :], S["kdsc"][:, col(gi)], None, ALU.mult),
              [kkg[i], S["kdsc"]], [w("kd")])
        for gi in tiles:
            PE(lambda e: e.matmul(qd(gi)[:], W["N", gi][:], IDENT, start=True, stop=True), [W["N", gi], cm], [qd(gi)])
        for gi in tiles:
            A(lambda e: e.activation(W["M", gi][:], qd(gi)[:], AF.Copy), [qd(gi)], [W["M", gi]])
        Mc = {gi: W["M", gi] for gi in tiles}
        Nc = {gi: W["N", gi] for gi in tiles}
        Rc = {gi: W["R0", gi] for gi in tiles}
        for lvl in range(6):
            sign = ALU.subtract if lvl == 0 else ALU.add
            apb = (lambda gi: A_h[gi][0]) if lvl % 2 == 0 else (lambda gi: A_h[gi][1])
            pm = (lambda gi: B_q[gi][0]) if lvl % 2 == 0 else (lambda gi: B_q[gi][2])
            pn = (lambda gi: B_q[gi][1]) if lvl % 2 == 0 else (lambda gi: B_q[gi][3])
            for gi in tiles:
                PE(lambda e: e.matmul(apb(gi)[:], Mc[gi][:], Rc[gi][:], start=True, stop=True), [Mc[gi], Rc[gi]], [apb(gi)])
                if lvl < 5:
                    PE(lambda e: e.matmul(pm(gi)[:], Nc[gi][:], Mc[gi][:], start=True, stop=True), [Nc[gi], Mc[gi]], [pm(gi)])
                if lvl < 4:
                    PE(lambda e: e.matmul(pn(gi)[:], Mc[gi][:], Nc[gi][:], start=True, stop=True), [Nc[gi], Mc[gi]], [pn(gi)])
            for gi in tiles:
                Rn = W["R1", gi] if Rc[gi] is W["R0", gi] else W["R0", gi]
                D(lambda e: e.tensor_tensor(Rn[:], Rc[gi][:], apb(gi)[:], sign), [Rc[gi], apb(gi)], [Rn])
                Rc[gi] = Rn
                if lvl < 5:
                    Mn = W["Ma", gi] if lvl % 2 == 0 else W["Mb", gi]
                    A(lambda e: e.activation(Mn[:], pm(gi)[:], AF.Copy), [pm(gi)], [Mn])
                if lvl < 4:
                    Nn = W["Na", gi] if lvl % 2 == 0 else W["Nb", gi]
                    A(lambda e: e.activation(Nn[:], pn(gi)[:], AF.Copy), [pn(gi)], [Nn])
                if lvl < 5:
                    Mc[gi] = Mn
                if lvl < 4:
                    Nc[gi] = Nn
        for gi in tiles:
            PE(lambda e: e.matmul(B_q[gi][0][:], Rc[gi][:, 128:256], IDENT, start=True, stop=True), [Rc[gi], cm], [B_q[gi][0]])
        for gi in tiles:
            A(lambda e: e.activation(W["kcdT", gi][:], B_q[gi][0][:], AF.Copy), [B_q[gi][0]], [W["kcdT", gi]])
        for gi in tiles:
            w = lambda k: W[k, gi]
            for c in range(2):
                lo, hi = c * 64, (c + 1) * 64
                s_old, s_new = St[cur], St[1 - cur]
                PE(lambda e: e.matmul(A_q[gi][0][:], w("kcdT")[:], s_old[:], start=True, stop=True), [w("kcdT"), s_old], [A_q[gi][0]])
                D(lambda e: e.tensor_tensor(w("u")[lo:hi, :], Rc[gi][lo:hi, 0:128], A_q[gi][0][lo:hi, :], ALU.subtract),
                  [Rc[gi], A_q[gi][0]], [w("u")])
                P.op("pe", lambda e: e.matmul(B_q[gi][1][:], w("qeg")[:], s_old[:], start=True, stop=False),
                     reads=[w("qeg"), s_old], writes=[B_q[gi][1]], defer=True)
                PE(lambda e: e.matmul(B_q[gi][1][:], w("QKm")[lo:hi, :], w("u")[lo:hi, :], start=False, stop=True),
                   [w("QKm"), w("u")], [B_q[gi][1]])
                PE(lambda e: e.matmul(A_q[gi][1][:], w("kd")[lo:hi, :], w("u")[lo:hi, :], start=True, stop=True),
                   [w("kd"), w("u")], [A_q[gi][1]])
                dec = S["dec0"] if c == 0 else S["dec1"]
                D(lambda e: e.scalar_tensor_tensor(s_new[:], s_old[:], dec[:, col(gi)], A_q[gi][1][:], ALU.mult, ALU.add),
                  [s_old, dec, A_q[gi][1]], [s_new])
                A(lambda e: e.activation(w("o")[lo:hi, :], B_q[gi][1][lo:hi, :], AF.Copy), [B_q[gi][1]], [w("o")])
                cur = 1 - cur
            A(lambda e: e.activation(w("sz")[:], w("o")[:], AF.Square, accum_out=w("ss")[:]), [w("o")], [w("sz"), w("ss")])
            A(lambda e: e.activation(w("rs")[:], w("ss")[:], AF.Sqrt, bias=EPS, scale=1.0 / 128), [w("ss")], [w("rs")])
            D(lambda e: e.reciprocal(w("rs")[:], w("rs")[:]), [w("rs")], [w("rs")])
            D(lambda e: e.scalar_tensor_tensor(w("o")[:], w("o")[:], w("rs")[:, 0:1], gn[:], ALU.mult, ALU.mult),
              [w("o"), w("rs"), gn], [w("o")])
            A(lambda e: e.activation(w("sz")[:], zkg[i][:, gi, :], AF.Silu), [zkg[i]], [w("sz")])
            D(lambda e: e.tensor_tensor(og[i][:, gi, :], w("o")[:], w("sz")[:], ALU.mult), [w("o"), w("sz")], [og[i]])
        P.dma("sp", out_d[g * 512:(g + 1) * 512, :].rearrange("(n p) d -> p n d", p=128), og[i][:],
              reads=[og[i]], sb=og[i])
    P.wait_all("sp", og)
    P.close()
    return nc


def build_ssd():
    T, NTILE, H, PD, NS = 8192, 64, 8, 64, 128
    nc = bass.Bass("TRN2", target_bir_lowering=False)
    P = Prog(nc)
    x_d = P.dram("x_tok", [T, 512], F32, "ExternalInput")
    z_d = P.dram("z_tok", [T, 512], F32, "ExternalInput")
    bt_d = P.dram("BT", [128, T], F32, "ExternalInput")
    ct_d = P.dram("CT", [128, T], F32, "ExternalInput")
    bk_d = P.dram("B_tok", [T, 128], F32, "ExternalInput")
    dtr_d = P.dram("dtr", [128, 512], F32, "ExternalInput")
    rv_d = P.dram("rowvec", [128, 24], F32, "ExternalInput")
    cm_d = P.dram("cmat", [128, 512], F32, "ExternalInput")
    out_d = P.dram("yz_tok", [T, 512], F32, "ExternalOutput")

    cm = P.sbuf([128, 512], F32, "cm")
    TRI, BLK, SEL0, SEL1 = (cm[:, i * 128:(i + 1) * 128] for i in range(4))
    rv = P.sbuf([128, 24], F32, "rv")
    dtr = P.sbuf([128, 512], F32, "dtr")
    names = ("xb", "ax", "ex", "ln", "dt", "da", "acs", "atot", "eacs", "dte", "dec0", "dec1", "aneg")
    S = {k: P.sbuf([128, 512], F32, "s_" + k) for k in names}
    ST = [P.sbuf([128, 512], F32, f"ST{i}") for i in range(2)]
    NB = 3
    xt = [P.sbuf([128, 512], F32, f"xt{i}") for i in range(NB)]
    zt = [P.sbuf([128, 512], F32, f"zt{i}") for i in range(NB)]
    btt = [P.sbuf([128, 128], F32, f"bt{i}") for i in range(NB)]
    ctt = [P.sbuf([128, 128], F32, f"ct{i}") for i in range(NB)]
    bkt = [P.sbuf([128, 128], F32, f"bk{i}") for i in range(NB)]
    xdd = [P.sbuf([128, 512], F32, f"xdd{i}") for i in range(2)]
    xdt = [P.sbuf([128, 512], F32, f"xdt{i}") for i in range(2)]
    cbm = [P.sbuf([128, 128], F32, f"cbm{i}") for i in range(2)]
    xx = [P.sbuf([128, 128], F32, f"xx{i}") for i in range(4)]
    mt = [P.sbuf([128, 128], F32, f"mt{i}") for i in range(4)]
    yt = [P.sbuf([128, 512], F32, f"yt{i}") for i in range(2)]
    uo = [P.sbuf([128, 512], F32, f"uo{i}") for i in range(2)]
    bank = [P.psum([128, 512], F32, f"bank{i}") for i in range(8)]

    P.dma("sp", cm[:], cm_d[:], writes=[cm], sb=cm)
    P.dma("sp", rv[:], rv_d[:], writes=[rv], sb=rv)
    P.dma("sp", dtr[:], dtr_d[:], writes=[dtr], sb=dtr)

    def bc8(ap):
        return ap.unsqueeze(2).broadcast_to([ap.shape[0], 8, 64])

    def v3(ap):
        return ap.rearrange("p (h d) -> p h d", d=64)

    def rep(ap):
        return ap.unsqueeze(1).broadcast_to([128, 64, 8])

    def t3(ap):
        return ap.rearrange("p (n h) -> p n h", h=8)

    D = lambda fn, r, w: P.op("dve", fn, reads=r, writes=w)
    A = lambda fn, r, w: P.op("act", fn, reads=r, writes=w)
    D(lambda e: e.tensor_tensor(t3(S["xb"][:]), t3(dtr[:]), rep(rv[:, 0:8]), ALU.add), [dtr, rv], [S["xb"]])
    A(lambda e: e.activation(S["ax"][:], S["xb"][:], AF.Abs), [S["xb"]], [S["ax"]])
    A(lambda e: e.activation(S["ex"][:], S["ax"][:], AF.Exp, scale=-1.0), [S["ax"]], [S["ex"]])
    A(lambda e: e.activation(S["ln"][:], S["ex"][:], AF.Ln, bias=1.0, scale=1.0), [S["ex"]], [S["ln"]])
    D(lambda e: e.scalar_tensor_tensor(S["dt"][:], S["xb"][:], 0.0, S["ln"][:], ALU.max, ALU.add),
      [S["xb"], S["ln"]], [S["dt"]])
    A(lambda e: e.activation(S["aneg"][:, 0:8], rv[:, 8:16], AF.Exp), [rv], [S["aneg"]])
    D(lambda e: e.tensor_tensor(t3(S["da"][:]), t3(S["dt"][:]), rep(S["aneg"][:, 0:8]), ALU.mult),
      [S["dt"], S["aneg"]], [S["da"]])
    D(lambda e: e.tensor_scalar(S["da"][:], S["da"][:], -1.0, None, ALU.mult), [S["da"]], [S["da"]])
    for (dst, mat) in (("acs", TRI), ("atot", BLK), ("dec0", SEL0), ("dec1", SEL1)):
        P.op("pe", lambda e: e.matmul(bank[0][:], mat, S["da"][:], start=True, stop=True),
             reads=[cm, S["da"]], writes=[bank[0]])
        if dst in ("dec0", "dec1"):
            A(lambda e: e.activation(S[dst][:], bank[0][:], AF.Exp), [bank[0]], [S[dst]])
        else:
            A(lambda e: e.activation(S[dst][:], bank[0][:], AF.Copy), [bank[0]], [S[dst]])
    A(lambda e: e.activation(S["eacs"][:], S["acs"][:], AF.Exp), [S["acs"]], [S["eacs"]])
    D(lambda e: e.tensor_tensor(S["dte"][:], S["atot"][:], S["acs"][:], ALU.subtract), [S["atot"], S["acs"]], [S["dte"]])
    A(lambda e: e.activation(S["dte"][:], S["dte"][:], AF.Exp), [S["dte"]], [S["dte"]])
    D(lambda e: e.tensor_tensor(S["dte"][:], S["dte"][:], S["dt"][:], ALU.mult), [S["dte"], S["dt"]], [S["dte"]])
    D(lambda e: e.memset(ST[0][:], 0.0), [], [ST[0]])

    def load_tile(n):
        i = n % NB
        r = slice(n * 128, (n + 1) * 128)
        P.dma("sp", xt[i][:], x_d[r, :], writes=[xt[i]], sb=xt[i])
        P.dma("act", zt[i][:], z_d[r, :], writes=[zt[i]], sb=zt[i])
        P.dma("sp", btt[i][:], bt_d[:, r], writes=[btt[i]], sb=btt[i])
        P.dma("act", ctt[i][:], ct_d[:, r], writes=[ctt[i]], sb=ctt[i])
        P.dma("sp", bkt[i][:], bk_d[r, :], writes=[bkt[i]], sb=bkt[i])

    load_tile(0)
    load_tile(1)
    cur = 0
    outs = []
    for n in range(NTILE):
        if n + 2 < NTILE:
            load_tile(n + 2)
        i = n % NB
        X, Z, BT, CT, BK = xt[i], zt[i], btt[i], ctt[i], bkt[i]
        sc = slice(n * 8, (n + 1) * 8)
        xd, xe = xdt[n % 2], xdd[n % 2]
        D(lambda e: e.tensor_tensor(v3(xd[:]), v3(X[:]), bc8(S["dt"][:, sc]), ALU.mult), [X, S["dt"]], [xd])
        D(lambda e: e.tensor_tensor(v3(xe[:]), v3(X[:]), bc8(S["dte"][:, sc]), ALU.mult), [X, S["dte"]], [xe])
        P.op("pe", lambda e: e.matmul(bank[1][:, 0:128], BT[:], CT[:], start=True, stop=True),
             reads=[BT, CT], writes=[bank[1]])
        cb = cbm[n % 2]
        D(lambda e: e.tensor_tensor(cb[:], bank[1][:, 0:128], TRI, ALU.mult), [bank[1], cm], [cb])
        yb = bank[2 + n % 2]
        for h in range(H):
            col = n * 8 + h
            rb = bank[4 + h % 2]
            P.op("pe", lambda e: e.matmul(rb[:, 0:128], S["da"][:, col:col + 1].broadcast_to([128, 128]), TRI,
                                          start=True, stop=True),
                 reads=[S["da"], cm], writes=[rb])
            x_ = xx[h % 4]
            D(lambda e: e.tensor_scalar(x_[:], rb[:, 0:128], S["acs"][:, col:col + 1], 0.0, ALU.subtract, ALU.min),
              [rb, S["acs"]], [x_])
            A(lambda e: e.activation(x_[:], x_[:], AF.Exp), [x_], [x_])
            m_ = mt[h % 4]
            D(lambda e: e.tensor_tensor(m_[:], x_[:], cb[:], ALU.mult), [x_, cb], [m_])
            P.op("pe", lambda e: e.matmul(yb[:, h * 64:(h + 1) * 64], m_[:], xd[:, h * 64:(h + 1) * 64],
                                          start=True, stop=True),
                 reads=[m_, xd], writes=[yb])
        y = yt[n % 2]
        D(lambda e: e.tensor_tensor(v3(y[:]), v3(X[:]), bc8(rv[:, 16:24]), ALU.mult), [X, rv], [y])
        D(lambda e: e.tensor_tensor(y[:], y[:], yb[:], ALU.add), [y, yb], [y])
        for c in range(2):
            lo, hi = c * 64, (c + 1) * 64
            s_old, s_new = ST[cur], ST[1 - cur]
            yo = bank[6]
            P.op("pe", lambda e: e.matmul(yo[:], CT[:], s_old[:], start=True, stop=True),
                 reads=[CT, s_old], writes=[yo])
            csb = bank[7]
            P.op("pe", lambda e: e.matmul(csb[:], BK[lo:hi, :], xe[lo:hi, :], start=True, stop=True),
                 reads=[BK, xe], writes=[csb])
            u = uo[c]
            D(lambda e: e.tensor_tensor(v3(u[lo:hi, :]), v3(yo[lo:hi, :]), bc8(S["eacs"][lo:hi, sc]), ALU.mult),
              [yo, S["eacs"]], [u])
            D(lambda e: e.tensor_tensor(y[lo:hi, :], y[lo:hi, :], u[lo:hi, :], ALU.add), [y, u], [y])
            dec = S["dec0"] if c == 0 else S["dec1"]
            D(lambda e: e.tensor_tensor(v3(s_new[:]), v3(s_old[:]), bc8(dec[:, sc]), ALU.mult), [s_old, dec], [s_new])
            D(lambda e: e.tensor_tensor(s_new[:], s_new[:], csb[:], ALU.add), [s_new, csb], [s_new])
            cur = 1 - cur
        A(lambda e: e.activation(Z[:], Z[:], AF.Silu), [Z], [Z])
        D(lambda e: e.tensor_tensor(y[:], y[:], Z[:], ALU.mult), [y, Z], [y])
        P.dma("sp", out_d[n * 128:(n + 1) * 128, :], y[:], reads=[y], sb=y)
    P.wait_all("sp", yt)
    P.close()
    return nc


def build_tail(layer):
    rms = layer == 1
    final = layer == 1
    C = 4096 if layer == 1 else 2048
    CC = C // 128
    NT = 1024
    nc = bass.Bass("TRN2", target_bir_lowering=False)
    P = Prog(nc)
    xT_d = P.dram("xT", [2048, NT], F32, "ExternalInput")
    cat_d = P.dram("catT", [C, NT], F32, "ExternalInput")
    vec_d = P.dram("vec", [128, 6 * 16], F32, "ExternalInput")
    gc_d = P.dram("gcat", [128, CC], F32, "ExternalInput")
    wout_d = P.dram("wout", [C, 2048], F32, "ExternalInput")
    wr_d = P.dram("wr", [2048, 36], F32, "ExternalInput")
    br_d = P.dram("br", [128, 36], F32, "ExternalInput")
    w1_d = P.dram("w1", [32, 2048, 512], F32, "ExternalInput")
    w3_d = P.dram("w3", [32, 2048, 512], F32, "ExternalInput")
    w2_d = P.dram("w2", [32, 512, 2048], F32, "ExternalInput")
    id_d = P.dram("ident", [128, 128], F32, "ExternalInput")
    out_d = P.dram("outT", [2048, NT], F32, "ExternalOutput")

    xT = [[P.sbuf([128, 512], F32, f"x{j}_{h}") for h in range(2)] for j in range(16)]
    hT = [P.sbuf([128, 512], BF16, f"h{i}") for i in range(32)]
    wsl = [P.sbuf([128, 8192], BF16, f"wsl{i}") for i in range(4)]
    aT = [[P.sbuf([128, 512], BF16, f"a{j}_{h}") for h in range(2)] for j in range(4)]
    G = [P.sbuf([128, 1024], F32, f"G{i}") for i in range(2)]
    tmp1 = [P.sbuf([128, 512], F32, f"t1_{i}") for i in range(2)]
    tmp2 = [P.sbuf([128, 512], F32, f"t2_{i}") for i in range(2)]
    stg = [P.sbuf([128, 512], F32, f"stg{i}") for i in range(2)]
    sqb = [P.sbuf([128, 512], F32, f"sq{i}") for i in range(2)]
    rstd = [P.sbuf([128, 512], F32, f"rstd{i}") for i in range(2)]
    ones = P.sbuf([128, 128], F32, "ones")
    ident = P.sbuf([128, 128], F32, "ident")
    wr = P.sbuf([128, 16, 36], F32, "wr")
    br = P.sbuf([128, 36], F32, "br")
    vec = P.sbuf([128, 96], F32, "vec")
    gc = P.sbuf([128, CC], F32, "gc")
    gs2 = P.sbuf([128, 16], F32, "gs2")
    gd = [P.sbuf([128, 32], F32, f"gd{i}") for i in range(8)]
    sm = {k: P.sbuf([128, 36], F32, "sm_" + k) for k in
          ("lg", "ge", "ohg", "pen", "em", "oh1", "em2", "oh2")}
    sc = {k: P.sbuf([128, 1], F32, "sc_" + k) for k in
          ("gmax", "ngmax", "gsum", "grp", "m1", "m2", "d", "ed", "den", "p1", "p2", "p1g", "p2g")}
    bank = [P.psum([128, 512], F32, f"bank{i}") for i in range(8)]

    def V(i):
        return vec[:, i * 16:(i + 1) * 16]
    GATE_MIX, SHIFT2, SCALE2, GATE_FFN, NORMG2, FING = range(6)

    P.dma("sp", vec[:], vec_d[:], writes=[vec], sb=vec)
    P.dma("sp", gc[:], gc_d[:], writes=[gc], sb=gc)
    P.dma("sp", ident[:], id_d[:], writes=[ident], sb=ident)
    P.dma("sp", wr[:], wr_d.t.rearrange("(j p) n -> p j n", p=128), writes=[wr], sb=wr)
    P.dma("sp", br[:], br_d[:], writes=[br], sb=br)
    P.op("dve", lambda e: e.memset(ones[:], 1.0), writes=[ones])
    for j in range(16):
        for h in range(2):
            b = xT[j][h]
            P.dma("act" if (j + h) % 2 else "sp", b[:], xT_d[j * 128:(j + 1) * 128, h * 512:(h + 1) * 512],
                  writes=[b], sb=b)

    ring = {"n": 0}

    def wload(src_ap, kind):
        s = wsl[ring["n"] % 4]
        ring["n"] += 1
        if kind == "k16":
            dst = s[:].rearrange("p (j n) -> p j n", n=512)
            src = src_ap.rearrange("(j p) n -> p j n", p=128)
        else:
            dst = s[:].rearrange("p (j n) -> p j n", n=2048)
            src = src_ap.rearrange("(j p) n -> p j n", p=128)
        P.dma("pool", dst, src, writes=[s], sb=s)
        return s, dst

    def sum_sq_rstd(srcs, h, n_feat):
        n = len(srcs)
        for i, (sbuf_, ap) in enumerate(srcs):
            q = sqb[i % 2]
            P.op("act", lambda e: e.activation(q[:], ap, AF.Square), reads=[sbuf_], writes=[q])
            P.op("pe", lambda e: e.matmul(bank[6][:], ones[:], q[:], start=(i == 0), stop=(i == n - 1)),
                 reads=[ones, q], writes=[bank[6]])
        t = tmp1[0]
        P.op("act", lambda e: e.activation(t[:], bank[6][:], AF.Sqrt, bias=EPS, scale=1.0 / n_feat),
             reads=[bank[6]], writes=[t])
        P.op("dve", lambda e: e.reciprocal(rstd[h][:], t[:]), reads=[t], writes=[rstd[h]])

    nmm = 0
    for h in range(2):
        cbufs = []
        for cc in range(CC):
            s = stg[cc % 2]
            P.dma("sp" if cc % 2 else "act", s[:], cat_d[cc * 128:(cc + 1) * 128, h * 512:(h + 1) * 512],
                  writes=[s], sb=s)
            if rms:
                q = sqb[cc % 2]
                P.op("act", lambda e: e.activation(q[:], s[:], AF.Square), reads=[s], writes=[q])
                P.op("pe", lambda e: e.matmul(bank[6][:], ones[:], q[:], start=(cc == 0), stop=(cc == CC - 1)),
                     reads=[ones, q], writes=[bank[6]])
            dst = hT[cc] if CC == 32 else hT[h * 16 + cc]
            P.op("dve", lambda e: e.tensor_scalar(dst[:], s[:], gc[:, cc:cc + 1], None, ALU.mult),
                 reads=[s, gc], writes=[dst])
            cbufs.append(dst)
        if rms:
            t = tmp1[0]
            P.op("act", lambda e: e.activation(t[:], bank[6][:], AF.Sqrt, bias=EPS, scale=1.0 / C),
                 reads=[bank[6]], writes=[t])
            P.op("dve", lambda e: e.reciprocal(rstd[h][:], t[:]), reads=[t], writes=[rstd[h]])
        for cb in range(4):
            slots = []
            for rb in range(C // 2048):
                slots.append(wload(wout_d[rb * 2048:(rb + 1) * 2048, cb * 512:(cb + 1) * 512], "k16"))
            for fc in range(4):
                j = cb * 4 + fc
                bk = bank[nmm % 2]
                nmm += 1
                for cc in range(CC):
                    sb_, view = slots[cc // 16]
                    P.op("pe", lambda e: e.matmul(bk[:], view[:, cc % 16, fc * 128:(fc + 1) * 128], cbufs[cc][:],
                                                  start=(cc == 0), stop=(cc == CC - 1)),
                         reads=[sb_, cbufs[cc]], writes=[bk], defer=(cc != CC - 1))
                xb = xT[j][h]
                if rms:
                    t = tmp2[j % 2]
                    P.op("dve", lambda e: e.tensor_tensor(t[:], bk[:], rstd[h][:], ALU.mult),
                         reads=[bk, rstd[h]], writes=[t])
                    P.op("dve", lambda e: e.scalar_tensor_tensor(xb[:], t[:], V(GATE_MIX)[:, j:j + 1], xb[:],
                                                                 ALU.mult, ALU.add),
                         reads=[t, vec, xb], writes=[xb])
                else:
                    P.op("dve", lambda e: e.scalar_tensor_tensor(xb[:], bk[:], V(GATE_MIX)[:, j:j + 1], xb[:],
                                                                 ALU.mult, ALU.add),
                         reads=[bk, vec, xb], writes=[xb])

    P.op("dve", lambda e: e.tensor_scalar(gs2[:], V(SCALE2), 1.0, None, ALU.add), reads=[vec], writes=[gs2])
    P.op("dve", lambda e: e.tensor_tensor(gs2[:], gs2[:], V(NORMG2), ALU.mult), reads=[gs2, vec], writes=[gs2])
    for h in range(2):
        sum_sq_rstd([(xT[j][h], xT[j][h][:]) for j in range(16)], h, 2048)
        for j in range(16):
            hf = stg[j % 2]
            P.op("dve", lambda e: e.tensor_tensor(hf[:], xT[j][h][:], rstd[h][:], ALU.mult),
                 reads=[xT[j][h], rstd[h]], writes=[hf])
            P.op("dve", lambda e: e.tensor_scalar(hf[:], hf[:], gs2[:, j:j + 1], V(SHIFT2)[:, j:j + 1],
                                                  ALU.mult, ALU.add),
                 reads=[hf, gs2, vec], writes=[hf])
            hb = hT[j * 2 + h]
            P.op("act", lambda e: e.activation(hb[:], hf[:], AF.Copy), reads=[hf], writes=[hb])
            for tt in range(4):
                bk = bank[2 + tt]
                P.op("pe", lambda e: e.matmul(bk[:, 0:36], hf[:, tt * 128:(tt + 1) * 128], wr[:, j, :],
                                              start=(j == 0), stop=(j == 15)),
                     reads=[hf, wr], writes=[bk], defer=not (j == 15 or tt == 3))
        for tt in range(4):
            bk = bank[2 + tt]
            g = gd[h * 4 + tt]
            lg, ge, ohg, pen, em, oh1, em2, oh2 = (sm[k] for k in ("lg", "ge", "ohg", "pen", "em", "oh1", "em2", "oh2"))
            D = lambda fn, r, w: P.op("dve", fn, reads=r, writes=w)
            A = lambda fn, r, w: P.op("act", fn, reads=r, writes=w)
            D(lambda e: e.tensor_tensor(lg[:], bk[:, 0:36], br[:], ALU.add), [bk, br], [lg])
            D(lambda e: e.tensor_reduce(sc["gmax"][:], lg[:, 0:4], AX.X, ALU.max), [lg], [sc["gmax"]])
            D(lambda e: e.tensor_scalar(sc["ngmax"][:], sc["gmax"][:], -1.0, None, ALU.mult), [sc["gmax"]], [sc["ngmax"]])
            A(lambda e: e.activation(ge[:, 0:4], lg[:, 0:4], AF.Exp, bias=sc["ngmax"][:, 0:1], scale=1.0),
              [lg, sc["ngmax"]], [ge])
            D(lambda e: e.tensor_reduce(sc["gsum"][:], ge[:, 0:4], AX.X, ALU.add), [ge], [sc["gsum"]])
            D(lambda e: e.reciprocal(sc["grp"][:], sc["gsum"][:]), [sc["gsum"]], [sc["grp"]])
            D(lambda e: e.tensor_scalar(ohg[:, 0:4], lg[:, 0:4], sc["gmax"][:, 0:1], None, ALU.is_equal),
              [lg, sc["gmax"]], [ohg])
            D(lambda e: e.tensor_scalar(pen[:, 0:4], ohg[:, 0:4], 1e30, -1e30, ALU.mult, ALU.add), [ohg], [pen])
            D(lambda e: e.tensor_tensor(em[:, 0:32].rearrange("p (g k) -> p g k", k=8),
                                        lg[:, 4:36].rearrange("p (g k) -> p g k", k=8),
                                        pen[:, 0:4].unsqueeze(2).broadcast_to([128, 4, 8]), ALU.add),
              [lg, pen], [em])
            D(lambda e: e.tensor_reduce(sc["m1"][:], em[:, 0:32], AX.X, ALU.max), [em], [sc["m1"]])
            D(lambda e: e.tensor_scalar(oh1[:, 0:32], em[:, 0:32], sc["m1"][:, 0:1], None, ALU.is_equal),
              [em, sc["m1"]], [oh1])
            D(lambda e: e.scalar_tensor_tensor(em2[:, 0:32], oh1[:, 0:32], -1e30, em[:, 0:32], ALU.mult, ALU.add),
              [oh1, em], [em2])
            D(lambda e: e.tensor_reduce(sc["m2"][:], em2[:, 0:32], AX.X, ALU.max), [em2], [sc["m2"]])
            D(lambda e: e.tensor_scalar(oh2[:, 0:32], em2[:, 0:32], sc["m2"][:, 0:1], None, ALU.is_equal),
              [em2, sc["m2"]], [oh2])
            D(lambda e: e.tensor_tensor(sc["d"][:], sc["m2"][:], sc["m1"][:], ALU.subtract),
              [sc["m2"], sc["m1"]], [sc["d"]])
            A(lambda e: e.activation(sc["ed"][:], sc["d"][:], AF.Exp), [sc["d"]], [sc["ed"]])
            D(lambda e: e.tensor_scalar(sc["den"][:], sc["ed"][:], 1.0, None, ALU.add), [sc["ed"]], [sc["den"]])
            D(lambda e: e.reciprocal(sc["p1"][:], sc["den"][:]), [sc["den"]], [sc["p1"]])
            D(lambda e: e.tensor_tensor(sc["p2"][:], sc["ed"][:], sc["p1"][:], ALU.mult),
              [sc["ed"], sc["p1"]], [sc["p2"]])
            D(lambda e: e.tensor_tensor(sc["p1g"][:], sc["p1"][:], sc["grp"][:], ALU.mult),
              [sc["p1"], sc["grp"]], [sc["p1g"]])
            D(lambda e: e.tensor_tensor(sc["p2g"][:], sc["p2"][:], sc["grp"][:], ALU.mult),
              [sc["p2"], sc["grp"]], [sc["p2g"]])
            D(lambda e: e.tensor_scalar(g[:], oh1[:, 0:32], sc["p1g"][:, 0:1], None, ALU.mult),
              [oh1, sc["p1g"]], [g])
            D(lambda e: e.scalar_tensor_tensor(g[:], oh2[:, 0:32], sc["p2g"][:, 0:1], g[:], ALU.mult, ALU.add),
              [oh2, sc["p2g"], g], [g])

    def emit_G(e_):
        for tt in range(8):
            bk = bank[6 + tt // 4]
            P.op("pe", lambda e: e.matmul(bk[:, (tt % 4) * 128:(tt % 4 + 1) * 128],
                                          gd[tt][:, e_:e_ + 1].broadcast_to([128, 128]), ident[:],
                                          start=True, stop=True),
                 reads=[gd[tt], ident], writes=[bk], defer=(tt % 4 != 3))
        gb = G[e_ % 2]
        for hh in range(2):
            P.op("act", lambda e: e.activation(gb[:, hh * 512:(hh + 1) * 512], bank[6 + hh][:], AF.Copy),
                 reads=[bank[6 + hh]], writes=[gb])

    NE = 32
    pend = []
    for m in range(2):
        pass
    loads = {}

    def issue_loads(e_):
        loads[e_] = (wload(w1_d[e_], "k16"), wload(w3_d[e_], "k16"), wload(w2_d[e_], "k4"))

    issue_loads(0)
    emit_G(0)
    k = 0
    for e_ in range(NE):
        (s1, v1), (s3, v3), (s2, v2) = loads[e_]
        gb = G[e_ % 2]
        for h in range(2):
            for jc in range(4):
                b1 = bank[k % 2]
                b3 = bank[2 + k % 2]
                for j in range(16):
                    P.op("pe", lambda e: e.matmul(b1[:], v1[:, j, jc * 128:(jc + 1) * 128], hT[j * 2 + h][:],
                                                  start=(j == 0), stop=(j == 15)),
                         reads=[s1, hT[j * 2 + h]], writes=[b1], defer=(j != 15))
                for j in range(16):
                    P.op("pe", lambda e: e.matmul(b3[:], v3[:, j, jc * 128:(jc + 1) * 128], hT[j * 2 + h][:],
                                                  start=(j == 0), stop=(j == 15)),
                         reads=[s3, hT[j * 2 + h]], writes=[b3], defer=(j != 15))
                t1 = tmp1[k % 2]
                t2 = tmp2[k % 2]
                P.op("act", lambda e: e.activation(t1[:], b1[:], AF.Silu), reads=[b1], writes=[t1])
                P.op("dve", lambda e: e.tensor_tensor(t2[:], t1[:], b3[:], ALU.mult), reads=[t1, b3], writes=[t2])
                P.op("dve", lambda e: e.tensor_tensor(aT[jc][h][:], t2[:], gb[:, h * 512:(h + 1) * 512], ALU.mult),
                     reads=[t2, gb], writes=[aT[jc][h]])
                k += 1
            if h == 0 and e_ + 1 < NE:
                pass
        if e_ + 1 < NE:
            emit_G(e_ + 1)
        for h in range(2):
            for fc in range(16):
                by = bank[4 + fc % 2]
                for jc in range(4):
                    P.op("pe", lambda e: e.matmul(by[:], v2[:, jc, fc * 128:(fc + 1) * 128], aT[jc][h][:],
                                                  start=(jc == 0), stop=(jc == 3)),
                         reads=[s2, aT[jc][h]], writes=[by], defer=(jc != 3))
                xb = xT[fc][h]
                P.op("dve", lambda e: e.scalar_tensor_tensor(xb[:], by[:], V(GATE_FFN)[:, fc:fc + 1], xb[:],
                                                             ALU.mult, ALU.add),
                     reads=[by, vec, xb], writes=[xb])
        if e_ + 1 < NE:
            issue_loads(e_ + 1)

    outs = []
    if final:
        for h in range(2):
            sum_sq_rstd([(xT[j][h], xT[j][h][:]) for j in range(16)], h, 2048)
            for j in range(16):
                xb = xT[j][h]
                P.op("dve", lambda e: e.tensor_tensor(xb[:], xb[:], rstd[h][:], ALU.mult),
                     reads=[xb, rstd[h]], writes=[xb])
                P.op("dve", lambda e: e.tensor_scalar(xb[:], xb[:], V(FING)[:, j:j + 1], None, ALU.mult),
                     reads=[xb, vec], writes=[xb])
    for j in range(16):
        for h in range(2):
            xb = xT[j][h]
            P.dma("sp" if (j + h) % 2 else "act", out_d[j * 128:(j + 1) * 128, h * 512:(h + 1) * 512], xb[:],
                  reads=[xb], sb=xb)
            outs.append(xb)
    P.wait_all("sp", outs)
    P.wait_all("act", outs)
    P.close()
    return nc

from concourse.bass_utils import run_bass_kernel_spmd

_CORES = list(range(8))


def _run(nc, maps):
    return run_bass_kernel_spmd(nc, maps, core_ids=_CORES).results


def tail_inputs(layer, xfull, cat, mod, inp):
    m_mix = mod[2 * layer]
    m_ffn = mod[2 * layer + 1]
    vec = np.concatenate([fm(m_mix[4096:]), fm(m_ffn[:2048]), fm(m_ffn[2048:4096]), fm(m_ffn[4096:]),
                          fm(inp["norm_g"][layer, 1]), fm(inp["final_norm_g"])], axis=1)
    vec = np.ascontiguousarray(vec.astype(np.float32))
    if layer == 0:
        gcat = np.ones(2048, np.float32)
        wout = inp["e_w_out"][0]
    else:
        gcat = inp["o_norm_g"][0]
        wout = inp["o_w_out"][0]
    wr = np.ascontiguousarray(np.concatenate([inp["moe_w_group"][layer], inp["moe_w_expert"][layer]], axis=1))
    br = np.concatenate([inp["moe_b_group"][layer], inp["moe_b_expert"][layer]])[None, :]
    br = np.ascontiguousarray(np.tile(br, (128, 1)).astype(np.float32))
    ident = np.eye(128, dtype=np.float32)
    w1 = np.ascontiguousarray(inp["moe_w1"][layer])
    w3 = np.ascontiguousarray(inp["moe_w3"][layer])
    w2 = np.ascontiguousarray(inp["moe_w2"][layer])
    maps = []
    for i in range(8):
        sl = slice(i * 1024, (i + 1) * 1024)
        maps.append({"xT": np.ascontiguousarray(xfull[sl].T), "catT": np.ascontiguousarray(cat[sl].T), "vec": vec,
                     "gcat": fm(gcat), "wout": np.ascontiguousarray(wout), "wr": wr, "br": br,
                     "w1": w1, "w3": w3, "w2": w2, "ident": ident})
    return maps


def kernel(**inputs):
    inp = {k: np.asarray(v, dtype=np.float32) for k, v in inputs.items()}
    x0 = inp["x"][0]
    c = inp["c"][0]
    cT = np.ascontiguousarray(c.reshape(16, 128).T)
    maps = [{"cT": cT, "wmod": np.ascontiguousarray(inp["w_mod"][:, :, i * 768:(i + 1) * 768]),
             "bmod": np.ascontiguousarray(inp["b_mod"][:, i * 768:(i + 1) * 768])} for i in range(8)]
    res = _run(build_mod(), maps)
    mod = np.concatenate([res[i]["mod"] for i in range(8)], axis=1)
    res = _run(build_pre(0), pre_inputs(0, x0, mod, inp))
    pout0 = np.concatenate([res[i]["pout"] for i in range(8)], axis=1)
    res = _run(build_gdn(), gdn_inputs(pout0, inp))
    a_out = np.concatenate([res[h]["o_tok"] for h in range(8)], axis=1)
    cat0 = np.concatenate([a_out, pout0[3072:4096].T], axis=1)
    res = _run(build_tail(0), tail_inputs(0, x0, cat0, mod, inp))
    x1 = np.concatenate([res[i]["outT"].T for i in range(8)], axis=0)
    res = _run(build_pre(1), pre_inputs(1, x1, mod, inp))
    pout1 = np.concatenate([res[i]["pout"] for i in range(8)], axis=1)
    res = _run(build_ssd(), ssd_inputs(pout1, inp))
    yz = np.concatenate([res[g]["yz_tok"] for g in range(8)], axis=1)
    res = _run(build_tail(1), tail_inputs(1, x1, yz, mod, inp))
    out = np.concatenate([res[i]["outT"].T for i in range(8)], axis=0)
    return np.ascontiguousarray(out[None].astype(np.float32))
```

```python
import numpy as np

from contextlib import ExitStack
import concourse.bass as bass
import concourse.mybir as mybir

F32 = mybir.dt.float32
BF16 = mybir.dt.bfloat16
I32 = mybir.dt.int32
AF = mybir.ActivationFunctionType
ALU = mybir.AluOpType
AX = mybir.AxisListType


class Buf:
    __slots__ = ("t", "name", "w", "r", "dsem")

    def __init__(self, t, name):
        self.t = t
        self.name = name
        self.w = None
        self.r = {}
        self.dsem = None

    def __getitem__(self, idx):
        return self.t[idx]


class View:
    __slots__ = ("buf", "ap")

    def __init__(self, buf, ap):
        self.buf = buf
        self.ap = ap

    def __getitem__(self, idx):
        return self.ap[idx]


class Prog:
    def __init__(self, nc):
        self.nc = nc
        self.st = ExitStack()
        self.eng = {"pe": nc.tensor, "act": nc.scalar, "dve": nc.vector,
                    "pool": nc.gpsimd, "sp": nc.sync}
        self.sems = {}
        self.cnt = {}
        self.waited = {e: {} for e in self.eng}
        for e in ("pe", "act", "dve", "pool"):
            self.sems[e] = self.st.enter_context(nc.semaphore("s_" + e))
            self.cnt[e] = 0
        self.nbuf = 0
        self.pending = {}
        self.dma_sem_free = []
        self.ndsem = 0

    def sbuf(self, shape, dt, name=None):
        self.nbuf += 1
        name = name or f"sb{self.nbuf}"
        t = self.st.enter_context(self.nc.sbuf_tensor("S_" + name, list(shape), dt))
        return Buf(t, name)

    def psum(self, shape, dt, name=None):
        self.nbuf += 1
        name = name or f"ps{self.nbuf}"
        t = self.st.enter_context(self.nc.psum_tensor("P_" + name, list(shape), dt))
        return Buf(t, name)

    def dram(self, name, shape, dt, kind):
        t = self.nc.dram_tensor(name, list(shape), dt, kind=kind)
        return Buf(t.ap(), name)

    def new_dma_sem(self):
        self.ndsem += 1
        k = f"d{self.ndsem}"
        self.sems[k] = self.st.enter_context(self.nc.semaphore("s_" + k))
        self.cnt[k] = 0
        return k

    def _wait(self, e, deps):
        eng = self.eng[e]
        need = {}
        for d in deps:
            if d is None:
                continue
            k, v = d
            if e == "pe" and k == "pe":
                continue
            if v > need.get(k, 0):
                need[k] = v
        for k, v in need.items():
            if self.waited[e].get(k, 0) < v:
                eng.wait_ge(self.sems[k], v)
                self.waited[e][k] = v

    def _deps(self, reads, writes):
        reads = [getattr(b, "buf", b) for b in reads]
        writes = [getattr(b, "buf", b) for b in writes]
        deps = []
        for b in reads:
            deps.append(b.w)
        for b in writes:
            deps.append(b.w)
            deps.extend(b.r.items())
        return deps

    def _mark(self, key, val, reads, writes):
        reads = [getattr(b, "buf", b) for b in reads]
        writes = [getattr(b, "buf", b) for b in writes]
        for b in reads:
            if b.r.get(key, 0) < val:
                b.r[key] = val
        for b in writes:
            b.w = (key, val)
            b.r = {}

    def op(self, e, fn, reads=(), writes=(), defer=False):
        self._wait(e, self._deps(reads, writes))
        inst = fn(self.eng[e])
        if defer:
            pr, pw = self.pending.setdefault(e, ([], []))
            pr.extend(reads)
            pw.extend(writes)
            self._mark(e, self.cnt[e] + 1, reads, writes)
            return inst
        self.cnt[e] += 1
        inst.then_inc(self.sems[e], 1)
        if e in self.pending:
            self.pending.pop(e)
        self._mark(e, self.cnt[e], reads, writes)
        return inst

    def dma(self, q, out, in_, reads=(), writes=(), sb=None, **kw):
        if sb.dsem is None:
            sb.dsem = self.new_dma_sem()
        sem = sb.dsem
        self._wait(q, self._deps(reads, writes))
        inst = self.eng[q].dma_start(out=out, in_=in_, **kw)
        self.cnt[sem] += 16
        inst.then_inc(self.sems[sem], 16)
        self._mark(sem, self.cnt[sem], reads, writes)
        return inst

    def wait_all(self, e, bufs):
        self._wait(e, self._deps(bufs, bufs))

    def close(self):
        self.st.close()

EPS = 1e-6


def fm(v):
    return np.ascontiguousarray(np.asarray(v, np.float32).reshape(-1, 128).T)

def fmk(w):
    K, n = w.shape[0], w.shape[1] // 128
    return np.ascontiguousarray(np.asarray(w, np.float32).reshape(K, n, 128).transpose(2, 0, 1).reshape(128, K * n))

def pre_inputs(layer, xfull, mod, inp):
    m = mod[2 * layer]
    vec = np.concatenate([fm(m[:2048]), fm(m[2048:4096]), fm(inp["norm_g"][layer, 0])], axis=1)
    if layer == 0:
        w = inp["e_w_in"][0]
        glu_a = w[:, 4112:5136].reshape(2048, 8, 128); glu_b = w[:, 5136:6160].reshape(2048, 8, 128)
        glu = np.stack([glu_a, glu_b], axis=2).reshape(2048, 2048)
        win = np.concatenate([w[:, 0:3072], glu, w[:, 3072:4096], w[:, 4096:4112]], axis=1)
        convw = fmk(inp["e_conv_qkv"][0]); convb = np.zeros((128, 24), np.float32)
        extra = {"dw": fmk(inp["e_conf_dw"][0]),
                 "dvec": np.concatenate([fm(inp["e_conf_dw_b"][0]), fm(inp["e_conf_ln_g"][0]), fm(inp["e_conf_ln_b"][0])], axis=1)}
    else:
        w = inp["o_w_in"][0]
        win = np.concatenate([w[:, 4096:10240], w[:, 0:4096], w[:, 10240:10304]], axis=1)
        convw = fmk(inp["o_conv_w"][0]); convb = fm(inp["o_conv_b"][0])
        extra = {}
    win = np.ascontiguousarray(win)
    xT = np.ascontiguousarray(xfull.T)
    maps = []
    for i in range(8):
        xs = np.zeros((2048, 1056), np.float32)
        lo = i * 1024 - 32
        if i == 0:
            xs[:, 32:] = xT[:, 0:1024]
        else:
            xs[:] = xT[:, lo:lo + 1056]
        hmask = np.full((128, 1), 0.0 if i == 0 else 1.0, np.float32)
        maps.append({"xT": xs, "vec": vec, "win": win, "convw": convw, "convb": convb, "hmask": hmask, **extra})
    return maps

def cmat():
    j = np.arange(128)[:, None]; i = np.arange(128)[None, :]
    same = (j // 64) == (i // 64)
    tri = ((j <= i) & same).astype(np.float32)
    blk = same.astype(np.float32)
    sel0 = np.broadcast_to(j < 64, (128, 128)).astype(np.float32)
    sel1 = np.broadcast_to(j >= 64, (128, 128)).astype(np.float32)
    return np.ascontiguousarray(np.concatenate([tri, blk, sel0, sel1], axis=1))

def ssd_inputs(pout, inp):
    maps = []
    cm = cmat()
    for g in range(8):
        xT = pout[g * 512:(g + 1) * 512]; zT = pout[6144 + g * 512:6144 + (g + 1) * 512]
        BT = np.ascontiguousarray(pout[4096 + g * 128:4096 + (g + 1) * 128]); CT = np.ascontiguousarray(pout[5120 + g * 128:5120 + (g + 1) * 128])
        dtr = pout[10240 + g * 8:10240 + (g + 1) * 8]
        dtr = np.ascontiguousarray(dtr.reshape(8, 64, 128).transpose(2, 1, 0).reshape(128, 512))
        hs = slice(g * 8, (g + 1) * 8)
        rv = np.concatenate([inp["o_dt_bias"][0][hs], inp["o_a_log"][0][hs], inp["o_d_skip"][0][hs]])[None, :]
        rv = np.ascontiguousarray(np.tile(rv, (128, 1)).astype(np.float32))
        maps.append({"x_tok": np.ascontiguousarray(xT.T), "z_tok": np.ascontiguousarray(zT.T), "BT": BT, "CT": CT,
                     "B_tok": np.ascontiguousarray(BT.T), "dtr": dtr, "rowvec": rv, "cmat": cm})
    return maps

def cmat_gdn():
    c = cmat()
    j = np.arange(128)[:, None]; i = np.arange(128)[None, :]
    masks = ((j > i) & ((j // 64) == (i // 64))).astype(np.float32)
    return np.ascontiguousarray(np.concatenate([c, np.eye(128, dtype=np.float32), masks], axis=1))

def gdn_inputs(pout, inp):
    cm = cmat_gdn()
    gn = np.ascontiguousarray(np.tile(inp["e_head_norm_g"][0][None, :], (128, 1)).astype(np.float32))
    maps = []
    for hd in range(8):
        r = slice(hd * 128, (hd + 1) * 128)
        qT = np.ascontiguousarray(pout[0:1024][r]); kT = np.ascontiguousarray(pout[1024:2048][r])
        vT = pout[2048:3072][r]; zT = pout[4096:5120][r]
        bl = np.ascontiguousarray(pout[5120 + hd].reshape(64, 128).T); al = np.ascontiguousarray(pout[5128 + hd].reshape(64, 128).T)
        rv = np.tile(np.array([[inp["e_a_log"][0][hd], inp["e_dt_bias"][0][hd]]], np.float32), (128, 1))
        maps.append({"qT": qT, "kT": kT, "k_tok": np.ascontiguousarray(kT.T), "v_tok": np.ascontiguousarray(vT.T),
                     "z_tok": np.ascontiguousarray(zT.T), "bl": bl, "al": al, "rowvec": np.ascontiguousarray(rv),
                     "gn": gn, "cmat": cm})
    return maps


def build_mod():
    nc = bass.Bass("TRN2", target_bir_lowering=False)
    P = Prog(nc)
    NCOL = 768
    c_d = P.dram("cT", [128, 16], F32, "ExternalInput")
    w_d = P.dram("wmod", [4, 2048, NCOL], F32, "ExternalInput")
    b_d = P.dram("bmod", [4, NCOL], F32, "ExternalInput")
    o_d = P.dram("mod", [4, NCOL], F32, "ExternalOutput")
    cs = P.sbuf([128, 16], F32, "cs")
    sc = P.sbuf([128, 16], F32, "sc")
    wb = [P.sbuf([128, 16, NCOL], F32, f"w{i}") for i in range(2)]
    bs = P.sbuf([1, 4 * NCOL], F32, "bs")
    os_ = P.sbuf([1, 4 * NCOL], F32, "os")
    bank = [P.psum([128, 512], F32, f"bank{i}") for i in range(2)]
    P.dma("sp", cs[:], c_d[:], writes=[cs], sb=cs)
    P.dma("sp", bs[:], b_d.t.rearrange("(o l) n -> o (l n)", o=1), writes=[bs], sb=bs)
    P.op("act", lambda e: e.activation(sc[:], cs[:], AF.Silu), reads=[cs], writes=[sc])
    k = 0
    for l in range(4):
        w = wb[l % 2]
        P.dma("sp" if l % 2 else "act", w[:], w_d[l].rearrange("(j p) n -> p j n", p=128), writes=[w], sb=w)
        for nh in range(2):
            bk = bank[k % 2]
            k += 1
            for j in range(16):
                P.op("pe", lambda e: e.matmul(bk[0:1, 0:384], sc[:, j:j + 1], w[:, j, nh * 384:(nh + 1) * 384],
                                              start=(j == 0), stop=(j == 15)),
                     reads=[sc, w], writes=[bk], defer=(j != 15))
            o = l * NCOL + nh * 384
            P.op("dve", lambda e: e.tensor_tensor(os_[0:1, o:o + 384], bk[0:1, 0:384], bs[0:1, o:o + 384], ALU.add),
                 reads=[bk, bs], writes=[os_])
    P.dma("sp", o_d.t.rearrange("(o l) n -> o (l n)", o=1), os_[:], reads=[os_], sb=os_)
    P.wait_all("sp", [os_])
    P.close()
    return nc


def build_pre(layer):
    NTK, HALO, NT = 1056, 32, 1024
    pieces = [(0, 32), (32, 544), (544, 1056)]
    if layer == 0:
        plan = [("conv", i) for i in range(24)] + [("glu", i) for i in range(16)] + \
               [("pass", i) for i in range(8)] + [("passp", 0)]
        NW, NOUT, NCONV, PW = 6160, 5136, 24, 16
        out_row = {"conv": 0, "glu": 3072, "pass": 4096, "passp": 5120}
    else:
        plan = [("conv", i) for i in range(48)] + [("pass", i) for i in range(32)] + [("passp", 0)]
        NW, NOUT, NCONV, PW = 10304, 10304, 48, 64
        out_row = {"conv": 0, "pass": 6144, "passp": 10240}
    nc = bass.Bass("TRN2", target_bir_lowering=False)
    P = Prog(nc)
    xT_d = P.dram("xT", [2048, NTK], F32, "ExternalInput")
    vec_d = P.dram("vec", [128, 48], F32, "ExternalInput")
    w_d = P.dram("win", [2048, NW], F32, "ExternalInput")
    cw_d = P.dram("convw", [128, 4 * NCONV], F32, "ExternalInput")
    cb_d = P.dram("convb", [128, NCONV], F32, "ExternalInput")
    hm_d = P.dram("hmask", [128, 1], F32, "ExternalInput")
    if layer == 0:
        dw_d = P.dram("dw", [128, 31 * 8], F32, "ExternalInput")
        dv_d = P.dram("dvec", [128, 24], F32, "ExternalInput")
    out_d = P.dram("pout", [NOUT, NT], F32, "ExternalOutput")

    big = [P.sbuf([128, NTK], F32, f"big{i}") for i in range(16)]
    hT = [P.sbuf([128, NTK], BF16, f"h{j}") for j in range(16)]
    wsl = [P.sbuf([128, 16, 512], BF16, f"wsl{i}") for i in range(3)]
    rstd = P.sbuf([128, NTK], F32, "rstd")
    sqb = [P.sbuf([128, 512], F32, f"sq{i}") for i in range(2)]
    tsm = [P.sbuf([128, 512], F32, f"tsm{i}") for i in range(2)]
    ones = P.sbuf([128, 128], F32, "ones")
    vec = P.sbuf([128, 48], F32, "vec")
    gs = P.sbuf([128, 16], F32, "gs")
    cw = P.sbuf([128, 4 * NCONV], F32, "cw")
    cb = P.sbuf([128, NCONV], F32, "cb")
    hm = P.sbuf([128, 1], F32, "hm")
    bank = [P.psum([128, 512], F32, f"bank{i}") for i in range(8)]
    if layer == 0:
        dw = P.sbuf([128, 31 * 8], F32, "dw")
        dv = P.sbuf([128, 24], F32, "dv")
        P.dma("sp", dw[:], dw_d[:], writes=[dw], sb=dw)
        P.dma("sp", dv[:], dv_d[:], writes=[dv], sb=dv)
    P.dma("sp", vec[:], vec_d[:], writes=[vec], sb=vec)
    P.dma("sp", cw[:], cw_d[:], writes=[cw], sb=cw)
    P.dma("sp", cb[:], cb_d[:], writes=[cb], sb=cb)
    P.dma("sp", hm[:], hm_d[:], writes=[hm], sb=hm)
    P.op("dve", lambda e: e.memset(ones[:], 1.0), writes=[ones])
    for j in range(16):
        P.dma("act" if j % 2 else "sp", big[j][:], xT_d[j * 128:(j + 1) * 128, :], writes=[big[j]], sb=big[j])

    P.op("dve", lambda e: e.tensor_scalar(gs[:], vec[:, 16:32], 1.0, None, ALU.add), reads=[vec], writes=[gs])
    P.op("dve", lambda e: e.tensor_tensor(gs[:], gs[:], vec[:, 32:48], ALU.mult), reads=[gs, vec], writes=[gs])
    for (lo, hi) in pieces:
        n = hi - lo
        for j in range(16):
            q = sqb[j % 2]
            P.op("act", lambda e: e.activation(q[:, 0:n], big[j][:, lo:hi], AF.Square), reads=[big[j]], writes=[q])
            P.op("pe", lambda e: e.matmul(bank[6][:, 0:n], ones[:], q[:, 0:n], start=(j == 0), stop=(j == 15)),
                 reads=[ones, q], writes=[bank[6]])
        t = tsm[0]
        P.op("act", lambda e: e.activation(t[:, 0:n], bank[6][:, 0:n], AF.Sqrt, bias=EPS, scale=1.0 / 2048),
             reads=[bank[6]], writes=[t])
        P.op("dve", lambda e: e.reciprocal(rstd[:, lo:hi], t[:, 0:n]), reads=[t], writes=[rstd])
    for j in range(16):
        P.op("dve", lambda e: e.tensor_tensor(big[j][:], big[j][:], rstd[:], ALU.mult),
             reads=[big[j], rstd], writes=[big[j]])
        P.op("dve", lambda e: e.tensor_scalar(hT[j][:], big[j][:], gs[:, j:j + 1], vec[:, j:j + 1], ALU.mult, ALU.add),
             reads=[big[j], gs, vec], writes=[hT[j]])

    rot = {"n": 0}
    if layer == 0:
        cv = big[0:8]
        pa = big[8]
        pool = big[9:16]
    else:
        pool = big

    def nextbuf():
        b = pool[rot["n"] % len(pool)]
        rot["n"] += 1
        return b

    nb = {"n": 0}
    wv = None
    for ci, (kind, idx) in enumerate(plan):
        blk = ci // 4
        if ci % 4 == 0:
            width = min(512, NW - blk * 512)
            ws = wsl[blk % 3]
            P.dma("pool", ws[:, :, 0:width], w_d[:, blk * 512:blk * 512 + width].rearrange("(j p) n -> p j n", p=128),
                  writes=[ws], sb=ws)
        off = (ci % 4) * 128
        wc = PW if kind == "passp" else 128
        halo = kind in ("conv", "glu")
        pr = pa if (kind == "glu" and idx % 2 == 0) else nextbuf()
        for pi, (lo, hi) in enumerate(pieces):
            if pi == 0 and not halo:
                continue
            n = hi - lo
            bk = bank[nb["n"] % 4]
            nb["n"] += 1
            for j in range(16):
                P.op("pe", lambda e: e.matmul(bk[0:wc, 0:n], ws[:, j, off:off + wc], hT[j][:, lo:hi],
                                              start=(j == 0), stop=(j == 15)),
                     reads=[ws, hT[j]], writes=[bk], defer=(j != 15))
            if pi == 0:
                P.op("act", lambda e: e.activation(pr[0:wc, lo:hi], bk[0:wc, 0:n], AF.Copy, scale=hm[0:wc, 0:1]),
                     reads=[bk, hm], writes=[pr])
            else:
                P.op("act", lambda e: e.activation(pr[0:wc, lo:hi], bk[0:wc, 0:n], AF.Copy), reads=[bk], writes=[pr])
        if kind == "conv":
            acc = nextbuf()
            P.op("dve", lambda e: e.tensor_scalar(acc[:, 0:NT], pr[:, 29:29 + NT], cw[:, idx:idx + 1], None, ALU.mult),
                 reads=[pr, cw], writes=[acc])
            for k in range(1, 4):
                P.op("dve", lambda e: e.scalar_tensor_tensor(acc[:, 0:NT], pr[:, 29 + k:29 + k + NT],
                                                             cw[:, k * NCONV + idx:k * NCONV + idx + 1], acc[:, 0:NT],
                                                             ALU.mult, ALU.add),
                     reads=[pr, cw, acc], writes=[acc])
            P.op("act", lambda e: e.activation(acc[:, 0:NT], acc[:, 0:NT], AF.Silu, bias=cb[:, idx:idx + 1], scale=1.0),
                 reads=[acc, cb], writes=[acc])
            if layer == 0 and idx < 16:
                qs = 128.0 ** -0.5 if idx < 8 else 1.0
                for hh in range(2):
                    sl = slice(hh * 512, (hh + 1) * 512)
                    q = sqb[hh]
                    P.op("act", lambda e: e.activation(q[:], acc[:, sl], AF.Square), reads=[acc], writes=[q])
                    P.op("pe", lambda e: e.matmul(bank[6][:], ones[:], q[:], start=True, stop=True),
                         reads=[ones, q], writes=[bank[6]])
                    t = tsm[hh]
                    P.op("act", lambda e: e.activation(t[:], bank[6][:], AF.Sqrt, bias=EPS, scale=1.0),
                         reads=[bank[6]], writes=[t])
                    P.op("dve", lambda e: e.reciprocal(t[:], t[:]), reads=[t], writes=[t])
                    P.op("dve", lambda e: e.scalar_tensor_tensor(acc[:, sl], acc[:, sl], qs, t[:], ALU.mult, ALU.mult),
                         reads=[acc, t], writes=[acc])
            r0 = out_row["conv"] + idx * 128
            P.dma("sp" if ci % 2 else "act", out_d[r0:r0 + 128, :], acc[:, 0:NT], reads=[acc], sb=acc)
        elif kind == "glu":
            if idx % 2 == 0:
                continue
            c = idx // 2
            P.op("act", lambda e: e.activation(pr[:], pr[:], AF.Sigmoid), reads=[pr], writes=[pr])
            P.op("dve", lambda e: e.tensor_tensor(pr[:], pr[:], pa[:], ALU.mult), reads=[pr, pa], writes=[pr])
            acc = cv[c]
            P.op("dve", lambda e: e.tensor_scalar(acc[:, 0:NT], pr[:, 2:2 + NT], dw[:, c:c + 1], None, ALU.mult),
                 reads=[pr, dw], writes=[acc])
            for k in range(1, 31):
                P.op("dve", lambda e: e.scalar_tensor_tensor(acc[:, 0:NT], pr[:, 2 + k:2 + k + NT],
                                                             dw[:, k * 8 + c:k * 8 + c + 1], acc[:, 0:NT],
                                                             ALU.mult, ALU.add),
                     reads=[pr, dw, acc], writes=[acc])
            P.op("dve", lambda e: e.tensor_scalar(acc[:, 0:NT], acc[:, 0:NT], dv[:, c:c + 1], None, ALU.add),
                 reads=[acc, dv], writes=[acc])
            if c == 7:
                for hh in range(2):
                    sl = slice(hh * 512, (hh + 1) * 512)
                    for c2 in range(8):
                        P.op("pe", lambda e: e.matmul(bank[6][:], ones[:], cv[c2][:, sl], start=(c2 == 0), stop=(c2 == 7)),
                             reads=[ones, cv[c2]], writes=[bank[6]], defer=(c2 != 7))
                    mb = tsm[0]
                    P.op("act", lambda e: e.activation(mb[:], bank[6][:], AF.Copy, scale=1.0 / 1024),
                         reads=[bank[6]], writes=[mb])
                    for c2 in range(8):
                        P.op("dve", lambda e: e.tensor_tensor(cv[c2][:, sl], cv[c2][:, sl], mb[:], ALU.subtract),
                             reads=[cv[c2], mb], writes=[cv[c2]])
                        q = sqb[c2 % 2]
                        P.op("act", lambda e: e.activation(q[:], cv[c2][:, sl], AF.Square), reads=[cv[c2]], writes=[q])
                        P.op("pe", lambda e: e.matmul(bank[7][:], ones[:], q[:], start=(c2 == 0), stop=(c2 == 7)),
                             reads=[ones, q], writes=[bank[7]])
                    t = tsm[1]
                    P.op("act", lambda e: e.activation(t[:], bank[7][:], AF.Sqrt, bias=EPS, scale=1.0 / 1024),
                         reads=[bank[7]], writes=[t])
                    P.op("dve", lambda e: e.reciprocal(t[:], t[:]), reads=[t], writes=[t])
                    for c2 in range(8):
                        P.op("dve", lambda e: e.tensor_tensor(cv[c2][:, sl], cv[c2][:, sl], t[:], ALU.mult),
                             reads=[cv[c2], t], writes=[cv[c2]])
                        P.op("act", lambda e: e.activation(cv[c2][:, sl], cv[c2][:, sl], AF.Silu,
                                                           bias=dv[:, 16 + c2:17 + c2], scale=dv[:, 8 + c2:9 + c2]),
                             reads=[cv[c2], dv], writes=[cv[c2]])
                for c2 in range(8):
                    r0 = out_row["glu"] + c2 * 128
                    P.dma("sp" if c2 % 2 else "act", out_d[r0:r0 + 128, :], cv[c2][:, 0:NT], reads=[cv[c2]], sb=cv[c2])
        else:
            r0 = out_row[kind] + idx * 128
            P.dma("sp" if ci % 2 else "act", out_d[r0:r0 + wc, :], pr[0:wc, HALO:NTK], reads=[pr], sb=pr)
    allb = big
    P.wait_all("sp", allb)
    P.wait_all("act", allb)
    P.close()
    return nc


def build_gdn():
    T, NTILE, GT = 8192, 64, 4
    nc = bass.Bass("TRN2", target_bir_lowering=False)
    P = Prog(nc)
    qT_d = P.dram("qT", [128, T], F32, "ExternalInput")
    kT_d = P.dram("kT", [128, T], F32, "ExternalInput")
    kk_d = P.dram("k_tok", [T, 128], F32, "ExternalInput")
    vk_d = P.dram("v_tok", [T, 128], F32, "ExternalInput")
    zk_d = P.dram("z_tok", [T, 128], F32, "ExternalInput")
    bl_d = P.dram("bl", [128, 64], F32, "ExternalInput")
    al_d = P.dram("al", [128, 64], F32, "ExternalInput")
    rv_d = P.dram("rowvec", [128, 2], F32, "ExternalInput")
    gn_d = P.dram("gn", [128, 128], F32, "ExternalInput")
    cm_d = P.dram("cmat", [128, 768], F32, "ExternalInput")
    out_d = P.dram("o_tok", [T, 128], F32, "ExternalOutput")

    cm = P.sbuf([128, 768], F32, "cm")
    TRI, BLK, SEL0, SEL1, IDENT, MASKS = (cm[:, i * 128:(i + 1) * 128] for i in range(6))
    rv = P.sbuf([128, 2], F32, "rv")
    gn = P.sbuf([128, 128], F32, "gn")
    names = ("bl", "al", "beta", "xb", "ax", "ex", "ln", "sp", "g", "gcs", "gtot", "eg", "kdsc", "beg",
             "dec0", "dec1", "aneg")
    S = {k: P.sbuf([128, 64], F32, "s_" + k) for k in names}
    St = [P.sbuf([128, 128], F32, f"St{i}") for i in range(2)]
    qTg = [P.sbuf([128, 512], F32, f"qTg{i}") for i in range(2)]
    kTg = [P.sbuf([128, 512], F32, f"kTg{i}") for i in range(2)]
    kkg = [P.sbuf([128, GT, 128], F32, f"kkg{i}") for i in range(2)]
    vkg = [P.sbuf([128, GT, 128], F32, f"vkg{i}") for i in range(2)]
    zkg = [P.sbuf([128, GT, 128], F32, f"zkg{i}") for i in range(2)]
    og = [P.sbuf([128, GT, 128], F32, f"og{i}") for i in range(2)]
    W = {}
    for gi in range(GT):
        for k in ("Y", "YT", "decn", "dect", "erb", "t", "N", "M", "Na", "Nb", "Ma", "Mb", "QKm", "qeg", "kd",
                  "kcdT", "u", "o", "sz", "rbs"):
            W[k, gi] = P.sbuf([128, 128], F32, f"w_{k}{gi}")
        for k in ("R0", "R1"):
            W[k, gi] = P.sbuf([128, 256], F32, f"w_{k}{gi}")
        for k in ("ss", "rs"):
            W[k, gi] = P.sbuf([128, 1], F32, f"w_{k}{gi}")
    bkA = [P.psum([128, 512], F32, f"bkA{i}") for i in range(GT)]
    bkB = [P.psum([128, 512], F32, f"bkB{i}") for i in range(GT)]
    def vw(b, lo, n):
        return View(b, b[:, lo:lo + n])
    A_q = [[vw(bkA[gi], q * 128, 128) for q in range(4)] for gi in range(GT)]
    A_h = [[vw(bkA[gi], h * 256, 256) for h in range(2)] for gi in range(GT)]
    B_q = [[vw(bkB[gi], q * 128, 128) for q in range(4)] for gi in range(GT)]
    pq = [B_q[0][0]]

    P.dma("sp", cm[:], cm_d[:], writes=[cm], sb=cm)
    P.dma("sp", rv[:], rv_d[:], writes=[rv], sb=rv)
    P.dma("sp", gn[:], gn_d[:], writes=[gn], sb=gn)
    P.dma("sp", S["bl"][:], bl_d[:], writes=[S["bl"]], sb=S["bl"])
    P.dma("sp", S["al"][:], al_d[:], writes=[S["al"]], sb=S["al"])

    D = lambda fn, r, w: P.op("dve", fn, reads=r, writes=w)
    A = lambda fn, r, w: P.op("act", fn, reads=r, writes=w)
    G = lambda fn, r, w: P.op("dve", fn, reads=r, writes=w)
    PE = lambda fn, r, w: P.op("pe", fn, reads=r, writes=w)

    A(lambda e: e.activation(S["beta"][:], S["bl"][:], AF.Sigmoid), [S["bl"]], [S["beta"]])
    D(lambda e: e.tensor_scalar(S["xb"][:], S["al"][:], rv[:, 1:2], None, ALU.add), [S["al"], rv], [S["xb"]])
    A(lambda e: e.activation(S["ax"][:], S["xb"][:], AF.Abs), [S["xb"]], [S["ax"]])
    A(lambda e: e.activation(S["ex"][:], S["ax"][:], AF.Exp, scale=-1.0), [S["ax"]], [S["ex"]])
    A(lambda e: e.activation(S["ln"][:], S["ex"][:], AF.Ln, bias=1.0, scale=1.0), [S["ex"]], [S["ln"]])
    D(lambda e: e.scalar_tensor_tensor(S["sp"][:], S["xb"][:], 0.0, S["ln"][:], ALU.max, ALU.add),
      [S["xb"], S["ln"]], [S["sp"]])
    A(lambda e: e.activation(S["aneg"][:, 0:1], rv[:, 0:1], AF.Exp), [rv], [S["aneg"]])
    D(lambda e: e.tensor_scalar(S["g"][:], S["sp"][:], S["aneg"][:, 0:1], -1.0, ALU.mult, ALU.mult),
      [S["sp"], S["aneg"]], [S["g"]])
    for (dst, mat, fn) in (("gcs", TRI, AF.Copy), ("gtot", BLK, AF.Copy), ("dec0", SEL0, AF.Exp), ("dec1", SEL1, AF.Exp)):
        PE(lambda e: e.matmul(pq[0][:, 0:64], mat, S["g"][:], start=True, stop=True), [cm, S["g"]], [pq[0]])
        A(lambda e: e.activation(S[dst][:], pq[0][:, 0:64], fn), [pq[0]], [S[dst]])
    A(lambda e: e.activation(S["eg"][:], S["gcs"][:], AF.Exp), [S["gcs"]], [S["eg"]])
    D(lambda e: e.tensor_tensor(S["kdsc"][:], S["gtot"][:], S["gcs"][:], ALU.subtract), [S["gtot"], S["gcs"]], [S["kdsc"]])
    A(lambda e: e.activation(S["kdsc"][:], S["kdsc"][:], AF.Exp), [S["kdsc"]], [S["kdsc"]])
    D(lambda e: e.tensor_tensor(S["beg"][:], S["beta"][:], S["eg"][:], ALU.mult), [S["beta"], S["eg"]], [S["beg"]])
    D(lambda e: e.memset(St[0][:], 0.0), [], [St[0]])

    def load_group(g):
        i = g % 2
        cs = slice(g * 512, (g + 1) * 512)
        P.dma("sp", qTg[i][:], qT_d[:, cs], writes=[qTg[i]], sb=qTg[i])
        P.dma("act", kTg[i][:], kT_d[:, cs], writes=[kTg[i]], sb=kTg[i])
        P.dma("sp", kkg[i][:], kk_d[cs, :].rearrange("(n p) d -> p n d", p=128), writes=[kkg[i]], sb=kkg[i])
        P.dma("act", vkg[i][:], vk_d[cs, :].rearrange("(n p) d -> p n d", p=128), writes=[vkg[i]], sb=vkg[i])
        P.dma("sp", zkg[i][:], zk_d[cs, :].rearrange("(n p) d -> p n d", p=128), writes=[zkg[i]], sb=zkg[i])

    NG = NTILE // GT
    load_group(0)
    cur = 0
    for g in range(NG):
        if g + 1 < NG:
            load_group(g + 1)
        i = g % 2
        tiles = range(GT)
        col = lambda gi: slice(g * GT + gi, g * GT + gi + 1)
        kT = lambda gi: kTg[i][:, gi * 128:(gi + 1) * 128]
        qT = lambda gi: qTg[i][:, gi * 128:(gi + 1) * 128]
        qa = lambda gi: A_q[gi][0]
        qb = lambda gi: A_q[gi][1]
        qc = lambda gi: A_q[gi][2]
        qd = lambda gi: B_q[gi][3]
        for gi in tiles:
            PE(lambda e: e.matmul(qa(gi)[:], S["g"][:, col(gi)].broadcast_to([128, 128]), TRI, start=True, stop=True),
               [S["g"], cm], [qa(gi)])
            PE(lambda e: e.matmul(qb(gi)[:], kT(gi), kT(gi), start=True, stop=True), [kTg[i]], [qb(gi)])
            PE(lambda e: e.matmul(qc(gi)[:], kT(gi), qT(gi), start=True, stop=True), [kTg[i], qTg[i]], [qc(gi)])
        for gi in tiles:
            w = lambda k: W[k, gi]
            gc_ = S["gcs"][:, col(gi)]
            D(lambda e: e.tensor_copy(w("rbs")[:], qa(gi)[:]), [qa(gi)], [w("rbs")])
            D(lambda e: e.tensor_scalar(w("Y")[:], w("rbs")[:], gc_, 0.0, ALU.subtract, ALU.max), [w("rbs"), S["gcs"]], [w("Y")])
            D(lambda e: e.tensor_scalar(w("YT")[:], w("rbs")[:], gc_, 0.0, ALU.subtract, ALU.min), [w("rbs"), S["gcs"]], [w("YT")])
        for gi in tiles:
            w = lambda k: W[k, gi]
            A(lambda e: e.activation(w("decn")[:], w("Y")[:], AF.Exp, scale=-1.0), [w("Y")], [w("decn")])
            A(lambda e: e.activation(w("dect")[:], w("YT")[:], AF.Exp), [w("YT")], [w("dect")])
            A(lambda e: e.activation(w("erb")[:], w("rbs")[:], AF.Exp), [w("rbs")], [w("erb")])
        for gi in tiles:
            w = lambda k: W[k, gi]
            G(lambda e: e.tensor_scalar(w("R0")[:, 0:128], vkg[i][:, gi, :], S["beta"][:, col(gi)], None, ALU.mult),
              [vkg[i], S["beta"]], [w("R0")])
            G(lambda e: e.tensor_scalar(w("R0")[:, 128:256], kkg[i][:, gi, :], S["beg"][:, col(gi)], None, ALU.mult),
              [kkg[i], S["beg"]], [w("R0")])
        for gi in tiles:
            w = lambda k: W[k, gi]
            D(lambda e: e.tensor_tensor(w("t")[:], qb(gi)[:], w("decn")[:], ALU.mult), [qb(gi), w("decn")], [w("t")])
            D(lambda e: e.scalar_tensor_tensor(w("N")[:], w("t")[:], S["beta"][:, col(gi)], MASKS, ALU.mult, ALU.mult),
              [w("t"), S["beta"], cm], [w("N")])
        for gi in tiles:
            PE(lambda e: e.matmul(qd(gi)[:], W["N", gi][:], IDENT, start=True, stop=True), [W["N", gi], cm], [qd(gi)])
        for gi in tiles:
            w = lambda k: W[k, gi]
            G(lambda e: e.tensor_tensor(w("dect")[:], w("dect")[:], TRI, ALU.mult), [w("dect"), cm], [w("dect")])
            D(lambda e: e.tensor_tensor(w("QKm")[:], qc(gi)[:], w("dect")[:], ALU.mult), [qc(gi), w("dect")], [w("QKm")])
            G(lambda e: e.tensor_tensor(w("qeg")[:], qT(gi), w("erb")[:], ALU.mult), [qTg[i], w("erb")], [w("qeg")])
            G(lambda e: e.tensor_scalar(w("kd")[:], kkg[i][:, gi, :], S["kdsc"][:, col(gi)], None, ALU.mult),
              [kkg[i], S["kdsc"]], [w("kd")])
        for gi in tiles:
            A(lambda e: e.activation(W["M", gi][:], qd(gi)[:], AF.Copy), [qd(gi)], [W["M", gi]])
        Mc = {gi: W["M", gi] for gi in tiles}
        Nc = {gi: W["N", gi] for gi in tiles}
        Rc = {gi: W["R0", gi] for gi in tiles}
        for lvl in range(6):
            sign = ALU.subtract if lvl == 0 else ALU.add
            apb = (lambda gi: A_h[gi][0]) if lvl % 2 == 0 else (lambda gi: A_h[gi][1])
            pm = (lambda gi: B_q[gi][0]) if lvl % 2 == 0 else (lambda gi: B_q[gi][2])
            pn = (lambda gi: B_q[gi][1]) if lvl % 2 == 0 else (lambda gi: B_q[gi][3])
            for gi in tiles:
                PE(lambda e: e.matmul(apb(gi)[:], Mc[gi][:], Rc[gi][:], start=True, stop=True), [Mc[gi], Rc[gi]], [apb(gi)])
                if lvl < 5:
                    PE(lambda e: e.matmul(pm(gi)[:], Nc[gi][:], Mc[gi][:], start=True, stop=True), [Nc[gi], Mc[gi]], [pm(gi)])
                if lvl < 4:
                    PE(lambda e: e.matmul(pn(gi)[:], Mc[gi][:], Nc[gi][:], start=True, stop=True), [Nc[gi], Mc[gi]], [pn(gi)])
            for gi in tiles:
                Rn = W["R1", gi] if Rc[gi] is W["R0", gi] else W["R0", gi]
                D(lambda e: e.tensor_tensor(Rn[:], Rc[gi][:], apb(gi)[:], sign), [Rc[gi], apb(gi)], [Rn])
                Rc[gi] = Rn
                if lvl < 5:
                    Mn = W["Ma", gi] if lvl % 2 == 0 else W["Mb", gi]
                    A(lambda e: e.activation(Mn[:], pm(gi)[:], AF.Copy), [pm(gi)], [Mn])
                if lvl < 4:
                    Nn = W["Na", gi] if lvl % 2 == 0 else W["Nb", gi]
                    A(lambda e: e.activation(Nn[:], pn(gi)[:], AF.Copy), [pn(gi)], [Nn])
                if lvl < 5:
                    Mc[gi] = Mn
                if lvl < 4:
                    Nc[gi] = Nn
        for gi in tiles:
            PE(lambda e: e.matmul(B_q[gi][0][:], Rc[gi][:, 128:256], IDENT, start=True, stop=True), [Rc[gi], cm], [B_q[gi][0]])
        for gi in tiles:
            A(lambda e: e.activation(W["kcdT", gi][:], B_q[gi][0][:], AF.Copy), [B_q[gi][0]], [W["kcdT", gi]])
        for gi in tiles:
            w = lambda k: W[k, gi]
            for c in range(2):
                lo, hi = c * 64, (c + 1) * 64
                s_old, s_new = St[cur], St[1 - cur]
                PE(lambda e: e.matmul(A_q[gi][0][:], w("kcdT")[:], s_old[:], start=True, stop=True), [w("kcdT"), s_old], [A_q[gi][0]])
                D(lambda e: e.tensor_tensor(w("u")[lo:hi, :], Rc[gi][lo:hi, 0:128], A_q[gi][0][lo:hi, :], ALU.subtract),
                  [Rc[gi], A_q[gi][0]], [w("u")])
                PE(lambda e: e.matmul(A_q[gi][1][:], w("kd")[lo:hi, :], w("u")[lo:hi, :], start=True, stop=True),
                   [w("kd"), w("u")], [A_q[gi][1]])
                P.op("pe", lambda e: e.matmul(B_q[gi][1][:], w("qeg")[:], s_old[:], start=True, stop=False),
                     reads=[w("qeg"), s_old], writes=[B_q[gi][1]], defer=True)
                PE(lambda e: e.matmul(B_q[gi][1][:], w("QKm")[lo:hi, :], w("u")[lo:hi, :], start=False, stop=True),
                   [w("QKm"), w("u")], [B_q[gi][1]])
                dec = S["dec0"] if c == 0 else S["dec1"]
                D(lambda e: e.scalar_tensor_tensor(s_new[:], s_old[:], dec[:, col(gi)], A_q[gi][1][:], ALU.mult, ALU.add),
                  [s_old, dec, A_q[gi][1]], [s_new])
                A(lambda e: e.activation(w("o")[lo:hi, :], B_q[gi][1][lo:hi, :], AF.Copy), [B_q[gi][1]], [w("o")])
                cur = 1 - cur
            A(lambda e: e.activation(w("sz")[:], w("o")[:], AF.Square, accum_out=w("ss")[:]), [w("o")], [w("sz"), w("ss")])
            A(lambda e: e.activation(w("rs")[:], w("ss")[:], AF.Sqrt, bias=EPS, scale=1.0 / 128), [w("ss")], [w("rs")])
            D(lambda e: e.reciprocal(w("rs")[:], w("rs")[:]), [w("rs")], [w("rs")])
            D(lambda e: e.scalar_tensor_tensor(w("o")[:], w("o")[:], w("rs")[:, 0:1], gn[:], ALU.mult, ALU.mult),
              [w("o"), w("rs"), gn], [w("o")])
            A(lambda e: e.activation(w("sz")[:], zkg[i][:, gi, :], AF.Silu), [zkg[i]], [w("sz")])
            D(lambda e: e.tensor_tensor(og[i][:, gi, :], w("o")[:], w("sz")[:], ALU.mult), [w("o"), w("sz")], [og[i]])
        P.dma("sp", out_d[g * 512:(g + 1) * 512, :].rearrange("(n p) d -> p n d", p=128), og[i][:],
              reads=[og[i]], sb=og[i])
    P.wait_all("sp", og)
    P.close()
    return nc


def build_ssd():
    T, NTILE, H, PD, NS = 8192, 64, 8, 64, 128
    nc = bass.Bass("TRN2", target_bir_lowering=False)
    P = Prog(nc)
    x_d = P.dram("x_tok", [T, 512], F32, "ExternalInput")
    z_d = P.dram("z_tok", [T, 512], F32, "ExternalInput")
    bt_d = P.dram("BT", [128, T], F32, "ExternalInput")
    ct_d = P.dram("CT", [128, T], F32, "ExternalInput")
    bk_d = P.dram("B_tok", [T, 128], F32, "ExternalInput")
    dtr_d = P.dram("dtr", [128, 512], F32, "ExternalInput")
    rv_d = P.dram("rowvec", [128, 24], F32, "ExternalInput")
    cm_d = P.dram("cmat", [128, 512], F32, "ExternalInput")
    out_d = P.dram("yz_tok", [T, 512], F32, "ExternalOutput")

    cm = P.sbuf([128, 512], F32, "cm")
    TRI, BLK, SEL0, SEL1 = (cm[:, i * 128:(i + 1) * 128] for i in range(4))
    rv = P.sbuf([128, 24], F32, "rv")
    dtr = P.sbuf([128, 512], F32, "dtr")
    names = ("xb", "ax", "ex", "ln", "dt", "da", "acs", "atot", "eacs", "dte", "dec0", "dec1", "aneg")
    S = {k: P.sbuf([128, 512], F32, "s_" + k) for k in names}
    ST = [P.sbuf([128, 512], F32, f"ST{i}") for i in range(2)]
    NB = 3
    xt = [P.sbuf([128, 512], F32, f"xt{i}") for i in range(NB)]
    zt = [P.sbuf([128, 512], F32, f"zt{i}") for i in range(NB)]
    btt = [P.sbuf([128, 128], F32, f"bt{i}") for i in range(NB)]
    ctt = [P.sbuf([128, 128], F32, f"ct{i}") for i in range(NB)]
    bkt = [P.sbuf([128, 128], F32, f"bk{i}") for i in range(NB)]
    xdd = [P.sbuf([128, 512], F32, f"xdd{i}") for i in range(2)]
    xdt = [P.sbuf([128, 512], F32, f"xdt{i}") for i in range(2)]
    cbm = [P.sbuf([128, 128], F32, f"cbm{i}") for i in range(2)]
    xx = [P.sbuf([128, 128], F32, f"xx{i}") for i in range(8)]
    mt = [P.sbuf([128, 128], F32, f"mt{i}") for i in range(8)]
    yt = [P.sbuf([128, 512], F32, f"yt{i}") for i in range(2)]
    uo = [P.sbuf([128, 512], F32, f"uo{i}") for i in range(2)]
    bank = [P.psum([128, 512], F32, f"bank{i}") for i in range(8)]

    rbv = [View(bank[4 + h // 4], bank[4 + h // 4][:, (h % 4) * 128:(h % 4 + 1) * 128]) for h in range(8)]
    csbk = [bank[7], bank[1]]
    P.dma("sp", cm[:], cm_d[:], writes=[cm], sb=cm)
    P.dma("sp", rv[:], rv_d[:], writes=[rv], sb=rv)
    P.dma("sp", dtr[:], dtr_d[:], writes=[dtr], sb=dtr)

    def bc8(ap):
        return ap.unsqueeze(2).broadcast_to([ap.shape[0], 8, 64])

    def v3(ap):
        return ap.rearrange("p (h d) -> p h d", d=64)

    def rep(ap):
        return ap.unsqueeze(1).broadcast_to([128, 64, 8])

    def t3(ap):
        return ap.rearrange("p (n h) -> p n h", h=8)

    D = lambda fn, r, w: P.op("dve", fn, reads=r, writes=w)
    A = lambda fn, r, w: P.op("act", fn, reads=r, writes=w)
    D(lambda e: e.tensor_tensor(t3(S["xb"][:]), t3(dtr[:]), rep(rv[:, 0:8]), ALU.add), [dtr, rv], [S["xb"]])
    A(lambda e: e.activation(S["ax"][:], S["xb"][:], AF.Abs), [S["xb"]], [S["ax"]])
    A(lambda e: e.activation(S["ex"][:], S["ax"][:], AF.Exp, scale=-1.0), [S["ax"]], [S["ex"]])
    A(lambda e: e.activation(S["ln"][:], S["ex"][:], AF.Ln, bias=1.0, scale=1.0), [S["ex"]], [S["ln"]])
    D(lambda e: e.scalar_tensor_tensor(S["dt"][:], S["xb"][:], 0.0, S["ln"][:], ALU.max, ALU.add),
      [S["xb"], S["ln"]], [S["dt"]])
    A(lambda e: e.activation(S["aneg"][:, 0:8], rv[:, 8:16], AF.Exp), [rv], [S["aneg"]])
    D(lambda e: e.tensor_tensor(t3(S["da"][:]), t3(S["dt"][:]), rep(S["aneg"][:, 0:8]), ALU.mult),
      [S["dt"], S["aneg"]], [S["da"]])
    D(lambda e: e.tensor_scalar(S["da"][:], S["da"][:], -1.0, None, ALU.mult), [S["da"]], [S["da"]])
    for (dst, mat) in (("acs", TRI), ("atot", BLK), ("dec0", SEL0), ("dec1", SEL1)):
        P.op("pe", lambda e: e.matmul(bank[0][:], mat, S["da"][:], start=True, stop=True),
             reads=[cm, S["da"]], writes=[bank[0]])
        if dst in ("dec0", "dec1"):
            A(lambda e: e.activation(S[dst][:], bank[0][:], AF.Exp), [bank[0]], [S[dst]])
        else:
            A(lambda e: e.activation(S[dst][:], bank[0][:], AF.Copy), [bank[0]], [S[dst]])
    A(lambda e: e.activation(S["eacs"][:], S["acs"][:], AF.Exp), [S["acs"]], [S["eacs"]])
    D(lambda e: e.tensor_tensor(S["dte"][:], S["atot"][:], S["acs"][:], ALU.subtract), [S["atot"], S["acs"]], [S["dte"]])
    A(lambda e: e.activation(S["dte"][:], S["dte"][:], AF.Exp), [S["dte"]], [S["dte"]])
    D(lambda e: e.tensor_tensor(S["dte"][:], S["dte"][:], S["dt"][:], ALU.mult), [S["dte"], S["dt"]], [S["dte"]])
    D(lambda e: e.memset(ST[0][:], 0.0), [], [ST[0]])

    def load_tile(n):
        i = n % NB
        r = slice(n * 128, (n + 1) * 128)
        P.dma("sp", xt[i][:], x_d[r, :], writes=[xt[i]], sb=xt[i])
        P.dma("act", zt[i][:], z_d[r, :], writes=[zt[i]], sb=zt[i])
        P.dma("sp", btt[i][:], bt_d[:, r], writes=[btt[i]], sb=btt[i])
        P.dma("act", ctt[i][:], ct_d[:, r], writes=[ctt[i]], sb=ctt[i])
        P.dma("sp", bkt[i][:], bk_d[r, :], writes=[bkt[i]], sb=bkt[i])

    G = lambda fn, r, w: P.op("pool", fn, reads=r, writes=w)
    load_tile(0)
    load_tile(1)
    state = {"cur": 0}

    def front(n):
        i = n % NB
        X, BT, CT = xt[i], btt[i], ctt[i]
        sc = slice(n * 8, (n + 1) * 8)
        xd, xe, y = xdt[n % 2], xdd[n % 2], yt[n % 2]
        G(lambda e: e.tensor_tensor(v3(xd[:]), v3(X[:]), bc8(S["dt"][:, sc]), ALU.mult), [X, S["dt"]], [xd])
        G(lambda e: e.tensor_tensor(v3(xe[:]), v3(X[:]), bc8(S["dte"][:, sc]), ALU.mult), [X, S["dte"]], [xe])
        G(lambda e: e.tensor_tensor(v3(y[:]), v3(X[:]), bc8(rv[:, 16:24]), ALU.mult), [X, rv], [y])
        P.op("pe", lambda e: e.matmul(bank[0][:, 0:128], BT[:], CT[:], start=True, stop=True),
             reads=[BT, CT], writes=[bank[0]])
        cb = cbm[n % 2]
        D(lambda e: e.tensor_tensor(cb[:], bank[0][:, 0:128], TRI, ALU.mult), [bank[0], cm], [cb])
        for h in range(H):
            col = n * 8 + h
            rb = rbv[h]
            P.op("pe", lambda e: e.matmul(rb[:], S["da"][:, col:col + 1].broadcast_to([128, 128]), TRI,
                                          start=True, stop=True),
                 reads=[S["da"], cm], writes=[rb])

    def middle(n):
        i = n % NB
        BK = bkt[i]
        xd, xe, cb = xdt[n % 2], xdd[n % 2], cbm[n % 2]
        yb = bank[2 + n % 2]
        for c in range(2):
            lo, hi = c * 64, (c + 1) * 64
            P.op("pe", lambda e: e.matmul(csbk[c][:], BK[lo:hi, :], xe[lo:hi, :], start=True, stop=True),
                 reads=[BK, xe], writes=[csbk[c]])
        for h in range(H):
            col = n * 8 + h
            rb = rbv[h]
            x_ = xx[h]
            D(lambda e: e.tensor_scalar(x_[:], rb[:], S["acs"][:, col:col + 1], 0.0, ALU.subtract, ALU.min),
              [rb, S["acs"]], [x_])
            A(lambda e: e.activation(x_[:], x_[:], AF.Exp), [x_], [x_])
        for h in range(H):
            x_ = xx[h]
            m_ = mt[h]
            D(lambda e: e.tensor_tensor(m_[:], x_[:], cb[:], ALU.mult), [x_, cb], [m_])
        for h in range(H):
            m_ = mt[h]
            P.op("pe", lambda e: e.matmul(yb[:, h * 64:(h + 1) * 64], m_[:], xd[:, h * 64:(h + 1) * 64],
                                          start=True, stop=True),
                 reads=[m_, xd], writes=[yb])

    def scan(n):
        i = n % NB
        Z, CT = zt[i], ctt[i]
        sc = slice(n * 8, (n + 1) * 8)
        y = yt[n % 2]
        yb = bank[2 + n % 2]
        D(lambda e: e.tensor_tensor(y[:], y[:], yb[:], ALU.add), [y, yb], [y])
        for c in range(2):
            lo, hi = c * 64, (c + 1) * 64
            s_old, s_new = ST[state["cur"]], ST[1 - state["cur"]]
            yo = bank[6]
            P.op("pe", lambda e: e.matmul(yo[:], CT[:], s_old[:], start=True, stop=True),
                 reads=[CT, s_old], writes=[yo])
            csb = csbk[c]
            u = uo[c]
            dec = S["dec0"] if c == 0 else S["dec1"]
            D(lambda e: e.tensor_tensor(v3(s_new[:]), v3(s_old[:]), bc8(dec[:, sc]), ALU.mult), [s_old, dec], [s_new])
            D(lambda e: e.tensor_tensor(s_new[:], s_new[:], csb[:], ALU.add), [s_new, csb], [s_new])
            D(lambda e: e.tensor_tensor(v3(u[lo:hi, :]), v3(yo[lo:hi, :]), bc8(S["eacs"][lo:hi, sc]), ALU.mult),
              [yo, S["eacs"]], [u])
            D(lambda e: e.tensor_tensor(y[lo:hi, :], y[lo:hi, :], u[lo:hi, :], ALU.add), [y, u], [y])
            state["cur"] = 1 - state["cur"]
        A(lambda e: e.activation(Z[:], Z[:], AF.Silu), [Z], [Z])
        D(lambda e: e.tensor_tensor(y[:], y[:], Z[:], ALU.mult), [y, Z], [y])
        P.dma("sp", out_d[n * 128:(n + 1) * 128, :], y[:], reads=[y], sb=y)

    front(0)
    for n in range(NTILE):
        if n + 2 < NTILE:
            load_tile(n + 2)
        middle(n)
        if n + 1 < NTILE:
            front(n + 1)
        scan(n)
    P.wait_all("sp", yt)
    P.close()
    return nc


def build_tail(layer):
    rms = layer == 1
    final = layer == 1
    C = 4096 if layer == 1 else 2048
    CC = C // 128
    NT = 1024
    nc = bass.Bass("TRN2", target_bir_lowering=False)
    P = Prog(nc)
    xT_d = P.dram("xT", [2048, NT], F32, "ExternalInput")
    cat_d = P.dram("catT", [C, NT], F32, "ExternalInput")
    vec_d = P.dram("vec", [128, 6 * 16], F32, "ExternalInput")
    gc_d = P.dram("gcat", [128, CC], F32, "ExternalInput")
    wout_d = P.dram("wout", [C, 2048], F32, "ExternalInput")
    wr_d = P.dram("wr", [2048, 36], F32, "ExternalInput")
    br_d = P.dram("br", [128, 36], F32, "ExternalInput")
    w1_d = P.dram("w1", [32, 2048, 512], F32, "ExternalInput")
    w3_d = P.dram("w3", [32, 2048, 512], F32, "ExternalInput")
    w2_d = P.dram("w2", [32, 512, 2048], F32, "ExternalInput")
    id_d = P.dram("ident", [128, 128], F32, "ExternalInput")
    out_d = P.dram("outT", [2048, NT], F32, "ExternalOutput")

    xT = [[P.sbuf([128, 512], F32, f"x{j}_{h}") for h in range(2)] for j in range(16)]
    hT = [P.sbuf([128, 512], BF16, f"h{i}") for i in range(32)]
    wsl = [P.sbuf([128, 8192], BF16, f"wsl{i}") for i in range(4)]
    aT = [[P.sbuf([128, 512], BF16, f"a{j}_{h}") for h in range(2)] for j in range(4)]
    G = [P.sbuf([128, 1024], F32, f"G{i}") for i in range(2)]
    tmp1 = [P.sbuf([128, 512], F32, f"t1_{i}") for i in range(2)]
    tmp2 = [P.sbuf([128, 512], F32, f"t2_{i}") for i in range(2)]
    stg = [P.sbuf([128, 512], F32, f"stg{i}") for i in range(2)]
    sqb = [P.sbuf([128, 512], F32, f"sq{i}") for i in range(2)]
    rstd = [P.sbuf([128, 512], F32, f"rstd{i}") for i in range(2)]
    ones = P.sbuf([128, 128], F32, "ones")
    ident = P.sbuf([128, 128], F32, "ident")
    wr = P.sbuf([128, 16, 36], F32, "wr")
    br = P.sbuf([128, 36], F32, "br")
    vec = P.sbuf([128, 96], F32, "vec")
    gc = P.sbuf([128, CC], F32, "gc")
    gs2 = P.sbuf([128, 16], F32, "gs2")
    gd = [P.sbuf([128, 32], F32, f"gd{i}") for i in range(8)]
    sm = {k: P.sbuf([128, 36], F32, "sm_" + k) for k in
          ("lg", "ge", "ohg", "pen", "em", "oh1", "em2", "oh2")}
    sc = {k: P.sbuf([128, 1], F32, "sc_" + k) for k in
          ("gmax", "ngmax", "gsum", "grp", "m1", "m2", "d", "ed", "den", "p1", "p2", "p1g", "p2g")}
    bank = [P.psum([128, 512], F32, f"bank{i}") for i in range(8)]

    def V(i):
        return vec[:, i * 16:(i + 1) * 16]
    GATE_MIX, SHIFT2, SCALE2, GATE_FFN, NORMG2, FING = range(6)

    P.dma("sp", vec[:], vec_d[:], writes=[vec], sb=vec)
    P.dma("sp", gc[:], gc_d[:], writes=[gc], sb=gc)
    P.dma("sp", ident[:], id_d[:], writes=[ident], sb=ident)
    P.dma("sp", wr[:], wr_d.t.rearrange("(j p) n -> p j n", p=128), writes=[wr], sb=wr)
    P.dma("sp", br[:], br_d[:], writes=[br], sb=br)
    P.op("dve", lambda e: e.memset(ones[:], 1.0), writes=[ones])
    for j in range(16):
        for h in range(2):
            b = xT[j][h]
            P.dma("act" if (j + h) % 2 else "sp", b[:], xT_d[j * 128:(j + 1) * 128, h * 512:(h + 1) * 512],
                  writes=[b], sb=b)

    ring = {"n": 0}

    def wload(src_ap, kind):
        s = wsl[ring["n"] % 4]
        ring["n"] += 1
        if kind == "k16":
            dst = s[:].rearrange("p (j n) -> p j n", n=512)
            src = src_ap.rearrange("(j p) n -> p j n", p=128)
        else:
            dst = s[:].rearrange("p (j n) -> p j n", n=2048)
            src = src_ap.rearrange("(j p) n -> p j n", p=128)
        P.dma("pool", dst, src, writes=[s], sb=s)
        return s, dst

    def sum_sq_rstd(srcs, h, n_feat):
        n = len(srcs)
        for i, (sbuf_, ap) in enumerate(srcs):
            q = sqb[i % 2]
            P.op("act", lambda e: e.activation(q[:], ap, AF.Square), reads=[sbuf_], writes=[q])
            P.op("pe", lambda e: e.matmul(bank[6][:], ones[:], q[:], start=(i == 0), stop=(i == n - 1)),
                 reads=[ones, q], writes=[bank[6]])
        t = tmp1[0]
        P.op("act", lambda e: e.activation(t[:], bank[6][:], AF.Sqrt, bias=EPS, scale=1.0 / n_feat),
             reads=[bank[6]], writes=[t])
        P.op("dve", lambda e: e.reciprocal(rstd[h][:], t[:]), reads=[t], writes=[rstd[h]])

    nmm = 0
    for h in range(2):
        cbufs = []
        for cc in range(CC):
            s = stg[cc % 2]
            P.dma("sp" if cc % 2 else "act", s[:], cat_d[cc * 128:(cc + 1) * 128, h * 512:(h + 1) * 512],
                  writes=[s], sb=s)
            if rms:
                q = sqb[cc % 2]
                P.op("act", lambda e: e.activation(q[:], s[:], AF.Square), reads=[s], writes=[q])
                P.op("pe", lambda e: e.matmul(bank[6][:], ones[:], q[:], start=(cc == 0), stop=(cc == CC - 1)),
                     reads=[ones, q], writes=[bank[6]])
            dst = hT[cc] if CC == 32 else hT[h * 16 + cc]
            P.op("dve", lambda e: e.tensor_scalar(dst[:], s[:], gc[:, cc:cc + 1], None, ALU.mult),
                 reads=[s, gc], writes=[dst])
            cbufs.append(dst)
        if rms:
            t = tmp1[0]
            P.op("act", lambda e: e.activation(t[:], bank[6][:], AF.Sqrt, bias=EPS, scale=1.0 / C),
                 reads=[bank[6]], writes=[t])
            P.op("dve", lambda e: e.reciprocal(rstd[h][:], t[:]), reads=[t], writes=[rstd[h]])
        for cb in range(4):
            slots = []
            for rb in range(C // 2048):
                slots.append(wload(wout_d[rb * 2048:(rb + 1) * 2048, cb * 512:(cb + 1) * 512], "k16"))
            for fc in range(4):
                j = cb * 4 + fc
                bk = bank[nmm % 2]
                nmm += 1
                for cc in range(CC):
                    sb_, view = slots[cc // 16]
                    P.op("pe", lambda e: e.matmul(bk[:], view[:, cc % 16, fc * 128:(fc + 1) * 128], cbufs[cc][:],
                                                  start=(cc == 0), stop=(cc == CC - 1)),
                         reads=[sb_, cbufs[cc]], writes=[bk], defer=(cc != CC - 1))
                xb = xT[j][h]
                if rms:
                    t = tmp2[j % 2]
                    P.op("dve", lambda e: e.tensor_tensor(t[:], bk[:], rstd[h][:], ALU.mult),
                         reads=[bk, rstd[h]], writes=[t])
                    P.op("dve", lambda e: e.scalar_tensor_tensor(xb[:], t[:], V(GATE_MIX)[:, j:j + 1], xb[:],
                                                                 ALU.mult, ALU.add),
                         reads=[t, vec, xb], writes=[xb])
                else:
                    P.op("dve", lambda e: e.scalar_tensor_tensor(xb[:], bk[:], V(GATE_MIX)[:, j:j + 1], xb[:],
                                                                 ALU.mult, ALU.add),
                         reads=[bk, vec, xb], writes=[xb])

    P.op("dve", lambda e: e.tensor_scalar(gs2[:], V(SCALE2), 1.0, None, ALU.add), reads=[vec], writes=[gs2])
    P.op("dve", lambda e: e.tensor_tensor(gs2[:], gs2[:], V(NORMG2), ALU.mult), reads=[gs2, vec], writes=[gs2])
    for h in range(2):
        sum_sq_rstd([(xT[j][h], xT[j][h][:]) for j in range(16)], h, 2048)
        for j in range(16):
            hf = stg[j % 2]
            P.op("dve", lambda e: e.tensor_tensor(hf[:], xT[j][h][:], rstd[h][:], ALU.mult),
                 reads=[xT[j][h], rstd[h]], writes=[hf])
            P.op("dve", lambda e: e.tensor_scalar(hf[:], hf[:], gs2[:, j:j + 1], V(SHIFT2)[:, j:j + 1],
                                                  ALU.mult, ALU.add),
                 reads=[hf, gs2, vec], writes=[hf])
            hb = hT[j * 2 + h]
            P.op("act", lambda e: e.activation(hb[:], hf[:], AF.Copy), reads=[hf], writes=[hb])
            for tt in range(4):
                bk = bank[2 + tt]
                P.op("pe", lambda e: e.matmul(bk[:, 0:36], hf[:, tt * 128:(tt + 1) * 128], wr[:, j, :],
                                              start=(j == 0), stop=(j == 15)),
                     reads=[hf, wr], writes=[bk], defer=not (j == 15 or tt == 3))
        for tt in range(4):
            bk = bank[2 + tt]
            g = gd[h * 4 + tt]
            lg, ge, ohg, pen, em, oh1, em2, oh2 = (sm[k] for k in ("lg", "ge", "ohg", "pen", "em", "oh1", "em2", "oh2"))
            D = lambda fn, r, w: P.op("dve", fn, reads=r, writes=w)
            A = lambda fn, r, w: P.op("act", fn, reads=r, writes=w)
            D(lambda e: e.tensor_tensor(lg[:], bk[:, 0:36], br[:], ALU.add), [bk, br], [lg])
            D(lambda e: e.tensor_reduce(sc["gmax"][:], lg[:, 0:4], AX.X, ALU.max), [lg], [sc["gmax"]])
            D(lambda e: e.tensor_scalar(sc["ngmax"][:], sc["gmax"][:], -1.0, None, ALU.mult), [sc["gmax"]], [sc["ngmax"]])
            A(lambda e: e.activation(ge[:, 0:4], lg[:, 0:4], AF.Exp, bias=sc["ngmax"][:, 0:1], scale=1.0),
              [lg, sc["ngmax"]], [ge])
            D(lambda e: e.tensor_reduce(sc["gsum"][:], ge[:, 0:4], AX.X, ALU.add), [ge], [sc["gsum"]])
            D(lambda e: e.reciprocal(sc["grp"][:], sc["gsum"][:]), [sc["gsum"]], [sc["grp"]])
            D(lambda e: e.tensor_scalar(ohg[:, 0:4], lg[:, 0:4], sc["gmax"][:, 0:1], None, ALU.is_equal),
              [lg, sc["gmax"]], [ohg])
            D(lambda e: e.tensor_scalar(pen[:, 0:4], ohg[:, 0:4], 1e30, -1e30, ALU.mult, ALU.add), [ohg], [pen])
            D(lambda e: e.tensor_tensor(em[:, 0:32].rearrange("p (g k) -> p g k", k=8),
                                        lg[:, 4:36].rearrange("p (g k) -> p g k", k=8),
                                        pen[:, 0:4].unsqueeze(2).broadcast_to([128, 4, 8]), ALU.add),
              [lg, pen], [em])
            D(lambda e: e.tensor_reduce(sc["m1"][:], em[:, 0:32], AX.X, ALU.max), [em], [sc["m1"]])
            D(lambda e: e.tensor_scalar(oh1[:, 0:32], em[:, 0:32], sc["m1"][:, 0:1], None, ALU.is_equal),
              [em, sc["m1"]], [oh1])
            D(lambda e: e.scalar_tensor_tensor(em2[:, 0:32], oh1[:, 0:32], -1e30, em[:, 0:32], ALU.mult, ALU.add),
              [oh1, em], [em2])
            D(lambda e: e.tensor_reduce(sc["m2"][:], em2[:, 0:32], AX.X, ALU.max), [em2], [sc["m2"]])
            D(lambda e: e.tensor_scalar(oh2[:, 0:32], em2[:, 0:32], sc["m2"][:, 0:1], None, ALU.is_equal),
              [em2, sc["m2"]], [oh2])
            D(lambda e: e.tensor_tensor(sc["d"][:], sc["m2"][:], sc["m1"][:], ALU.subtract),
              [sc["m2"], sc["m1"]], [sc["d"]])
            A(lambda e: e.activation(sc["ed"][:], sc["d"][:], AF.Exp), [sc["d"]], [sc["ed"]])
            D(lambda e: e.tensor_scalar(sc["den"][:], sc["ed"][:], 1.0, None, ALU.add), [sc["ed"]], [sc["den"]])
            D(lambda e: e.reciprocal(sc["p1"][:], sc["den"][:]), [sc["den"]], [sc["p1"]])
            D(lambda e: e.tensor_tensor(sc["p2"][:], sc["ed"][:], sc["p1"][:], ALU.mult),
              [sc["ed"], sc["p1"]], [sc["p2"]])
            D(lambda e: e.tensor_tensor(sc["p1g"][:], sc["p1"][:], sc["grp"][:], ALU.mult),
              [sc["p1"], sc["grp"]], [sc["p1g"]])
            D(lambda e: e.tensor_tensor(sc["p2g"][:], sc["p2"][:], sc["grp"][:], ALU.mult),
              [sc["p2"], sc["grp"]], [sc["p2g"]])
            D(lambda e: e.tensor_scalar(g[:], oh1[:, 0:32], sc["p1g"][:, 0:1], None, ALU.mult),
              [oh1, sc["p1g"]], [g])
            D(lambda e: e.scalar_tensor_tensor(g[:], oh2[:, 0:32], sc["p2g"][:, 0:1], g[:], ALU.mult, ALU.add),
              [oh2, sc["p2g"], g], [g])

    def emit_G(e_):
        for tt in range(8):
            bk = bank[6 + tt // 4]
            P.op("pe", lambda e: e.matmul(bk[:, (tt % 4) * 128:(tt % 4 + 1) * 128],
                                          gd[tt][:, e_:e_ + 1].broadcast_to([128, 128]), ident[:],
                                          start=True, stop=True),
                 reads=[gd[tt], ident], writes=[bk], defer=(tt % 4 != 3))
        gb = G[e_ % 2]
        for hh in range(2):
            P.op("act", lambda e: e.activation(gb[:, hh * 512:(hh + 1) * 512], bank[6 + hh][:], AF.Copy),
                 reads=[bank[6 + hh]], writes=[gb])

    NE = 32
    pend = []
    for m in range(2):
        pass
    loads = {}

    def issue_loads(e_):
        loads[e_] = (wload(w1_d[e_], "k16"), wload(w3_d[e_], "k16"), wload(w2_d[e_], "k4"))

    issue_loads(0)
    emit_G(0)
    k = 0
    for e_ in range(NE):
        (s1, v1), (s3, v3), (s2, v2) = loads[e_]
        gb = G[e_ % 2]
        for h in range(2):
            for jc in range(4):
                b1 = bank[k % 2]
                b3 = bank[2 + k % 2]
                for j in range(16):
                    P.op("pe", lambda e: e.matmul(b1[:], v1[:, j, jc * 128:(jc + 1) * 128], hT[j * 2 + h][:],
                                                  start=(j == 0), stop=(j == 15)),
                         reads=[s1, hT[j * 2 + h]], writes=[b1], defer=(j != 15))
                for j in range(16):
                    P.op("pe", lambda e: e.matmul(b3[:], v3[:, j, jc * 128:(jc + 1) * 128], hT[j * 2 + h][:],
                                                  start=(j == 0), stop=(j == 15)),
                         reads=[s3, hT[j * 2 + h]], writes=[b3], defer=(j != 15))
                t1 = tmp1[k % 2]
                t2 = tmp2[k % 2]
                P.op("act", lambda e: e.activation(t1[:], b1[:], AF.Silu), reads=[b1], writes=[t1])
                P.op("dve", lambda e: e.tensor_tensor(t2[:], t1[:], b3[:], ALU.mult), reads=[t1, b3], writes=[t2])
                P.op("dve", lambda e: e.tensor_tensor(aT[jc][h][:], t2[:], gb[:, h * 512:(h + 1) * 512], ALU.mult),
                     reads=[t2, gb], writes=[aT[jc][h]])
                k += 1
            if h == 0 and e_ + 1 < NE:
                pass
        if e_ + 1 < NE:
            emit_G(e_ + 1)
        for h in range(2):
            for fc in range(16):
                by = bank[4 + fc % 2]
                for jc in range(4):
                    P.op("pe", lambda e: e.matmul(by[:], v2[:, jc, fc * 128:(fc + 1) * 128], aT[jc][h][:],
                                                  start=(jc == 0), stop=(jc == 3)),
                         reads=[s2, aT[jc][h]], writes=[by], defer=(jc != 3))
                xb = xT[fc][h]
                P.op("dve", lambda e: e.scalar_tensor_tensor(xb[:], by[:], V(GATE_FFN)[:, fc:fc + 1], xb[:],
                                                             ALU.mult, ALU.add),
                     reads=[by, vec, xb], writes=[xb])
        if e_ + 1 < NE:
            issue_loads(e_ + 1)

    outs = []
    if final:
        for h in range(2):
            sum_sq_rstd([(xT[j][h], xT[j][h][:]) for j in range(16)], h, 2048)
            for j in range(16):
                xb = xT[j][h]
                P.op("dve", lambda e: e.tensor_tensor(xb[:], xb[:], rstd[h][:], ALU.mult),
                     reads=[xb, rstd[h]], writes=[xb])
                P.op("dve", lambda e: e.tensor_scalar(xb[:], xb[:], V(FING)[:, j:j + 1], None, ALU.mult),
                     reads=[xb, vec], writes=[xb])
    for j in range(16):
        for h in range(2):
            xb = xT[j][h]
            P.dma("sp" if (j + h) % 2 else "act", out_d[j * 128:(j + 1) * 128, h * 512:(h + 1) * 512], xb[:],
                  reads=[xb], sb=xb)
            outs.append(xb)
    P.wait_all("sp", outs)
    P.wait_all("act", outs)
    P.close()
    return nc

from concourse.bass_utils import run_bass_kernel_spmd

_CORES = list(range(8))


def _run(nc, maps):
    return run_bass_kernel_spmd(nc, maps, core_ids=_CORES).results


def tail_inputs(layer, xfull, cat, mod, inp):
    m_mix = mod[2 * layer]
    m_ffn = mod[2 * layer + 1]
    vec = np.concatenate([fm(m_mix[4096:]), fm(m_ffn[:2048]), fm(m_ffn[2048:4096]), fm(m_ffn[4096:]),
                          fm(inp["norm_g"][layer, 1]), fm(inp["final_norm_g"])], axis=1)
    vec = np.ascontiguousarray(vec.astype(np.float32))
    if layer == 0:
        gcat = np.ones(2048, np.float32)
        wout = inp["e_w_out"][0]
    else:
        gcat = inp["o_norm_g"][0]
        wout = inp["o_w_out"][0]
    wr = np.ascontiguousarray(np.concatenate([inp["moe_w_group"][layer], inp["moe_w_expert"][layer]], axis=1))
    br = np.concatenate([inp["moe_b_group"][layer], inp["moe_b_expert"][layer]])[None, :]
    br = np.ascontiguousarray(np.tile(br, (128, 1)).astype(np.float32))
    ident = np.eye(128, dtype=np.float32)
    w1 = np.ascontiguousarray(inp["moe_w1"][layer])
    w3 = np.ascontiguousarray(inp["moe_w3"][layer])
    w2 = np.ascontiguousarray(inp["moe_w2"][layer])
    maps = []
    for i in range(8):
        sl = slice(i * 1024, (i + 1) * 1024)
        maps.append({"xT": np.ascontiguousarray(xfull[sl].T), "catT": np.ascontiguousarray(cat[sl].T), "vec": vec,
                     "gcat": fm(gcat), "wout": np.ascontiguousarray(wout), "wr": wr, "br": br,
                     "w1": w1, "w3": w3, "w2": w2, "ident": ident})
    return maps


def kernel(**inputs):
    inp = {k: np.asarray(v, dtype=np.float32) for k, v in inputs.items()}
    x0 = inp["x"][0]
    c = inp["c"][0]
    cT = np.ascontiguousarray(c.reshape(16, 128).T)
    maps = [{"cT": cT, "wmod": np.ascontiguousarray(inp["w_mod"][:, :, i * 768:(i + 1) * 768]),
             "bmod": np.ascontiguousarray(inp["b_mod"][:, i * 768:(i + 1) * 768])} for i in range(8)]
    res = _run(build_mod(), maps)
    mod = np.concatenate([res[i]["mod"] for i in range(8)], axis=1)
    res = _run(build_pre(0), pre_inputs(0, x0, mod, inp))
    pout0 = np.concatenate([res[i]["pout"] for i in range(8)], axis=1)
    res = _run(build_gdn(), gdn_inputs(pout0, inp))
    a_out = np.concatenate([res[h]["o_tok"] for h in range(8)], axis=1)
    cat0 = np.concatenate([a_out, pout0[3072:4096].T], axis=1)
    res = _run(build_tail(0), tail_inputs(0, x0, cat0, mod, inp))
    x1 = np.concatenate([res[i]["outT"].T for i in range(8)], axis=0)
    res = _run(build_pre(1), pre_inputs(1, x1, mod, inp))
    pout1 = np.concatenate([res[i]["pout"] for i in range(8)], axis=1)
    res = _run(build_ssd(), ssd_inputs(pout1, inp))
    yz = np.concatenate([res[g]["yz_tok"] for g in range(8)], axis=1)
    res = _run(build_tail(1), tail_inputs(1, x1, yz, mod, inp))
    out = np.concatenate([res[i]["outT"].T for i in range(8)], axis=0)
    return np.ascontiguousarray(out[None].astype(np.float32))
```

```python
import numpy as np

from contextlib import ExitStack
import concourse.bass as bass
import concourse.mybir as mybir

F32 = mybir.dt.float32
BF16 = mybir.dt.bfloat16
I32 = mybir.dt.int32
AF = mybir.ActivationFunctionType
ALU = mybir.AluOpType
AX = mybir.AxisListType


class Buf:
    __slots__ = ("t", "name", "w", "r", "dsem")

    def __init__(self, t, name):
        self.t = t
        self.name = name
        self.w = None
        self.r = {}
        self.dsem = None

    def __getitem__(self, idx):
        return self.t[idx]


class View:
    __slots__ = ("buf", "ap")

    def __init__(self, buf, ap):
        self.buf = buf
        self.ap = ap

    def __getitem__(self, idx):
        return self.ap[idx]


class Prog:
    def __init__(self, nc):
        self.nc = nc
        self.st = ExitStack()
        self.eng = {"pe": nc.tensor, "act": nc.scalar, "dve": nc.vector,
                    "pool": nc.gpsimd, "sp": nc.sync}
        self.sems = {}
        self.cnt = {}
        self.waited = {e: {} for e in self.eng}
        for e in ("pe", "act", "dve", "pool"):
            self.sems[e] = self.st.enter_context(nc.semaphore("s_" + e))
            self.cnt[e] = 0
        self.nbuf = 0
        self.pending = {}
        self.dma_sem_free = []
        self.ndsem = 0

    def sbuf(self, shape, dt, name=None):
        self.nbuf += 1
        name = name or f"sb{self.nbuf}"
        t = self.st.enter_context(self.nc.sbuf_tensor("S_" + name, list(shape), dt))
        return Buf(t, name)

    def psum(self, shape, dt, name=None):
        self.nbuf += 1
        name = name or f"ps{self.nbuf}"
        t = self.st.enter_context(self.nc.psum_tensor("P_" + name, list(shape), dt))
        return Buf(t, name)

    def dram(self, name, shape, dt, kind):
        t = self.nc.dram_tensor(name, list(shape), dt, kind=kind)
        return Buf(t.ap(), name)

    def new_dma_sem(self):
        self.ndsem += 1
        k = f"d{self.ndsem}"
        self.sems[k] = self.st.enter_context(self.nc.semaphore("s_" + k))
        self.cnt[k] = 0
        return k

    def _wait(self, e, deps):
        eng = self.eng[e]
        need = {}
        for d in deps:
            if d is None:
                continue
            k, v = d
            if e == "pe" and k == "pe":
                continue
            if v > need.get(k, 0):
                need[k] = v
        for k, v in need.items():
            if self.waited[e].get(k, 0) < v:
                eng.wait_ge(self.sems[k], v)
                self.waited[e][k] = v

    def _deps(self, reads, writes):
        reads = [getattr(b, "buf", b) for b in reads]
        writes = [getattr(b, "buf", b) for b in writes]
        deps = []
        for b in reads:
            deps.append(b.w)
        for b in writes:
            deps.append(b.w)
            deps.extend(b.r.items())
        return deps

    def _mark(self, key, val, reads, writes):
        reads = [getattr(b, "buf", b) for b in reads]
        writes = [getattr(b, "buf", b) for b in writes]
        for b in reads:
            if b.r.get(key, 0) < val:
                b.r[key] = val
        for b in writes:
            b.w = (key, val)
            b.r = {}

    def op(self, e, fn, reads=(), writes=(), defer=False):
        self._wait(e, self._deps(reads, writes))
        inst = fn(self.eng[e])
        if defer:
            pr, pw = self.pending.setdefault(e, ([], []))
            pr.extend(reads)
            pw.extend(writes)
            self._mark(e, self.cnt[e] + 1, reads, writes)
            return inst
        self.cnt[e] += 1
        inst.then_inc(self.sems[e], 1)
        if e in self.pending:
            self.pending.pop(e)
        self._mark(e, self.cnt[e], reads, writes)
        return inst

    def dma(self, q, out, in_, reads=(), writes=(), sb=None, **kw):
        if sb.dsem is None:
            sb.dsem = self.new_dma_sem()
        sem = sb.dsem
        self._wait(q, self._deps(reads, writes))
        inst = self.eng[q].dma_start(out=out, in_=in_, **kw)
        self.cnt[sem] += 16
        inst.then_inc(self.sems[sem], 16)
        self._mark(sem, self.cnt[sem], reads, writes)
        return inst

    def wait_all(self, e, bufs):
        self._wait(e, self._deps(bufs, bufs))

    def close(self):
        self.st.close()

EPS = 1e-6


def fm(v):
    return np.ascontiguousarray(np.asarray(v, np.float32).reshape(-1, 128).T)

def fmk(w):
    K, n = w.shape[0], w.shape[1] // 128
    return np.ascontiguousarray(np.asarray(w, np.float32).reshape(K, n, 128).transpose(2, 0, 1).reshape(128, K * n))

def pre_inputs(layer, xfull, mod, inp):
    m = mod[2 * layer]
    vec = np.concatenate([fm(m[:2048]), fm(m[2048:4096]), fm(inp["norm_g"][layer, 0])], axis=1)
    if layer == 0:
        w = inp["e_w_in"][0]
        glu_a = w[:, 4112:5136].reshape(2048, 8, 128); glu_b = w[:, 5136:6160].reshape(2048, 8, 128)
        glu = np.stack([glu_a, glu_b], axis=2).reshape(2048, 2048)
        win = np.concatenate([w[:, 0:3072], glu, w[:, 3072:4096], w[:, 4096:4112]], axis=1)
        convw = fmk(inp["e_conv_qkv"][0]); convb = np.zeros((128, 24), np.float32)
        extra = {"dw": fmk(inp["e_conf_dw"][0]),
                 "dvec": np.concatenate([fm(inp["e_conf_dw_b"][0]), fm(inp["e_conf_ln_g"][0]), fm(inp["e_conf_ln_b"][0])], axis=1)}
    else:
        w = inp["o_w_in"][0]
        win = np.concatenate([w[:, 4096:10240], w[:, 0:4096], w[:, 10240:10304]], axis=1)
        convw = fmk(inp["o_conv_w"][0]); convb = fm(inp["o_conv_b"][0])
        extra = {}
    win = np.ascontiguousarray(win)
    xT = np.ascontiguousarray(xfull.T)
    maps = []
    for i in range(8):
        xs = np.zeros((2048, 1056), np.float32)
        lo = i * 1024 - 32
        if i == 0:
            xs[:, 32:] = xT[:, 0:1024]
        else:
            xs[:] = xT[:, lo:lo + 1056]
        hmask = np.full((128, 1), 0.0 if i == 0 else 1.0, np.float32)
        maps.append({"xT": xs, "vec": vec, "win": win, "convw": convw, "convb": convb, "hmask": hmask, **extra})
    return maps

def cmat():
    j = np.arange(128)[:, None]; i = np.arange(128)[None, :]
    same = (j // 64) == (i // 64)
    tri = ((j <= i) & same).astype(np.float32)
    blk = same.astype(np.float32)
    sel0 = np.broadcast_to(j < 64, (128, 128)).astype(np.float32)
    sel1 = np.broadcast_to(j >= 64, (128, 128)).astype(np.float32)
    return np.ascontiguousarray(np.concatenate([tri, blk, sel0, sel1], axis=1))

def ssd_inputs(pout, inp):
    maps = []
    cm = cmat()
    for g in range(8):
        xT = pout[g * 512:(g + 1) * 512]; zT = pout[6144 + g * 512:6144 + (g + 1) * 512]
        BT = np.ascontiguousarray(pout[4096 + g * 128:4096 + (g + 1) * 128]); CT = np.ascontiguousarray(pout[5120 + g * 128:5120 + (g + 1) * 128])
        dtr = pout[10240 + g * 8:10240 + (g + 1) * 8]
        dtr = np.ascontiguousarray(dtr.reshape(8, 64, 128).transpose(2, 1, 0).reshape(128, 512))
        hs = slice(g * 8, (g + 1) * 8)
        rv = np.concatenate([inp["o_dt_bias"][0][hs], inp["o_a_log"][0][hs], inp["o_d_skip"][0][hs]])[None, :]
        rv = np.ascontiguousarray(np.tile(rv, (128, 1)).astype(np.float32))
        maps.append({"x_tok": np.ascontiguousarray(xT.T), "z_tok": np.ascontiguousarray(zT.T), "BT": BT, "CT": CT,
                     "B_tok": np.ascontiguousarray(BT.T), "dtr": dtr, "rowvec": rv, "cmat": cm})
    return maps

def cmat_gdn():
    c = cmat()
    j = np.arange(128)[:, None]; i = np.arange(128)[None, :]
    masks = ((j > i) & ((j // 64) == (i // 64))).astype(np.float32)
    return np.ascontiguousarray(np.concatenate([c, np.eye(128, dtype=np.float32), masks], axis=1))

def gdn_inputs(pout, inp):
    cm = cmat_gdn()
    gn = np.ascontiguousarray(np.tile(inp["e_head_norm_g"][0][None, :], (128, 1)).astype(np.float32))
    maps = []
    for hd in range(8):
        r = slice(hd * 128, (hd + 1) * 128)
        qT = np.ascontiguousarray(pout[0:1024][r]); kT = np.ascontiguousarray(pout[1024:2048][r])
        vT = pout[2048:3072][r]; zT = pout[4096:5120][r]
        bl = np.ascontiguousarray(pout[5120 + hd].reshape(64, 128).T); al = np.ascontiguousarray(pout[5128 + hd].reshape(64, 128).T)
        rv = np.tile(np.array([[inp["e_a_log"][0][hd], inp["e_dt_bias"][0][hd]]], np.float32), (128, 1))
        maps.append({"qT": qT, "kT": kT, "k_tok": np.ascontiguousarray(kT.T), "v_tok": np.ascontiguousarray(vT.T),
                     "z_tok": np.ascontiguousarray(zT.T), "bl": bl, "al": al, "rowvec": np.ascontiguousarray(rv),
                     "gn": gn, "cmat": cm})
    return maps


def build_mod():
    nc = bass.Bass("TRN2", target_bir_lowering=False)
    P = Prog(nc)
    NCOL = 768
    c_d = P.dram("cT", [128, 16], F32, "ExternalInput")
    w_d = P.dram("wmod", [4, 2048, NCOL], F32, "ExternalInput")
    b_d = P.dram("bmod", [4, NCOL], F32, "ExternalInput")
    o_d = P.dram("mod", [4, NCOL], F32, "ExternalOutput")
    cs = P.sbuf([128, 16], F32, "cs")
    sc = P.sbuf([128, 16], F32, "sc")
    wb = [P.sbuf([128, 16, NCOL], F32, f"w{i}") for i in range(2)]
    bs = P.sbuf([1, 4 * NCOL], F32, "bs")
    os_ = P.sbuf([1, 4 * NCOL], F32, "os")
    bank = [P.psum([128, 512], F32, f"bank{i}") for i in range(2)]
    P.dma("sp", cs[:], c_d[:], writes=[cs], sb=cs)
    P.dma("sp", bs[:], b_d.t.rearrange("(o l) n -> o (l n)", o=1), writes=[bs], sb=bs)
    P.op("act", lambda e: e.activation(sc[:], cs[:], AF.Silu), reads=[cs], writes=[sc])
    k = 0
    for l in range(4):
        w = wb[l % 2]
        P.dma("sp" if l % 2 else "act", w[:], w_d[l].rearrange("(j p) n -> p j n", p=128), writes=[w], sb=w)
        for nh in range(2):
            bk = bank[k % 2]
            k += 1
            for j in range(16):
                P.op("pe", lambda e: e.matmul(bk[0:1, 0:384], sc[:, j:j + 1], w[:, j, nh * 384:(nh + 1) * 384],
                                              start=(j == 0), stop=(j == 15)),
                     reads=[sc, w], writes=[bk], defer=(j != 15))
            o = l * NCOL + nh * 384
            P.op("dve", lambda e: e.tensor_tensor(os_[0:1, o:o + 384], bk[0:1, 0:384], bs[0:1, o:o + 384], ALU.add),
                 reads=[bk, bs], writes=[os_])
    P.dma("sp", o_d.t.rearrange("(o l) n -> o (l n)", o=1), os_[:], reads=[os_], sb=os_)
    P.wait_all("sp", [os_])
    P.close()
    return nc


def build_pre(layer):
    NTK, HALO, NT = 1056, 32, 1024
    pieces = [(0, 32), (32, 544), (544, 1056)]
    if layer == 0:
        plan = [("conv", i) for i in range(24)] + [("glu", i) for i in range(16)] + \
               [("pass", i) for i in range(8)] + [("passp", 0)]
        NW, NOUT, NCONV, PW = 6160, 5136, 24, 16
        out_row = {"conv": 0, "glu": 3072, "pass": 4096, "passp": 5120}
    else:
        plan = [("conv", i) for i in range(48)] + [("pass", i) for i in range(32)] + [("passp", 0)]
        NW, NOUT, NCONV, PW = 10304, 10304, 48, 64
        out_row = {"conv": 0, "pass": 6144, "passp": 10240}
    nc = bass.Bass("TRN2", target_bir_lowering=False)
    P = Prog(nc)
    xT_d = P.dram("xT", [2048, NTK], F32, "ExternalInput")
    vec_d = P.dram("vec", [128, 48], F32, "ExternalInput")
    w_d = P.dram("win", [2048, NW], F32, "ExternalInput")
    cw_d = P.dram("convw", [128, 4 * NCONV], F32, "ExternalInput")
    cb_d = P.dram("convb", [128, NCONV], F32, "ExternalInput")
    hm_d = P.dram("hmask", [128, 1], F32, "ExternalInput")
    if layer == 0:
        dw_d = P.dram("dw", [128, 31 * 8], F32, "ExternalInput")
        dv_d = P.dram("dvec", [128, 24], F32, "ExternalInput")
    out_d = P.dram("pout", [NOUT, NT], F32, "ExternalOutput")

    big = [P.sbuf([128, NTK], F32, f"big{i}") for i in range(16)]
    hT = [P.sbuf([128, NTK], BF16, f"h{j}") for j in range(16)]
    wsl = [P.sbuf([128, 16, 512], BF16, f"wsl{i}") for i in range(3)]
    rstd = P.sbuf([128, NTK], F32, "rstd")
    sqb = [P.sbuf([128, 512], F32, f"sq{i}") for i in range(2)]
    tsm = [P.sbuf([128, 512], F32, f"tsm{i}") for i in range(2)]
    ones = P.sbuf([128, 128], F32, "ones")
    vec = P.sbuf([128, 48], F32, "vec")
    gs = P.sbuf([128, 16], F32, "gs")
    cw = P.sbuf([128, 4 * NCONV], F32, "cw")
    cb = P.sbuf([128, NCONV], F32, "cb")
    hm = P.sbuf([128, 1], F32, "hm")
    bank = [P.psum([128, 512], F32, f"bank{i}") for i in range(8)]
    if layer == 0:
        dw = P.sbuf([128, 31 * 8], F32, "dw")
        dv = P.sbuf([128, 24], F32, "dv")
        P.dma("sp", dw[:], dw_d[:], writes=[dw], sb=dw)
        P.dma("sp", dv[:], dv_d[:], writes=[dv], sb=dv)
    P.dma("sp", vec[:], vec_d[:], writes=[vec], sb=vec)
    P.dma("sp", cw[:], cw_d[:], writes=[cw], sb=cw)
    P.dma("sp", cb[:], cb_d[:], writes=[cb], sb=cb)
    P.dma("sp", hm[:], hm_d[:], writes=[hm], sb=hm)
    P.op("dve", lambda e: e.memset(ones[:], 1.0), writes=[ones])
    for j in range(16):
        P.dma("act" if j % 2 else "sp", big[j][:], xT_d[j * 128:(j + 1) * 128, :], writes=[big[j]], sb=big[j])

    P.op("dve", lambda e: e.tensor_scalar(gs[:], vec[:, 16:32], 1.0, None, ALU.add), reads=[vec], writes=[gs])
    P.op("dve", lambda e: e.tensor_tensor(gs[:], gs[:], vec[:, 32:48], ALU.mult), reads=[gs, vec], writes=[gs])
    for (lo, hi) in pieces:
        n = hi - lo
        for j in range(16):
            q = sqb[j % 2]
            P.op("act", lambda e: e.activation(q[:, 0:n], big[j][:, lo:hi], AF.Square), reads=[big[j]], writes=[q])
            P.op("pe", lambda e: e.matmul(bank[6][:, 0:n], ones[:], q[:, 0:n], start=(j == 0), stop=(j == 15)),
                 reads=[ones, q], writes=[bank[6]])
        t = tsm[0]
        P.op("act", lambda e: e.activation(t[:, 0:n], bank[6][:, 0:n], AF.Sqrt, bias=EPS, scale=1.0 / 2048),
             reads=[bank[6]], writes=[t])
        P.op("dve", lambda e: e.reciprocal(rstd[:, lo:hi], t[:, 0:n]), reads=[t], writes=[rstd])
    for j in range(16):
        P.op("dve", lambda e: e.tensor_tensor(big[j][:], big[j][:], rstd[:], ALU.mult),
             reads=[big[j], rstd], writes=[big[j]])
        P.op("dve", lambda e: e.tensor_scalar(hT[j][:], big[j][:], gs[:, j:j + 1], vec[:, j:j + 1], ALU.mult, ALU.add),
             reads=[big[j], gs, vec], writes=[hT[j]])

    rot = {"n": 0}
    if layer == 0:
        cv = big[0:8]
        pa = big[8]
        pool = big[9:16]
    else:
        pool = big

    def nextbuf():
        b = pool[rot["n"] % len(pool)]
        rot["n"] += 1
        return b

    nb = {"n": 0}
    wv = None
    for ci, (kind, idx) in enumerate(plan):
        blk = ci // 4
        if ci % 4 == 0:
            width = min(512, NW - blk * 512)
            ws = wsl[blk % 3]
            P.dma("pool", ws[:, :, 0:width], w_d[:, blk * 512:blk * 512 + width].rearrange("(j p) n -> p j n", p=128),
                  writes=[ws], sb=ws)
        off = (ci % 4) * 128
        wc = PW if kind == "passp" else 128
        halo = kind in ("conv", "glu")
        pr = pa if (kind == "glu" and idx % 2 == 0) else nextbuf()
        for pi, (lo, hi) in enumerate(pieces):
            if pi == 0 and not halo:
                continue
            n = hi - lo
            bk = bank[nb["n"] % 4]
            nb["n"] += 1
            for j in range(16):
                P.op("pe", lambda e: e.matmul(bk[0:wc, 0:n], ws[:, j, off:off + wc], hT[j][:, lo:hi],
                                              start=(j == 0), stop=(j == 15)),
                     reads=[ws, hT[j]], writes=[bk], defer=(j != 15))
            if pi == 0:
                P.op("act", lambda e: e.activation(pr[0:wc, lo:hi], bk[0:wc, 0:n], AF.Copy, scale=hm[0:wc, 0:1]),
                     reads=[bk, hm], writes=[pr])
            else:
                P.op("act", lambda e: e.activation(pr[0:wc, lo:hi], bk[0:wc, 0:n], AF.Copy), reads=[bk], writes=[pr])
        if kind == "conv":
            acc = nextbuf()
            P.op("dve", lambda e: e.tensor_scalar(acc[:, 0:NT], pr[:, 29:29 + NT], cw[:, idx:idx + 1], None, ALU.mult),
                 reads=[pr, cw], writes=[acc])
            for k in range(1, 4):
                P.op("dve", lambda e: e.scalar_tensor_tensor(acc[:, 0:NT], pr[:, 29 + k:29 + k + NT],
                                                             cw[:, k * NCONV + idx:k * NCONV + idx + 1], acc[:, 0:NT],
                                                             ALU.mult, ALU.add),
                     reads=[pr, cw, acc], writes=[acc])
            P.op("act", lambda e: e.activation(acc[:, 0:NT], acc[:, 0:NT], AF.Silu, bias=cb[:, idx:idx + 1], scale=1.0),
                 reads=[acc, cb], writes=[acc])
            if layer == 0 and idx < 16:
                qs = 128.0 ** -0.5 if idx < 8 else 1.0
                for hh in range(2):
                    sl = slice(hh * 512, (hh + 1) * 512)
                    q = sqb[hh]
                    P.op("act", lambda e: e.activation(q[:], acc[:, sl], AF.Square), reads=[acc], writes=[q])
                    P.op("pe", lambda e: e.matmul(bank[6][:], ones[:], q[:], start=True, stop=True),
                         reads=[ones, q], writes=[bank[6]])
                    t = tsm[hh]
                    P.op("act", lambda e: e.activation(t[:], bank[6][:], AF.Sqrt, bias=EPS, scale=1.0),
                         reads=[bank[6]], writes=[t])
                    P.op("dve", lambda e: e.reciprocal(t[:], t[:]), reads=[t], writes=[t])
                    P.op("dve", lambda e: e.scalar_tensor_tensor(acc[:, sl], acc[:, sl], qs, t[:], ALU.mult, ALU.mult),
                         reads=[acc, t], writes=[acc])
            r0 = out_row["conv"] + idx * 128
            P.dma("sp" if ci % 2 else "act", out_d[r0:r0 + 128, :], acc[:, 0:NT], reads=[acc], sb=acc)
        elif kind == "glu":
            if idx % 2 == 0:
                continue
            c = idx // 2
            P.op("act", lambda e: e.activation(pr[:], pr[:], AF.Sigmoid), reads=[pr], writes=[pr])
            P.op("dve", lambda e: e.tensor_tensor(pr[:], pr[:], pa[:], ALU.mult), reads=[pr, pa], writes=[pr])
            acc = cv[c]
            P.op("dve", lambda e: e.tensor_scalar(acc[:, 0:NT], pr[:, 2:2 + NT], dw[:, c:c + 1], None, ALU.mult),
                 reads=[pr, dw], writes=[acc])
            for k in range(1, 31):
                P.op("dve", lambda e: e.scalar_tensor_tensor(acc[:, 0:NT], pr[:, 2 + k:2 + k + NT],
                                                             dw[:, k * 8 + c:k * 8 + c + 1], acc[:, 0:NT],
                                                             ALU.mult, ALU.add),
                     reads=[pr, dw, acc], writes=[acc])
            P.op("dve", lambda e: e.tensor_scalar(acc[:, 0:NT], acc[:, 0:NT], dv[:, c:c + 1], None, ALU.add),
                 reads=[acc, dv], writes=[acc])
            if c == 7:
                for hh in range(2):
                    sl = slice(hh * 512, (hh + 1) * 512)
                    for c2 in range(8):
                        P.op("pe", lambda e: e.matmul(bank[6][:], ones[:], cv[c2][:, sl], start=(c2 == 0), stop=(c2 == 7)),
                             reads=[ones, cv[c2]], writes=[bank[6]], defer=(c2 != 7))
                    mb = tsm[0]
                    P.op("act", lambda e: e.activation(mb[:], bank[6][:], AF.Copy, scale=1.0 / 1024),
                         reads=[bank[6]], writes=[mb])
                    for c2 in range(8):
                        P.op("dve", lambda e: e.tensor_tensor(cv[c2][:, sl], cv[c2][:, sl], mb[:], ALU.subtract),
                             reads=[cv[c2], mb], writes=[cv[c2]])
                        q = sqb[c2 % 2]
                        P.op("act", lambda e: e.activation(q[:], cv[c2][:, sl], AF.Square), reads=[cv[c2]], writes=[q])
                        P.op("pe", lambda e: e.matmul(bank[7][:], ones[:], q[:], start=(c2 == 0), stop=(c2 == 7)),
                             reads=[ones, q], writes=[bank[7]])
                    t = tsm[1]
                    P.op("act", lambda e: e.activation(t[:], bank[7][:], AF.Sqrt, bias=EPS, scale=1.0 / 1024),
                         reads=[bank[7]], writes=[t])
                    P.op("dve", lambda e: e.reciprocal(t[:], t[:]), reads=[t], writes=[t])
                    for c2 in range(8):
                        P.op("dve", lambda e: e.tensor_tensor(cv[c2][:, sl], cv[c2][:, sl], t[:], ALU.mult),
                             reads=[cv[c2], t], writes=[cv[c2]])
                        P.op("act", lambda e: e.activation(cv[c2][:, sl], cv[c2][:, sl], AF.Silu,
                                                           bias=dv[:, 16 + c2:17 + c2], scale=dv[:, 8 + c2:9 + c2]),
                             reads=[cv[c2], dv], writes=[cv[c2]])
                for c2 in range(8):
                    r0 = out_row["glu"] + c2 * 128
                    P.dma("sp" if c2 % 2 else "act", out_d[r0:r0 + 128, :], cv[c2][:, 0:NT], reads=[cv[c2]], sb=cv[c2])
        else:
            r0 = out_row[kind] + idx * 128
            P.dma("sp" if ci % 2 else "act", out_d[r0:r0 + wc, :], pr[0:wc, HALO:NTK], reads=[pr], sb=pr)
    allb = big
    P.wait_all("sp", allb)
    P.wait_all("act", allb)
    P.close()
    return nc


def build_gdn():
    T, NTILE, GT = 8192, 64, 4
    nc = bass.Bass("TRN2", target_bir_lowering=False)
    P = Prog(nc)
    qT_d = P.dram("qT", [128, T], F32, "ExternalInput")
    kT_d = P.dram("kT", [128, T], F32, "ExternalInput")
    kk_d = P.dram("k_tok", [T, 128], F32, "ExternalInput")
    vk_d = P.dram("v_tok", [T, 128], F32, "ExternalInput")
    zk_d = P.dram("z_tok", [T, 128], F32, "ExternalInput")
    bl_d = P.dram("bl", [128, 64], F32, "ExternalInput")
    al_d = P.dram("al", [128, 64], F32, "ExternalInput")
    rv_d = P.dram("rowvec", [128, 2], F32, "ExternalInput")
    gn_d = P.dram("gn", [128, 128], F32, "ExternalInput")
    cm_d = P.dram("cmat", [128, 768], F32, "ExternalInput")
    out_d = P.dram("o_tok", [T, 128], F32, "ExternalOutput")

    cm = P.sbuf([128, 768], F32, "cm")
    TRI, BLK, SEL0, SEL1, IDENT, MASKS = (cm[:, i * 128:(i + 1) * 128] for i in range(6))
    rv = P.sbuf([128, 2], F32, "rv")
    gn = P.sbuf([128, 128], F32, "gn")
    names = ("bl", "al", "beta", "xb", "ax", "ex", "ln", "sp", "g", "gcs", "gtot", "eg", "kdsc", "beg",
             "dec0", "dec1", "aneg")
    S = {k: P.sbuf([128, 64], F32, "s_" + k) for k in names}
    St = [P.sbuf([128, 128], F32, f"St{i}") for i in range(2)]
    qTg = [P.sbuf([128, 512], F32, f"qTg{i}") for i in range(2)]
    kTg = [P.sbuf([128, 512], F32, f"kTg{i}") for i in range(2)]
    kkg = [P.sbuf([128, GT, 128], F32, f"kkg{i}") for i in range(2)]
    vkg = [P.sbuf([128, GT, 128], F32, f"vkg{i}") for i in range(2)]
    zkg = [P.sbuf([128, GT, 128], F32, f"zkg{i}") for i in range(2)]
    og = [P.sbuf([128, GT, 128], F32, f"og{i}") for i in range(2)]
    W = {}
    for gi in range(GT):
        for k in ("Y", "YT", "decn", "dect", "erb", "t", "N", "M", "Na", "Nb", "Ma", "Mb", "QKm", "qeg", "kd",
                  "kcdT", "u", "o", "sz", "rbs"):
            W[k, gi] = P.sbuf([128, 128], F32, f"w_{k}{gi}")
        for k in ("R0", "R1"):
            W[k, gi] = P.sbuf([128, 256], F32, f"w_{k}{gi}")
        for k in ("ss", "rs"):
            W[k, gi] = P.sbuf([128, 1], F32, f"w_{k}{gi}")
    bkA = [P.psum([128, 512], F32, f"bkA{i}") for i in range(GT)]
    bkB = [P.psum([128, 512], F32, f"bkB{i}") for i in range(GT)]
    def vw(b, lo, n):
        return View(b, b[:, lo:lo + n])
    A_q = [[vw(bkA[gi], q * 128, 128) for q in range(4)] for gi in range(GT)]
    A_h = [[vw(bkA[gi], h * 256, 256) for h in range(2)] for gi in range(GT)]
    B_q = [[vw(bkB[gi], q * 128, 128) for q in range(4)] for gi in range(GT)]
    pq = [B_q[0][0]]

    P.dma("sp", cm[:], cm_d[:], writes=[cm], sb=cm)
    P.dma("sp", rv[:], rv_d[:], writes=[rv], sb=rv)
    P.dma("sp", gn[:], gn_d[:], writes=[gn], sb=gn)
    P.dma("sp", S["bl"][:], bl_d[:], writes=[S["bl"]], sb=S["bl"])
    P.dma("sp", S["al"][:], al_d[:], writes=[S["al"]], sb=S["al"])

    D = lambda fn, r, w: P.op("dve", fn, reads=r, writes=w)
    A = lambda fn, r, w: P.op("act", fn, reads=r, writes=w)
    G = lambda fn, r, w: P.op("dve", fn, reads=r, writes=w)
    PE = lambda fn, r, w: P.op("pe", fn, reads=r, writes=w)

    A(lambda e: e.activation(S["beta"][:], S["bl"][:], AF.Sigmoid), [S["bl"]], [S["beta"]])
    D(lambda e: e.tensor_scalar(S["xb"][:], S["al"][:], rv[:, 1:2], None, ALU.add), [S["al"], rv], [S["xb"]])
    A(lambda e: e.activation(S["ax"][:], S["xb"][:], AF.Abs), [S["xb"]], [S["ax"]])
    A(lambda e: e.activation(S["ex"][:], S["ax"][:], AF.Exp, scale=-1.0), [S["ax"]], [S["ex"]])
    A(lambda e: e.activation(S["ln"][:], S["ex"][:], AF.Ln, bias=1.0, scale=1.0), [S["ex"]], [S["ln"]])
    D(lambda e: e.scalar_tensor_tensor(S["sp"][:], S["xb"][:], 0.0, S["ln"][:], ALU.max, ALU.add),
      [S["xb"], S["ln"]], [S["sp"]])
    A(lambda e: e.activation(S["aneg"][:, 0:1], rv[:, 0:1], AF.Exp), [rv], [S["aneg"]])
    D(lambda e: e.tensor_scalar(S["g"][:], S["sp"][:], S["aneg"][:, 0:1], -1.0, ALU.mult, ALU.mult),
      [S["sp"], S["aneg"]], [S["g"]])
    for (dst, mat, fn) in (("gcs", TRI, AF.Copy), ("gtot", BLK, AF.Copy), ("dec0", SEL0, AF.Exp), ("dec1", SEL1, AF.Exp)):
        PE(lambda e: e.matmul(pq[0][:, 0:64], mat, S["g"][:], start=True, stop=True), [cm, S["g"]], [pq[0]])
        A(lambda e: e.activation(S[dst][:], pq[0][:, 0:64], fn), [pq[0]], [S[dst]])
    A(lambda e: e.activation(S["eg"][:], S["gcs"][:], AF.Exp), [S["gcs"]], [S["eg"]])
    D(lambda e: e.tensor_tensor(S["kdsc"][:], S["gtot"][:], S["gcs"][:], ALU.subtract), [S["gtot"], S["gcs"]], [S["kdsc"]])
    A(lambda e: e.activation(S["kdsc"][:], S["kdsc"][:], AF.Exp), [S["kdsc"]], [S["kdsc"]])
    D(lambda e: e.tensor_tensor(S["beg"][:], S["beta"][:], S["eg"][:], ALU.mult), [S["beta"], S["eg"]], [S["beg"]])
    D(lambda e: e.memset(St[0][:], 0.0), [], [St[0]])

    def load_group(g):
        i = g % 2
        cs = slice(g * 512, (g + 1) * 512)
        P.dma("sp", qTg[i][:], qT_d[:, cs], writes=[qTg[i]], sb=qTg[i])
        P.dma("act", kTg[i][:], kT_d[:, cs], writes=[kTg[i]], sb=kTg[i])
        P.dma("sp", kkg[i][:], kk_d[cs, :].rearrange("(n p) d -> p n d", p=128), writes=[kkg[i]], sb=kkg[i])
        P.dma("act", vkg[i][:], vk_d[cs, :].rearrange("(n p) d -> p n d", p=128), writes=[vkg[i]], sb=vkg[i])
        P.dma("sp", zkg[i][:], zk_d[cs, :].rearrange("(n p) d -> p n d", p=128), writes=[zkg[i]], sb=zkg[i])

    NG = NTILE // GT
    load_group(0)
    cur = 0
    for g in range(NG):
        if g + 1 < NG:
            load_group(g + 1)
        i = g % 2
        tiles = range(GT)
        col = lambda gi: slice(g * GT + gi, g * GT + gi + 1)
        kT = lambda gi: kTg[i][:, gi * 128:(gi + 1) * 128]
        qT = lambda gi: qTg[i][:, gi * 128:(gi + 1) * 128]
        qa = lambda gi: A_q[gi][0]
        qb = lambda gi: A_q[gi][1]
        qc = lambda gi: A_q[gi][2]
        qd = lambda gi: B_q[gi][3]
        for gi in tiles:
            PE(lambda e: e.matmul(qa(gi)[:], S["g"][:, col(gi)].broadcast_to([128, 128]), TRI, start=True, stop=True),
               [S["g"], cm], [qa(gi)])
            PE(lambda e: e.matmul(qb(gi)[:], kT(gi), kT(gi), start=True, stop=True), [kTg[i]], [qb(gi)])
            PE(lambda e: e.matmul(qc(gi)[:], kT(gi), qT(gi), start=True, stop=True), [kTg[i], qTg[i]], [qc(gi)])
        for gi in tiles:
            w = lambda k: W[k, gi]
            gc_ = S["gcs"][:, col(gi)]
            D(lambda e: e.tensor_copy(w("rbs")[:], qa(gi)[:]), [qa(gi)], [w("rbs")])
            D(lambda e: e.tensor_scalar(w("Y")[:], w("rbs")[:], gc_, 0.0, ALU.subtract, ALU.max), [w("rbs"), S["gcs"]], [w("Y")])
            D(lambda e: e.tensor_scalar(w("YT")[:], w("rbs")[:], gc_, 0.0, ALU.subtract, ALU.min), [w("rbs"), S["gcs"]], [w("YT")])
        for gi in tiles:
            w = lambda k: W[k, gi]
            A(lambda e: e.activation(w("decn")[:], w("Y")[:], AF.Exp, scale=-1.0), [w("Y")], [w("decn")])
            A(lambda e: e.activation(w("dect")[:], w("YT")[:], AF.Exp), [w("YT")], [w("dect")])
            A(lambda e: e.activation(w("erb")[:], w("rbs")[:], AF.Exp), [w("rbs")], [w("erb")])
        for gi in tiles:
            w = lambda k: W[k, gi]
            G(lambda e: e.tensor_scalar(w("R0")[:, 0:128], vkg[i][:, gi, :], S["beta"][:, col(gi)], None, ALU.mult),
              [vkg[i], S["beta"]], [w("R0")])
            G(lambda e: e.tensor_scalar(w("R0")[:, 128:256], kkg[i][:, gi, :], S["beg"][:, col(gi)], None, ALU.mult),
              [kkg[i], S["beg"]], [w("R0")])
        for gi in tiles:
            w = lambda k: W[k, gi]
            D(lambda e: e.tensor_tensor(w("t")[:], qb(gi)[:], w("decn")[:], ALU.mult), [qb(gi), w("decn")], [w("t")])
            D(lambda e: e.scalar_tensor_tensor(w("N")[:], w("t")[:], S["beta"][:, col(gi)], MASKS, ALU.mult, ALU.mult),
              [w("t"), S["beta"], cm], [w("N")])
        for gi in tiles:
            PE(lambda e: e.matmul(qd(gi)[:], W["N", gi][:], IDENT, start=True, stop=True), [W["N", gi], cm], [qd(gi)])
        for gi in tiles:
            w = lambda k: W[k, gi]
            G(lambda e: e.tensor_tensor(w("dect")[:], w("dect")[:], TRI, ALU.mult), [w("dect"), cm], [w("dect")])
            D(lambda e: e.tensor_tensor(w("QKm")[:], qc(gi)[:], w("dect")[:], ALU.mult), [qc(gi), w("dect")], [w("QKm")])
            G(lambda e: e.tensor_tensor(w("qeg")[:], qT(gi), w("erb")[:], ALU.mult), [qTg[i], w("erb")], [w("qeg")])
            G(lambda e: e.tensor_scalar(w("kd")[:], kkg[i][:, gi, :], S["kdsc"][:, col(gi)], None, ALU.mult),
              [kkg[i], S["kdsc"]], [w("kd")])
        for gi in tiles:
            A(lambda e: e.activation(W["M", gi][:], qd(gi)[:], AF.Copy), [qd(gi)], [W["M", gi]])
        Mc = {gi: W["M", gi] for gi in tiles}
        Nc = {gi: W["N", gi] for gi in tiles}
        Rc = {gi: W["R0", gi] for gi in tiles}
        for lvl in range(6):
            sign = ALU.subtract if lvl == 0 else ALU.add
            apb = (lambda gi: A_h[gi][0]) if lvl % 2 == 0 else (lambda gi: A_h[gi][1])
            pm = (lambda gi: B_q[gi][0]) if lvl % 2 == 0 else (lambda gi: B_q[gi][2])
            pn = (lambda gi: B_q[gi][1]) if lvl % 2 == 0 else (lambda gi: B_q[gi][3])
            for gi in tiles:
                PE(lambda e: e.matmul(apb(gi)[:], Mc[gi][:], Rc[gi][:], start=True, stop=True), [Mc[gi], Rc[gi]], [apb(gi)])
                if lvl < 5:
                    PE(lambda e: e.matmul(pm(gi)[:], Nc[gi][:], Mc[gi][:], start=True, stop=True), [Nc[gi], Mc[gi]], [pm(gi)])
                if lvl < 4:
                    PE(lambda e: e.matmul(pn(gi)[:], Mc[gi][:], Nc[gi][:], start=True, stop=True), [Nc[gi], Mc[gi]], [pn(gi)])
            for gi in tiles:
                Rn = W["R1", gi] if Rc[gi] is W["R0", gi] else W["R0", gi]
                D(lambda e: e.tensor_tensor(Rn[:], Rc[gi][:], apb(gi)[:], sign), [Rc[gi], apb(gi)], [Rn])
                Rc[gi] = Rn
                if lvl < 5:
                    Mn = W["Ma", gi] if lvl % 2 == 0 else W["Mb", gi]
                    A(lambda e: e.activation(Mn[:], pm(gi)[:], AF.Copy), [pm(gi)], [Mn])
                if lvl < 4:
                    Nn = W["Na", gi] if lvl % 2 == 0 else W["Nb", gi]
                    A(lambda e: e.activation(Nn[:], pn(gi)[:], AF.Copy), [pn(gi)], [Nn])
                if lvl < 5:
                    Mc[gi] = Mn
                if lvl < 4:
                    Nc[gi] = Nn
        for gi in tiles:
            PE(lambda e: e.matmul(B_q[gi][0][:], Rc[gi][:, 128:256], IDENT, start=True, stop=True), [Rc[gi], cm], [B_q[gi][0]])
        for gi in tiles:
            A(lambda e: e.activation(W["kcdT", gi][:], B_q[gi][0][:], AF.Copy), [B_q[gi][0]], [W["kcdT", gi]])
        for gi in tiles:
            w = lambda k: W[k, gi]
            for c in range(2):
                lo, hi = c * 64, (c + 1) * 64
                s_old, s_new = St[cur], St[1 - cur]
                PE(lambda e: e.matmul(A_q[gi][0][:], w("kcdT")[:], s_old[:], start=True, stop=True), [w("kcdT"), s_old], [A_q[gi][0]])
                D(lambda e: e.tensor_tensor(w("u")[lo:hi, :], Rc[gi][lo:hi, 0:128], A_q[gi][0][lo:hi, :], ALU.subtract),
                  [Rc[gi], A_q[gi][0]], [w("u")])
                PE(lambda e: e.matmul(A_q[gi][1][:], w("kd")[lo:hi, :], w("u")[lo:hi, :], start=True, stop=True),
                   [w("kd"), w("u")], [A_q[gi][1]])
                P.op("pe", lambda e: e.matmul(B_q[gi][1][:], w("qeg")[:], s_old[:], start=True, stop=False),
                     reads=[w("qeg"), s_old], writes=[B_q[gi][1]], defer=True)
                PE(lambda e: e.matmul(B_q[gi][1][:], w("QKm")[lo:hi, :], w("u")[lo:hi, :], start=False, stop=True),
                   [w("QKm"), w("u")], [B_q[gi][1]])
                dec = S["dec0"] if c == 0 else S["dec1"]
                D(lambda e: e.scalar_tensor_tensor(s_new[:], s_old[:], dec[:, col(gi)], A_q[gi][1][:], ALU.mult, ALU.add),
                  [s_old, dec, A_q[gi][1]], [s_new])
                A(lambda e: e.activation(w("o")[lo:hi, :], B_q[gi][1][lo:hi, :], AF.Copy), [B_q[gi][1]], [w("o")])
                cur = 1 - cur
            A(lambda e: e.activation(w("sz")[:], w("o")[:], AF.Square, accum_out=w("ss")[:]), [w("o")], [w("sz"), w("ss")])
            A(lambda e: e.activation(w("rs")[:], w("ss")[:], AF.Sqrt, bias=EPS, scale=1.0 / 128), [w("ss")], [w("rs")])
            D(lambda e: e.reciprocal(w("rs")[:], w("rs")[:]), [w("rs")], [w("rs")])
            D(lambda e: e.scalar_tensor_tensor(w("o")[:], w("o")[:], w("rs")[:, 0:1], gn[:], ALU.mult, ALU.mult),
              [w("o"), w("rs"), gn], [w("o")])
            A(lambda e: e.activation(w("sz")[:], zkg[i][:, gi, :], AF.Silu), [zkg[i]], [w("sz")])
            D(lambda e: e.tensor_tensor(og[i][:, gi, :], w("o")[:], w("sz")[:], ALU.mult), [w("o"), w("sz")], [og[i]])
        P.dma("sp", out_d[g * 512:(g + 1) * 512, :].rearrange("(n p) d -> p n d", p=128), og[i][:],
              reads=[og[i]], sb=og[i])
    P.wait_all("sp", og)
    P.close()
    return nc


def build_ssd():
    T, NTILE, H, PD, NS = 8192, 64, 8, 64, 128
    nc = bass.Bass("TRN2", target_bir_lowering=False)
    P = Prog(nc)
    x_d = P.dram("x_tok", [T, 512], F32, "ExternalInput")
    z_d = P.dram("z_tok", [T, 512], F32, "ExternalInput")
    bt_d = P.dram("BT", [128, T], F32, "ExternalInput")
    ct_d = P.dram("CT", [128, T], F32, "ExternalInput")
    bk_d = P.dram("B_tok", [T, 128], F32, "ExternalInput")
    dtr_d = P.dram("dtr", [128, 512], F32, "ExternalInput")
    rv_d = P.dram("rowvec", [128, 24], F32, "ExternalInput")
    cm_d = P.dram("cmat", [128, 512], F32, "ExternalInput")
    out_d = P.dram("yz_tok", [T, 512], F32, "ExternalOutput")

    cm = P.sbuf([128, 512], F32, "cm")
    TRI, BLK, SEL0, SEL1 = (cm[:, i * 128:(i + 1) * 128] for i in range(4))
    rv = P.sbuf([128, 24], F32, "rv")
    dtr = P.sbuf([128, 512], F32, "dtr")
    names = ("xb", "ax", "ex", "ln", "dt", "da", "acs", "atot", "eacs", "dte", "dec0", "dec1", "aneg", "nacs")
    S = {k: P.sbuf([128, 512], F32, "s_" + k) for k in names}
    ST = [P.sbuf([128, 512], F32, f"ST{i}") for i in range(2)]
    NB = 3
    xt = [P.sbuf([128, 512], F32, f"xt{i}") for i in range(NB)]
    zt = [P.sbuf([128, 512], F32, f"zt{i}") for i in range(NB)]
    btt = [P.sbuf([128, 128], BF16, f"bt{i}") for i in range(NB)]
    ctt = [P.sbuf([128, 128], BF16, f"ct{i}") for i in range(NB)]
    bkt = [P.sbuf([128, 128], BF16, f"bk{i}") for i in range(NB)]
    xdd = [P.sbuf([128, 512], BF16, f"xdd{i}") for i in range(2)]
    xdt = [P.sbuf([128, 512], BF16, f"xdt{i}") for i in range(2)]
    cbm = [P.sbuf([128, 128], F32, f"cbm{i}") for i in range(2)]
    xx = [P.sbuf([128, 128], F32, f"xx{i}") for i in range(8)]
    mt = [P.sbuf([128, 128], BF16, f"mt{i}") for i in range(8)]
    STb = [P.sbuf([128, 512], BF16, f"STb{i}") for i in range(2)]
    yt = [P.sbuf([128, 512], F32, f"yt{i}") for i in range(2)]
    uo = [P.sbuf([128, 512], F32, f"uo{i}") for i in range(2)]
    bank = [P.psum([128, 512], F32, f"bank{i}") for i in range(8)]

    rbv = [View(bank[4 + h // 4], bank[4 + h // 4][:, (h % 4) * 128:(h % 4 + 1) * 128]) for h in range(8)]
    csbk = [bank[7], bank[1]]
    P.dma("sp", cm[:], cm_d[:], writes=[cm], sb=cm)
    P.dma("sp", rv[:], rv_d[:], writes=[rv], sb=rv)
    P.dma("sp", dtr[:], dtr_d[:], writes=[dtr], sb=dtr)

    def bc8(ap):
        return ap.unsqueeze(2).broadcast_to([ap.shape[0], 8, 64])

    def v3(ap):
        return ap.rearrange("p (h d) -> p h d", d=64)

    def rep(ap):
        return ap.unsqueeze(1).broadcast_to([128, 64, 8])

    def t3(ap):
        return ap.rearrange("p (n h) -> p n h", h=8)

    D = lambda fn, r, w: P.op("dve", fn, reads=r, writes=w)
    A = lambda fn, r, w: P.op("act", fn, reads=r, writes=w)
    D(lambda e: e.tensor_tensor(t3(S["xb"][:]), t3(dtr[:]), rep(rv[:, 0:8]), ALU.add), [dtr, rv], [S["xb"]])
    A(lambda e: e.activation(S["ax"][:], S["xb"][:], AF.Abs), [S["xb"]], [S["ax"]])
    A(lambda e: e.activation(S["ex"][:], S["ax"][:], AF.Exp, scale=-1.0), [S["ax"]], [S["ex"]])
    A(lambda e: e.activation(S["ln"][:], S["ex"][:], AF.Ln, bias=1.0, scale=1.0), [S["ex"]], [S["ln"]])
    D(lambda e: e.scalar_tensor_tensor(S["dt"][:], S["xb"][:], 0.0, S["ln"][:], ALU.max, ALU.add),
      [S["xb"], S["ln"]], [S["dt"]])
    A(lambda e: e.activation(S["aneg"][:, 0:8], rv[:, 8:16], AF.Exp), [rv], [S["aneg"]])
    D(lambda e: e.tensor_tensor(t3(S["da"][:]), t3(S["dt"][:]), rep(S["aneg"][:, 0:8]), ALU.mult),
      [S["dt"], S["aneg"]], [S["da"]])
    D(lambda e: e.tensor_scalar(S["da"][:], S["da"][:], -1.0, None, ALU.mult), [S["da"]], [S["da"]])
    for (dst, mat) in (("acs", TRI), ("atot", BLK), ("dec0", SEL0), ("dec1", SEL1)):
        P.op("pe", lambda e: e.matmul(bank[0][:], mat, S["da"][:], start=True, stop=True),
             reads=[cm, S["da"]], writes=[bank[0]])
        if dst in ("dec0", "dec1"):
            A(lambda e: e.activation(S[dst][:], bank[0][:], AF.Exp), [bank[0]], [S[dst]])
        else:
            A(lambda e: e.activation(S[dst][:], bank[0][:], AF.Copy), [bank[0]], [S[dst]])
    A(lambda e: e.activation(S["eacs"][:], S["acs"][:], AF.Exp), [S["acs"]], [S["eacs"]])
    A(lambda e: e.activation(S["nacs"][:], S["acs"][:], AF.Copy, scale=-1.0), [S["acs"]], [S["nacs"]])
    D(lambda e: e.tensor_tensor(S["dte"][:], S["atot"][:], S["acs"][:], ALU.subtract), [S["atot"], S["acs"]], [S["dte"]])
    A(lambda e: e.activation(S["dte"][:], S["dte"][:], AF.Exp), [S["dte"]], [S["dte"]])
    D(lambda e: e.tensor_tensor(S["dte"][:], S["dte"][:], S["dt"][:], ALU.mult), [S["dte"], S["dt"]], [S["dte"]])
    D(lambda e: e.memset(ST[0][:], 0.0), [], [ST[0]])
    D(lambda e: e.memset(STb[0][:], 0.0), [], [STb[0]])

    def load_tile(n):
        i = n % NB
        r = slice(n * 128, (n + 1) * 128)
        P.dma("sp", xt[i][:], x_d[r, :], writes=[xt[i]], sb=xt[i])
        P.dma("act", zt[i][:], z_d[r, :], writes=[zt[i]], sb=zt[i])
        P.dma("pool", btt[i][:], bt_d[:, r], writes=[btt[i]], sb=btt[i])
        P.dma("pool", ctt[i][:], ct_d[:, r], writes=[ctt[i]], sb=ctt[i])
        P.dma("pool", bkt[i][:], bk_d[r, :], writes=[bkt[i]], sb=bkt[i])

    G = lambda fn, r, w: P.op("pool", fn, reads=r, writes=w)
    load_tile(0)
    load_tile(1)
    state = {"cur": 0}

    def front(n):
        i = n % NB
        X, BT, CT = xt[i], btt[i], ctt[i]
        sc = slice(n * 8, (n + 1) * 8)
        xd, xe, y = xdt[n % 2], xdd[n % 2], yt[n % 2]
        G(lambda e: e.tensor_tensor(v3(xd[:]), v3(X[:]), bc8(S["dt"][:, sc]), ALU.mult), [X, S["dt"]], [xd])
        G(lambda e: e.tensor_tensor(v3(xe[:]), v3(X[:]), bc8(S["dte"][:, sc]), ALU.mult), [X, S["dte"]], [xe])
        G(lambda e: e.tensor_tensor(v3(y[:]), v3(X[:]), bc8(rv[:, 16:24]), ALU.mult), [X, rv], [y])
        P.op("pe", lambda e: e.matmul(bank[0][:, 0:128], BT[:], CT[:], start=True, stop=True),
             reads=[BT, CT], writes=[bank[0]])
        cb = cbm[n % 2]
        D(lambda e: e.tensor_tensor(cb[:], bank[0][:, 0:128], TRI, ALU.mult), [bank[0], cm], [cb])
        for h in range(H):
            col = n * 8 + h
            rb = rbv[h]
            P.op("pe", lambda e: e.matmul(rb[:], S["da"][:, col:col + 1].broadcast_to([128, 128]), TRI,
                                          start=True, stop=True),
                 reads=[S["da"], cm], writes=[rb])

    def middle(n):
        i = n % NB
        BK = bkt[i]
        xd, xe, cb = xdt[n % 2], xdd[n % 2], cbm[n % 2]
        yb = bank[2 + n % 2]
        for c in range(2):
            lo, hi = c * 64, (c + 1) * 64
            P.op("pe", lambda e: e.matmul(csbk[c][:], BK[lo:hi, :], xe[lo:hi, :], start=True, stop=True),
                 reads=[BK, xe], writes=[csbk[c]])
        for h in range(H):
            col = n * 8 + h
            rb = rbv[h]
            x_ = xx[h]
            A(lambda e: e.activation(x_[:], rb[:], AF.Exp, bias=S["nacs"][:, col:col + 1], scale=1.0),
              [rb, S["nacs"]], [x_])
        for h in range(H):
            x_ = xx[h]
            m_ = mt[h]
            D(lambda e: e.scalar_tensor_tensor(m_[:], x_[:], 1.0, cb[:], ALU.min, ALU.mult), [x_, cb], [m_])
        for h in range(H):
            m_ = mt[h]
            P.op("pe", lambda e: e.matmul(yb[:, h * 64:(h + 1) * 64], m_[:], xd[:, h * 64:(h + 1) * 64],
                                          start=True, stop=True),
                 reads=[m_, xd], writes=[yb])

    def scan(n):
        i = n % NB
        Z, CT = zt[i], ctt[i]
        sc = slice(n * 8, (n + 1) * 8)
        y = yt[n % 2]
        yb = bank[2 + n % 2]
        D(lambda e: e.tensor_tensor(y[:], y[:], yb[:], ALU.add), [y, yb], [y])
        for c in range(2):
            lo, hi = c * 64, (c + 1) * 64
            s_old, s_new = ST[state["cur"]], ST[1 - state["cur"]]
            yo = bank[6]
            sb_old, sb_new = STb[state["cur"]], STb[1 - state["cur"]]
            P.op("pe", lambda e: e.matmul(yo[:], CT[:], sb_old[:], start=True, stop=True),
                 reads=[CT, sb_old], writes=[yo])
            csb = csbk[c]
            u = uo[c]
            dec = S["dec0"] if c == 0 else S["dec1"]
            D(lambda e: e.tensor_tensor(v3(s_new[:]), v3(s_old[:]), bc8(dec[:, sc]), ALU.mult), [s_old, dec], [s_new])
            D(lambda e: e.tensor_tensor(s_new[:], s_new[:], csb[:], ALU.add), [s_new, csb], [s_new])
            A(lambda e: e.activation(sb_new[:], s_new[:], AF.Copy), [s_new], [sb_new])
            D(lambda e: e.tensor_tensor(v3(u[lo:hi, :]), v3(yo[lo:hi, :]), bc8(S["eacs"][lo:hi, sc]), ALU.mult),
              [yo, S["eacs"]], [u])
            G(lambda e: e.tensor_tensor(y[lo:hi, :], y[lo:hi, :], u[lo:hi, :], ALU.add), [y, u], [y])
            state["cur"] = 1 - state["cur"]
        A(lambda e: e.activation(Z[:], Z[:], AF.Silu), [Z], [Z])
        D(lambda e: e.tensor_tensor(y[:], y[:], Z[:], ALU.mult), [y, Z], [y])
        P.dma("sp", out_d[n * 128:(n + 1) * 128, :], y[:], reads=[y], sb=y)

    front(0)
    for n in range(NTILE):
        if n + 2 < NTILE:
            load_tile(n + 2)
        middle(n)
        if n + 1 < NTILE:
            front(n + 1)
        scan(n)
    P.wait_all("sp", yt)
    P.close()
    return nc


def build_tail(layer):
    rms = layer == 1
    final = layer == 1
    C = 4096 if layer == 1 else 2048
    CC = C // 128
    NT = 1024
    nc = bass.Bass("TRN2", target_bir_lowering=False)
    P = Prog(nc)
    xT_d = P.dram("xT", [2048, NT], F32, "ExternalInput")
    cat_d = P.dram("catT", [C, NT], F32, "ExternalInput")
    vec_d = P.dram("vec", [128, 6 * 16], F32, "ExternalInput")
    gc_d = P.dram("gcat", [128, CC], F32, "ExternalInput")
    wout_d = P.dram("wout", [C, 2048], F32, "ExternalInput")
    wr_d = P.dram("wr", [2048, 36], F32, "ExternalInput")
    br_d = P.dram("br", [128, 36], F32, "ExternalInput")
    w1_d = P.dram("w1", [32, 2048, 512], F32, "ExternalInput")
    w3_d = P.dram("w3", [32, 2048, 512], F32, "ExternalInput")
    w2_d = P.dram("w2", [32, 512, 2048], F32, "ExternalInput")
    id_d = P.dram("ident", [128, 128], F32, "ExternalInput")
    out_d = P.dram("outT", [2048, NT], F32, "ExternalOutput")

    xT = [[P.sbuf([128, 512], F32, f"x{j}_{h}") for h in range(2)] for j in range(16)]
    hT = [P.sbuf([128, 512], BF16, f"h{i}") for i in range(32)]
    wsl = [P.sbuf([128, 8192], BF16, f"wsl{i}") for i in range(4)]
    aT = [[P.sbuf([128, 512], BF16, f"a{j}_{h}") for h in range(2)] for j in range(4)]
    G = [P.sbuf([128, 1024], F32, f"G{i}") for i in range(2)]
    tmp1 = [P.sbuf([128, 512], F32, f"t1_{i}") for i in range(2)]
    tmp2 = [P.sbuf([128, 512], F32, f"t2_{i}") for i in range(2)]
    stg = [P.sbuf([128, 512], F32, f"stg{i}") for i in range(2)]
    sqb = [P.sbuf([128, 512], F32, f"sq{i}") for i in range(2)]
    rstd = [P.sbuf([128, 512], F32, f"rstd{i}") for i in range(2)]
    ones = P.sbuf([128, 128], F32, "ones")
    ident = P.sbuf([128, 128], F32, "ident")
    wr = P.sbuf([128, 16, 36], F32, "wr")
    br = P.sbuf([128, 36], F32, "br")
    vec = P.sbuf([128, 96], F32, "vec")
    gc = P.sbuf([128, CC], F32, "gc")
    gs2 = P.sbuf([128, 16], F32, "gs2")
    gd = [P.sbuf([128, 32], F32, f"gd{i}") for i in range(8)]
    sm = {k: P.sbuf([128, 36], F32, "sm_" + k) for k in
          ("lg", "ge", "ohg", "pen", "em", "oh1", "em2", "oh2")}
    sc = {k: P.sbuf([128, 1], F32, "sc_" + k) for k in
          ("gmax", "ngmax", "gsum", "grp", "m1", "m2", "d", "ed", "den", "p1", "p2", "p1g", "p2g")}
    bank = [P.psum([128, 512], F32, f"bank{i}") for i in range(8)]

    def V(i):
        return vec[:, i * 16:(i + 1) * 16]
    GATE_MIX, SHIFT2, SCALE2, GATE_FFN, NORMG2, FING = range(6)

    P.dma("sp", vec[:], vec_d[:], writes=[vec], sb=vec)
    P.dma("sp", gc[:], gc_d[:], writes=[gc], sb=gc)
    P.dma("sp", ident[:], id_d[:], writes=[ident], sb=ident)
    P.dma("sp", wr[:], wr_d.t.rearrange("(j p) n -> p j n", p=128), writes=[wr], sb=wr)
    P.dma("sp", br[:], br_d[:], writes=[br], sb=br)
    P.op("dve", lambda e: e.memset(ones[:], 1.0), writes=[ones])
    for j in range(16):
        for h in range(2):
            b = xT[j][h]
            P.dma("act" if (j + h) % 2 else "sp", b[:], xT_d[j * 128:(j + 1) * 128, h * 512:(h + 1) * 512],
                  writes=[b], sb=b)

    ring = {"n": 0}

    def wload(src_ap, kind):
        s = wsl[ring["n"] % 4]
        ring["n"] += 1
        if kind == "k16":
            dst = s[:].rearrange("p (j n) -> p j n", n=512)
            src = src_ap.rearrange("(j p) n -> p j n", p=128)
        else:
            dst = s[:].rearrange("p (j n) -> p j n", n=2048)
            src = src_ap.rearrange("(j p) n -> p j n", p=128)
        P.dma("pool", dst, src, writes=[s], sb=s)
        return s, dst

    def sum_sq_rstd(srcs, h, n_feat):
        n = len(srcs)
        for i, (sbuf_, ap) in enumerate(srcs):
            q = sqb[i % 2]
            P.op("act", lambda e: e.activation(q[:], ap, AF.Square), reads=[sbuf_], writes=[q])
            P.op("pe", lambda e: e.matmul(bank[6][:], ones[:], q[:], start=(i == 0), stop=(i == n - 1)),
                 reads=[ones, q], writes=[bank[6]])
        t = tmp1[0]
        P.op("act", lambda e: e.activation(t[:], bank[6][:], AF.Sqrt, bias=EPS, scale=1.0 / n_feat),
             reads=[bank[6]], writes=[t])
        P.op("dve", lambda e: e.reciprocal(rstd[h][:], t[:]), reads=[t], writes=[rstd[h]])

    nmm = 0
    for h in range(2):
        cbufs = []
        for cc in range(CC):
            s = stg[cc % 2]
            P.dma("sp" if cc % 2 else "act", s[:], cat_d[cc * 128:(cc + 1) * 128, h * 512:(h + 1) * 512],
                  writes=[s], sb=s)
            if rms:
                q = sqb[cc % 2]
                P.op("act", lambda e: e.activation(q[:], s[:], AF.Square), reads=[s], writes=[q])
                P.op("pe", lambda e: e.matmul(bank[6][:], ones[:], q[:], start=(cc == 0), stop=(cc == CC - 1)),
                     reads=[ones, q], writes=[bank[6]])
            dst = hT[cc] if CC == 32 else hT[h * 16 + cc]
            P.op("dve", lambda e: e.tensor_scalar(dst[:], s[:], gc[:, cc:cc + 1], None, ALU.mult),
                 reads=[s, gc], writes=[dst])
            cbufs.append(dst)
        if rms:
            t = tmp1[0]
            P.op("act", lambda e: e.activation(t[:], bank[6][:], AF.Sqrt, bias=EPS, scale=1.0 / C),
                 reads=[bank[6]], writes=[t])
            P.op("dve", lambda e: e.reciprocal(rstd[h][:], t[:]), reads=[t], writes=[rstd[h]])
        for cb in range(4):
            slots = []
            for rb in range(C // 2048):
                slots.append(wload(wout_d[rb * 2048:(rb + 1) * 2048, cb * 512:(cb + 1) * 512], "k16"))
            for fc in range(4):
                j = cb * 4 + fc
                bk = bank[nmm % 2]
                nmm += 1
                for cc in range(CC):
                    sb_, view = slots[cc // 16]
                    P.op("pe", lambda e: e.matmul(bk[:], view[:, cc % 16, fc * 128:(fc + 1) * 128], cbufs[cc][:],
                                                  start=(cc == 0), stop=(cc == CC - 1)),
                         reads=[sb_, cbufs[cc]], writes=[bk], defer=(cc != CC - 1))
                xb = xT[j][h]
                if rms:
                    t = tmp2[j % 2]
                    P.op("dve", lambda e: e.tensor_tensor(t[:], bk[:], rstd[h][:], ALU.mult),
                         reads=[bk, rstd[h]], writes=[t])
                    P.op("dve", lambda e: e.scalar_tensor_tensor(xb[:], t[:], V(GATE_MIX)[:, j:j + 1], xb[:],
                                                                 ALU.mult, ALU.add),
                         reads=[t, vec, xb], writes=[xb])
                else:
                    P.op("dve", lambda e: e.scalar_tensor_tensor(xb[:], bk[:], V(GATE_MIX)[:, j:j + 1], xb[:],
                                                                 ALU.mult, ALU.add),
                         reads=[bk, vec, xb], writes=[xb])

    P.op("dve", lambda e: e.tensor_scalar(gs2[:], V(SCALE2), 1.0, None, ALU.add), reads=[vec], writes=[gs2])
    P.op("dve", lambda e: e.tensor_tensor(gs2[:], gs2[:], V(NORMG2), ALU.mult), reads=[gs2, vec], writes=[gs2])
    for h in range(2):
        sum_sq_rstd([(xT[j][h], xT[j][h][:]) for j in range(16)], h, 2048)
        for j in range(16):
            hf = stg[j % 2]
            P.op("dve", lambda e: e.tensor_tensor(hf[:], xT[j][h][:], rstd[h][:], ALU.mult),
                 reads=[xT[j][h], rstd[h]], writes=[hf])
            P.op("dve", lambda e: e.tensor_scalar(hf[:], hf[:], gs2[:, j:j + 1], V(SHIFT2)[:, j:j + 1],
                                                  ALU.mult, ALU.add),
                 reads=[hf, gs2, vec], writes=[hf])
            hb = hT[j * 2 + h]
            P.op("act", lambda e: e.activation(hb[:], hf[:], AF.Copy), reads=[hf], writes=[hb])
            for tt in range(4):
                bk = bank[2 + tt]
                P.op("pe", lambda e: e.matmul(bk[:, 0:36], hf[:, tt * 128:(tt + 1) * 128], wr[:, j, :],
                                              start=(j == 0), stop=(j == 15)),
                     reads=[hf, wr], writes=[bk], defer=not (j == 15 or tt == 3))
        for tt in range(4):
            bk = bank[2 + tt]
            g = gd[h * 4 + tt]
            lg, ge, ohg, pen, em, oh1, em2, oh2 = (sm[k] for k in ("lg", "ge", "ohg", "pen", "em", "oh1", "em2", "oh2"))
            D = lambda fn, r, w: P.op("dve", fn, reads=r, writes=w)
            A = lambda fn, r, w: P.op("act", fn, reads=r, writes=w)
            D(lambda e: e.tensor_tensor(lg[:], bk[:, 0:36], br[:], ALU.add), [bk, br], [lg])
            D(lambda e: e.tensor_reduce(sc["gmax"][:], lg[:, 0:4], AX.X, ALU.max), [lg], [sc["gmax"]])
            D(lambda e: e.tensor_scalar(sc["ngmax"][:], sc["gmax"][:], -1.0, None, ALU.mult), [sc["gmax"]], [sc["ngmax"]])
            A(lambda e: e.activation(ge[:, 0:4], lg[:, 0:4], AF.Exp, bias=sc["ngmax"][:, 0:1], scale=1.0),
              [lg, sc["ngmax"]], [ge])
            D(lambda e: e.tensor_reduce(sc["gsum"][:], ge[:, 0:4], AX.X, ALU.add), [ge], [sc["gsum"]])
            D(lambda e: e.reciprocal(sc["grp"][:], sc["gsum"][:]), [sc["gsum"]], [sc["grp"]])
            D(lambda e: e.tensor_scalar(ohg[:, 0:4], lg[:, 0:4], sc["gmax"][:, 0:1], None, ALU.is_equal),
              [lg, sc["gmax"]], [ohg])
            D(lambda e: e.tensor_scalar(pen[:, 0:4], ohg[:, 0:4], 1e30, -1e30, ALU.mult, ALU.add), [ohg], [pen])
            D(lambda e: e.tensor_tensor(em[:, 0:32].rearrange("p (g k) -> p g k", k=8),
                                        lg[:, 4:36].rearrange("p (g k) -> p g k", k=8),
                                        pen[:, 0:4].unsqueeze(2).broadcast_to([128, 4, 8]), ALU.add),
              [lg, pen], [em])
            D(lambda e: e.tensor_reduce(sc["m1"][:], em[:, 0:32], AX.X, ALU.max), [em], [sc["m1"]])
            D(lambda e: e.tensor_scalar(oh1[:, 0:32], em[:, 0:32], sc["m1"][:, 0:1], None, ALU.is_equal),
              [em, sc["m1"]], [oh1])
            D(lambda e: e.scalar_tensor_tensor(em2[:, 0:32], oh1[:, 0:32], -1e30, em[:, 0:32], ALU.mult, ALU.add),
              [oh1, em], [em2])
            D(lambda e: e.tensor_reduce(sc["m2"][:], em2[:, 0:32], AX.X, ALU.max), [em2], [sc["m2"]])
            D(lambda e: e.tensor_scalar(oh2[:, 0:32], em2[:, 0:32], sc["m2"][:, 0:1], None, ALU.is_equal),
              [em2, sc["m2"]], [oh2])
            D(lambda e: e.tensor_tensor(sc["d"][:], sc["m2"][:], sc["m1"][:], ALU.subtract),
              [sc["m2"], sc["m1"]], [sc["d"]])
            A(lambda e: e.activation(sc["ed"][:], sc["d"][:], AF.Exp), [sc["d"]], [sc["ed"]])
            D(lambda e: e.tensor_scalar(sc["den"][:], sc["ed"][:], 1.0, None, ALU.add), [sc["ed"]], [sc["den"]])
            D(lambda e: e.reciprocal(sc["p1"][:], sc["den"][:]), [sc["den"]], [sc["p1"]])
            D(lambda e: e.tensor_tensor(sc["p2"][:], sc["ed"][:], sc["p1"][:], ALU.mult),
              [sc["ed"], sc["p1"]], [sc["p2"]])
            D(lambda e: e.tensor_tensor(sc["p1g"][:], sc["p1"][:], sc["grp"][:], ALU.mult),
              [sc["p1"], sc["grp"]], [sc["p1g"]])
            D(lambda e: e.tensor_tensor(sc["p2g"][:], sc["p2"][:], sc["grp"][:], ALU.mult),
              [sc["p2"], sc["grp"]], [sc["p2g"]])
            D(lambda e: e.tensor_scalar(g[:], oh1[:, 0:32], sc["p1g"][:, 0:1], None, ALU.mult),
              [oh1, sc["p1g"]], [g])
            D(lambda e: e.scalar_tensor_tensor(g[:], oh2[:, 0:32], sc["p2g"][:, 0:1], g[:], ALU.mult, ALU.add),
              [oh2, sc["p2g"], g], [g])

    def emit_G(e_):
        for tt in range(8):
            bk = bank[6 + tt // 4]
            P.op("pe", lambda e: e.matmul(bk[:, (tt % 4) * 128:(tt % 4 + 1) * 128],
                                          gd[tt][:, e_:e_ + 1].broadcast_to([128, 128]), ident[:],
                                          start=True, stop=True),
                 reads=[gd[tt], ident], writes=[bk], defer=(tt % 4 != 3))
        gb = G[e_ % 2]
        for hh in range(2):
            P.op("act", lambda e: e.activation(gb[:, hh * 512:(hh + 1) * 512], bank[6 + hh][:], AF.Copy),
                 reads=[bank[6 + hh]], writes=[gb])

    NE = 32
    pend = []
    for m in range(2):
        pass
    loads = {}

    def issue_loads(e_):
        loads[e_] = (wload(w1_d[e_], "k16"), wload(w3_d[e_], "k16"), wload(w2_d[e_], "k4"))

    issue_loads(0)
    emit_G(0)
    k = 0
    for e_ in range(NE):
        (s1, v1), (s3, v3), (s2, v2) = loads[e_]
        gb = G[e_ % 2]
        for h in range(2):
            for jc in range(4):
                b1 = bank[k % 2]
                b3 = bank[2 + k % 2]
                for j in range(16):
                    P.op("pe", lambda e: e.matmul(b1[:], v1[:, j, jc * 128:(jc + 1) * 128], hT[j * 2 + h][:],
                                                  start=(j == 0), stop=(j == 15)),
                         reads=[s1, hT[j * 2 + h]], writes=[b1], defer=(j != 15))
                for j in range(16):
                    P.op("pe", lambda e: e.matmul(b3[:], v3[:, j, jc * 128:(jc + 1) * 128], hT[j * 2 + h][:],
                                                  start=(j == 0), stop=(j == 15)),
                         reads=[s3, hT[j * 2 + h]], writes=[b3], defer=(j != 15))
                t1 = tmp1[k % 2]
                t2 = tmp2[k % 2]
                P.op("act", lambda e: e.activation(t1[:], b1[:], AF.Silu), reads=[b1], writes=[t1])
                P.op("dve", lambda e: e.tensor_tensor(t2[:], t1[:], b3[:], ALU.mult), reads=[t1, b3], writes=[t2])
                P.op("dve", lambda e: e.tensor_tensor(aT[jc][h][:], t2[:], gb[:, h * 512:(h + 1) * 512], ALU.mult),
                     reads=[t2, gb], writes=[aT[jc][h]])
                k += 1
            if h == 0 and e_ + 1 < NE:
                pass
        if e_ + 1 < NE:
            emit_G(e_ + 1)
        for h in range(2):
            for fc in range(16):
                by = bank[4 + fc % 2]
                for jc in range(4):
                    P.op("pe", lambda e: e.matmul(by[:], v2[:, jc, fc * 128:(fc + 1) * 128], aT[jc][h][:],
                                                  start=(jc == 0), stop=(jc == 3)),
                         reads=[s2, aT[jc][h]], writes=[by], defer=(jc != 3))
                xb = xT[fc][h]
                P.op("dve", lambda e: e.scalar_tensor_tensor(xb[:], by[:], V(GATE_FFN)[:, fc:fc + 1], xb[:],
                                                             ALU.mult, ALU.add),
                     reads=[by, vec, xb], writes=[xb])
        if e_ + 1 < NE:
            issue_loads(e_ + 1)

    outs = []
    if final:
        for h in range(2):
            sum_sq_rstd([(xT[j][h], xT[j][h][:]) for j in range(16)], h, 2048)
            for j in range(16):
                xb = xT[j][h]
                P.op("dve", lambda e: e.tensor_tensor(xb[:], xb[:], rstd[h][:], ALU.mult),
                     reads=[xb, rstd[h]], writes=[xb])
                P.op("dve", lambda e: e.tensor_scalar(xb[:], xb[:], V(FING)[:, j:j + 1], None, ALU.mult),
                     reads=[xb, vec], writes=[xb])
    for j in range(16):
        for h in range(2):
            xb = xT[j][h]
            P.dma("sp" if (j + h) % 2 else "act", out_d[j * 128:(j + 1) * 128, h * 512:(h + 1) * 512], xb[:],
                  reads=[xb], sb=xb)
            outs.append(xb)
    P.wait_all("sp", outs)
    P.wait_all("act", outs)
    P.close()
    return nc

from concourse.bass_utils import run_bass_kernel_spmd

_CORES = list(range(8))


def _run(nc, maps):
    return run_bass_kernel_spmd(nc, maps, core_ids=_CORES).results


def tail_inputs(layer, xfull, cat, mod, inp):
    m_mix = mod[2 * layer]
    m_ffn = mod[2 * layer + 1]
    vec = np.concatenate([fm(m_mix[4096:]), fm(m_ffn[:2048]), fm(m_ffn[2048:4096]), fm(m_ffn[4096:]),
                          fm(inp["norm_g"][layer, 1]), fm(inp["final_norm_g"])], axis=1)
    vec = np.ascontiguousarray(vec.astype(np.float32))
    if layer == 0:
        gcat = np.ones(2048, np.float32)
        wout = inp["e_w_out"][0]
    else:
        gcat = inp["o_norm_g"][0]
        wout = inp["o_w_out"][0]
    wr = np.ascontiguousarray(np.concatenate([inp["moe_w_group"][layer], inp["moe_w_expert"][layer]], axis=1))
    br = np.concatenate([inp["moe_b_group"][layer], inp["moe_b_expert"][layer]])[None, :]
    br = np.ascontiguousarray(np.tile(br, (128, 1)).astype(np.float32))
    ident = np.eye(128, dtype=np.float32)
    w1 = np.ascontiguousarray(inp["moe_w1"][layer])
    w3 = np.ascontiguousarray(inp["moe_w3"][layer])
    w2 = np.ascontiguousarray(inp["moe_w2"][layer])
    maps = []
    for i in range(8):
        sl = slice(i * 1024, (i + 1) * 1024)
        maps.append({"xT": np.ascontiguousarray(xfull[sl].T), "catT": np.ascontiguousarray(cat[sl].T), "vec": vec,
                     "gcat": fm(gcat), "wout": np.ascontiguousarray(wout), "wr": wr, "br": br,
                     "w1": w1, "w3": w3, "w2": w2, "ident": ident})
    return maps


def kernel(**inputs):
    inp = {k: np.asarray(v, dtype=np.float32) for k, v in inputs.items()}
    x0 = inp["x"][0]
    c = inp["c"][0]
    cT = np.ascontiguousarray(c.reshape(16, 128).T)
    maps = [{"cT": cT, "wmod": np.ascontiguousarray(inp["w_mod"][:, :, i * 768:(i + 1) * 768]),
             "bmod": np.ascontiguousarray(inp["b_mod"][:, i * 768:(i + 1) * 768])} for i in range(8)]
    res = _run(build_mod(), maps)
    mod = np.concatenate([res[i]["mod"] for i in range(8)], axis=1)
    res = _run(build_pre(0), pre_inputs(0, x0, mod, inp))
    pout0 = np.concatenate([res[i]["pout"] for i in range(8)], axis=1)
    res = _run(build_gdn(), gdn_inputs(pout0, inp))
    a_out = np.concatenate([res[h]["o_tok"] for h in range(8)], axis=1)
    cat0 = np.concatenate([a_out, pout0[3072:4096].T], axis=1)
    res = _run(build_tail(0), tail_inputs(0, x0, cat0, mod, inp))
    x1 = np.concatenate([res[i]["outT"].T for i in range(8)], axis=0)
    res = _run(build_pre(1), pre_inputs(1, x1, mod, inp))
    pout1 = np.concatenate([res[i]["pout"] for i in range(8)], axis=1)
    res = _run(build_ssd(), ssd_inputs(pout1, inp))
    yz = np.concatenate([res[g]["yz_tok"] for g in range(8)], axis=1)
    res = _run(build_tail(1), tail_inputs(1, x1, yz, mod, inp))
    out = np.concatenate([res[i]["outT"].T for i in range(8)], axis=0)
    return np.ascontiguousarray(out[None].astype(np.float32))
```

```python
import numpy as np

from contextlib import ExitStack
import concourse.bass as bass
import concourse.mybir as mybir

F32 = mybir.dt.float32
BF16 = mybir.dt.bfloat16
I32 = mybir.dt.int32
AF = mybir.ActivationFunctionType
ALU = mybir.AluOpType
AX = mybir.AxisListType


class Buf:
    __slots__ = ("t", "name", "w", "r", "dsem")

    def __init__(self, t, name):
        self.t = t
        self.name = name
        self.w = None
        self.r = {}
        self.dsem = None

    def __getitem__(self, idx):
        return self.t[idx]


class View:
    __slots__ = ("buf", "ap")

    def __init__(self, buf, ap):
        self.buf = buf
        self.ap = ap

    def __getitem__(self, idx):
        return self.ap[idx]


class Prog:
    def __init__(self, nc):
        self.nc = nc
        self.st = ExitStack()
        self.eng = {"pe": nc.tensor, "act": nc.scalar, "dve": nc.vector,
                    "pool": nc.gpsimd, "sp": nc.sync}
        self.sems = {}
        self.cnt = {}
        self.waited = {e: {} for e in self.eng}
        for e in ("pe", "act", "dve", "pool"):
            self.sems[e] = self.st.enter_context(nc.semaphore("s_" + e))
            self.cnt[e] = 0
        self.nbuf = 0
        self.pending = {}
        self.dma_sem_free = []
        self.ndsem = 0

    def sbuf(self, shape, dt, name=None):
        self.nbuf += 1
        name = name or f"sb{self.nbuf}"
        t = self.st.enter_context(self.nc.sbuf_tensor("S_" + name, list(shape), dt))
        return Buf(t, name)

    def psum(self, shape, dt, name=None):
        self.nbuf += 1
        name = name or f"ps{self.nbuf}"
        t = self.st.enter_context(self.nc.psum_tensor("P_" + name, list(shape), dt))
        return Buf(t, name)

    def dram(self, name, shape, dt, kind):
        t = self.nc.dram_tensor(name, list(shape), dt, kind=kind)
        return Buf(t.ap(), name)

    def new_dma_sem(self):
        self.ndsem += 1
        k = f"d{self.ndsem}"
        self.sems[k] = self.st.enter_context(self.nc.semaphore("s_" + k))
        self.cnt[k] = 0
        return k

    def _wait(self, e, deps):
        eng = self.eng[e]
        need = {}
        for d in deps:
            if d is None:
                continue
            k, v = d
            if e == "pe" and k == "pe":
                continue
            if v > need.get(k, 0):
                need[k] = v
        for k, v in need.items():
            if self.waited[e].get(k, 0) < v:
                eng.wait_ge(self.sems[k], v)
                self.waited[e][k] = v

    def _deps(self, reads, writes):
        reads = [getattr(b, "buf", b) for b in reads]
        writes = [getattr(b, "buf", b) for b in writes]
        deps = []
        for b in reads:
            deps.append(b.w)
        for b in writes:
            deps.append(b.w)
            deps.extend(b.r.items())
        return deps

    def _mark(self, key, val, reads, writes):
        reads = [getattr(b, "buf", b) for b in reads]
        writes = [getattr(b, "buf", b) for b in writes]
        for b in reads:
            if b.r.get(key, 0) < val:
                b.r[key] = val
        for b in writes:
            b.w = (key, val)
            b.r = {}

    def op(self, e, fn, reads=(), writes=(), defer=False):
        self._wait(e, self._deps(reads, writes))
        inst = fn(self.eng[e])
        if defer:
            pr, pw = self.pending.setdefault(e, ([], []))
            pr.extend(reads)
            pw.extend(writes)
            self._mark(e, self.cnt[e] + 1, reads, writes)
            return inst
        self.cnt[e] += 1
        inst.then_inc(self.sems[e], 1)
        if e in self.pending:
            self.pending.pop(e)
        self._mark(e, self.cnt[e], reads, writes)
        return inst

    def dma(self, q, out, in_, reads=(), writes=(), sb=None, **kw):
        if sb.dsem is None:
            sb.dsem = self.new_dma_sem()
        sem = sb.dsem
        self._wait(q, self._deps(reads, writes))
        inst = self.eng[q].dma_start(out=out, in_=in_, **kw)
        self.cnt[sem] += 16
        inst.then_inc(self.sems[sem], 16)
        self._mark(sem, self.cnt[sem], reads, writes)
        return inst

    def wait_all(self, e, bufs):
        self._wait(e, self._deps(bufs, bufs))

    def close(self):
        self.st.close()

EPS = 1e-6


def fm(v):
    return np.ascontiguousarray(np.asarray(v, np.float32).reshape(-1, 128).T)

def fmk(w):
    K, n = w.shape[0], w.shape[1] // 128
    return np.ascontiguousarray(np.asarray(w, np.float32).reshape(K, n, 128).transpose(2, 0, 1).reshape(128, K * n))

def pre_inputs(layer, xfull, mod, inp):
    m = mod[2 * layer]
    vec = np.concatenate([fm(m[:2048]), fm(m[2048:4096]), fm(inp["norm_g"][layer, 0])], axis=1)
    if layer == 0:
        w = inp["e_w_in"][0]
        glu_a = w[:, 4112:5136].reshape(2048, 8, 128); glu_b = w[:, 5136:6160].reshape(2048, 8, 128)
        glu = np.stack([glu_a, glu_b], axis=2).reshape(2048, 2048)
        win = np.concatenate([w[:, 0:3072], glu, w[:, 3072:4096], w[:, 4096:4112]], axis=1)
        convw = fmk(inp["e_conv_qkv"][0]); convb = np.zeros((128, 24), np.float32)
        extra = {"dw": fmk(inp["e_conf_dw"][0]),
                 "dvec": np.concatenate([fm(inp["e_conf_dw_b"][0]), fm(inp["e_conf_ln_g"][0]), fm(inp["e_conf_ln_b"][0])], axis=1)}
    else:
        w = inp["o_w_in"][0]
        win = np.concatenate([w[:, 4096:10240], w[:, 0:4096], w[:, 10240:10304]], axis=1)
        convw = fmk(inp["o_conv_w"][0]); convb = fm(inp["o_conv_b"][0])
        extra = {}
    win = np.ascontiguousarray(win)
    xT = np.ascontiguousarray(xfull.T)
    maps = []
    for i in range(8):
        xs = np.zeros((2048, 1056), np.float32)
        lo = i * 1024 - 32
        if i == 0:
            xs[:, 32:] = xT[:, 0:1024]
        else:
            xs[:] = xT[:, lo:lo + 1056]
        hmask = np.full((128, 1), 0.0 if i == 0 else 1.0, np.float32)
        maps.append({"xT": xs, "vec": vec, "win": win, "convw": convw, "convb": convb, "hmask": hmask, **extra})
    return maps

def cmat():
    j = np.arange(128)[:, None]; i = np.arange(128)[None, :]
    same = (j // 64) == (i // 64)
    tri = ((j <= i) & same).astype(np.float32)
    blk = same.astype(np.float32)
    sel0 = np.broadcast_to(j < 64, (128, 128)).astype(np.float32)
    sel1 = np.broadcast_to(j >= 64, (128, 128)).astype(np.float32)
    return np.ascontiguousarray(np.concatenate([tri, blk, sel0, sel1], axis=1))

def ssd_inputs(pout, inp):
    maps = []
    cm = cmat()
    for g in range(8):
        xT = pout[g * 512:(g + 1) * 512]; zT = pout[6144 + g * 512:6144 + (g + 1) * 512]
        BT = np.ascontiguousarray(pout[4096 + g * 128:4096 + (g + 1) * 128]); CT = np.ascontiguousarray(pout[5120 + g * 128:5120 + (g + 1) * 128])
        dtr = pout[10240 + g * 8:10240 + (g + 1) * 8]
        dtr = np.ascontiguousarray(dtr.reshape(8, 64, 128).transpose(2, 1, 0).reshape(128, 512))
        hs = slice(g * 8, (g + 1) * 8)
        rv = np.concatenate([inp["o_dt_bias"][0][hs], inp["o_a_log"][0][hs], inp["o_d_skip"][0][hs]])[None, :]
        rv = np.ascontiguousarray(np.tile(rv, (128, 1)).astype(np.float32))
        maps.append({"x_tok": np.ascontiguousarray(xT.T), "z_tok": np.ascontiguousarray(zT.T), "BT": BT, "CT": CT,
                     "B_tok": np.ascontiguousarray(BT.T), "dtr": dtr, "rowvec": rv, "cmat": cm})
    return maps

def cmat_gdn():
    c = cmat()
    j = np.arange(128)[:, None]; i = np.arange(128)[None, :]
    masks = ((j > i) & ((j // 64) == (i // 64))).astype(np.float32)
    return np.ascontiguousarray(np.concatenate([c, np.eye(128, dtype=np.float32), masks], axis=1))

def gdn_inputs(pout, inp):
    cm = cmat_gdn()
    gn = np.ascontiguousarray(np.tile(inp["e_head_norm_g"][0][None, :], (128, 1)).astype(np.float32))
    maps = []
    for hd in range(8):
        r = slice(hd * 128, (hd + 1) * 128)
        qT = np.ascontiguousarray(pout[0:1024][r]); kT = np.ascontiguousarray(pout[1024:2048][r])
        vT = pout[2048:3072][r]; zT = pout[4096:5120][r]
        bl = np.ascontiguousarray(pout[5120 + hd].reshape(64, 128).T); al = np.ascontiguousarray(pout[5128 + hd].reshape(64, 128).T)
        rv = np.tile(np.array([[inp["e_a_log"][0][hd], inp["e_dt_bias"][0][hd]]], np.float32), (128, 1))
        maps.append({"qT": qT, "kT": kT, "k_tok": np.ascontiguousarray(kT.T), "v_tok": np.ascontiguousarray(vT.T),
                     "z_tok": np.ascontiguousarray(zT.T), "bl": bl, "al": al, "rowvec": np.ascontiguousarray(rv),
                     "gn": gn, "cmat": cm})
    return maps


def build_mod():
    nc = bass.Bass("TRN2", target_bir_lowering=False)
    P = Prog(nc)
    NCOL = 768
    c_d = P.dram("cT", [128, 16], F32, "ExternalInput")
    w_d = P.dram("wmod", [4, 2048, NCOL], F32, "ExternalInput")
    b_d = P.dram("bmod", [4, NCOL], F32, "ExternalInput")
    o_d = P.dram("mod", [4, NCOL], F32, "ExternalOutput")
    cs = P.sbuf([128, 16], F32, "cs")
    sc = P.sbuf([128, 16], F32, "sc")
    wb = [P.sbuf([128, 16, NCOL], F32, f"w{i}") for i in range(2)]
    bs = P.sbuf([1, 4 * NCOL], F32, "bs")
    os_ = P.sbuf([1, 4 * NCOL], F32, "os")
    bank = [P.psum([128, 512], F32, f"bank{i}") for i in range(2)]
    P.dma("sp", cs[:], c_d[:], writes=[cs], sb=cs)
    P.dma("sp", bs[:], b_d.t.rearrange("(o l) n -> o (l n)", o=1), writes=[bs], sb=bs)
    P.op("act", lambda e: e.activation(sc[:], cs[:], AF.Silu), reads=[cs], writes=[sc])
    k = 0
    for l in range(4):
        w = wb[l % 2]
        P.dma("sp" if l % 2 else "act", w[:], w_d[l].rearrange("(j p) n -> p j n", p=128), writes=[w], sb=w)
        for nh in range(2):
            bk = bank[k % 2]
            k += 1
            for j in range(16):
                P.op("pe", lambda e: e.matmul(bk[0:1, 0:384], sc[:, j:j + 1], w[:, j, nh * 384:(nh + 1) * 384],
                                              start=(j == 0), stop=(j == 15)),
                     reads=[sc, w], writes=[bk], defer=(j != 15))
            o = l * NCOL + nh * 384
            P.op("dve", lambda e: e.tensor_tensor(os_[0:1, o:o + 384], bk[0:1, 0:384], bs[0:1, o:o + 384], ALU.add),
                 reads=[bk, bs], writes=[os_])
    P.dma("sp", o_d.t.rearrange("(o l) n -> o (l n)", o=1), os_[:], reads=[os_], sb=os_)
    P.wait_all("sp", [os_])
    P.close()
    return nc


def build_pre(layer):
    NTK, HALO, NT = 1056, 32, 1024
    pieces = [(0, 32), (32, 544), (544, 1056)]
    if layer == 0:
        plan = [("conv", i) for i in range(24)] + [("glu", i) for i in range(16)] + \
               [("pass", i) for i in range(8)] + [("passp", 0)]
        NW, NOUT, NCONV, PW = 6160, 5136, 24, 16
        out_row = {"conv": 0, "glu": 3072, "pass": 4096, "passp": 5120}
    else:
        plan = [("conv", i) for i in range(48)] + [("pass", i) for i in range(32)] + [("passp", 0)]
        NW, NOUT, NCONV, PW = 10304, 10304, 48, 64
        out_row = {"conv": 0, "pass": 6144, "passp": 10240}
    nc = bass.Bass("TRN2", target_bir_lowering=False)
    P = Prog(nc)
    xT_d = P.dram("xT", [2048, NTK], F32, "ExternalInput")
    vec_d = P.dram("vec", [128, 48], F32, "ExternalInput")
    w_d = P.dram("win", [2048, NW], F32, "ExternalInput")
    cw_d = P.dram("convw", [128, 4 * NCONV], F32, "ExternalInput")
    cb_d = P.dram("convb", [128, NCONV], F32, "ExternalInput")
    hm_d = P.dram("hmask", [128, 1], F32, "ExternalInput")
    if layer == 0:
        dw_d = P.dram("dw", [128, 31 * 8], F32, "ExternalInput")
        dv_d = P.dram("dvec", [128, 24], F32, "ExternalInput")
    out_d = P.dram("pout", [NOUT, NT], F32, "ExternalOutput")

    big = [P.sbuf([128, NTK], F32, f"big{i}") for i in range(16)]
    hT = [P.sbuf([128, NTK], BF16, f"h{j}") for j in range(16)]
    wsl = [P.sbuf([128, 16, 512], BF16, f"wsl{i}") for i in range(3)]
    rstd = P.sbuf([128, NTK], F32, "rstd")
    sqb = [P.sbuf([128, 512], BF16, f"sq{i}") for i in range(2)]
    tsm = [P.sbuf([128, 512], F32, f"tsm{i}") for i in range(2)]
    ones = P.sbuf([128, 128], F32, "ones")
    onesb = P.sbuf([128, 128], BF16, "onesb")
    vec = P.sbuf([128, 48], F32, "vec")
    gs = P.sbuf([128, 16], F32, "gs")
    cw = P.sbuf([128, 4 * NCONV], F32, "cw")
    cb = P.sbuf([128, NCONV], F32, "cb")
    hm = P.sbuf([128, 1], F32, "hm")
    bank = [P.psum([128, 512], F32, f"bank{i}") for i in range(8)]
    if layer == 0:
        dw = P.sbuf([128, 31 * 8], F32, "dw")
        dv = P.sbuf([128, 24], F32, "dv")
        P.dma("sp", dw[:], dw_d[:], writes=[dw], sb=dw)
        P.dma("sp", dv[:], dv_d[:], writes=[dv], sb=dv)
    P.dma("sp", vec[:], vec_d[:], writes=[vec], sb=vec)
    P.dma("sp", cw[:], cw_d[:], writes=[cw], sb=cw)
    P.dma("sp", cb[:], cb_d[:], writes=[cb], sb=cb)
    P.dma("sp", hm[:], hm_d[:], writes=[hm], sb=hm)
    P.op("dve", lambda e: e.memset(ones[:], 1.0), writes=[ones])
    P.op("dve", lambda e: e.memset(onesb[:], 1.0), writes=[onesb])
    for j in range(16):
        P.dma("act" if j % 2 else "sp", big[j][:], xT_d[j * 128:(j + 1) * 128, :], writes=[big[j]], sb=big[j])

    P.op("dve", lambda e: e.tensor_scalar(gs[:], vec[:, 16:32], 1.0, None, ALU.add), reads=[vec], writes=[gs])
    P.op("dve", lambda e: e.tensor_tensor(gs[:], gs[:], vec[:, 32:48], ALU.mult), reads=[gs, vec], writes=[gs])
    for (lo, hi) in pieces:
        n = hi - lo
        for j in range(16):
            q = sqb[j % 2]
            P.op("act", lambda e: e.activation(q[:, 0:n], big[j][:, lo:hi], AF.Square), reads=[big[j]], writes=[q])
            P.op("pe", lambda e: e.matmul(bank[6][:, 0:n], onesb[:], q[:, 0:n], start=(j == 0), stop=(j == 15)),
                 reads=[onesb, q], writes=[bank[6]])
        t = tsm[0]
        P.op("act", lambda e: e.activation(t[:, 0:n], bank[6][:, 0:n], AF.Sqrt, bias=EPS, scale=1.0 / 2048),
             reads=[bank[6]], writes=[t])
        P.op("dve", lambda e: e.reciprocal(rstd[:, lo:hi], t[:, 0:n]), reads=[t], writes=[rstd])
    for j in range(16):
        P.op("dve", lambda e: e.tensor_tensor(big[j][:], big[j][:], rstd[:], ALU.mult),
             reads=[big[j], rstd], writes=[big[j]])
        P.op("dve", lambda e: e.tensor_scalar(hT[j][:], big[j][:], gs[:, j:j + 1], vec[:, j:j + 1], ALU.mult, ALU.add),
             reads=[big[j], gs, vec], writes=[hT[j]])

    rot = {"n": 0}
    if layer == 0:
        cv = big[0:8]
        pa = big[8]
        pool = big[9:16]
    else:
        pool = big

    def nextbuf():
        b = pool[rot["n"] % len(pool)]
        rot["n"] += 1
        return b

    nb = {"n": 0}
    wv = None
    for ci, (kind, idx) in enumerate(plan):
        blk = ci // 4
        if ci % 4 == 0:
            width = min(512, NW - blk * 512)
            ws = wsl[blk % 3]
            P.dma("pool", ws[:, :, 0:width], w_d[:, blk * 512:blk * 512 + width].rearrange("(j p) n -> p j n", p=128),
                  writes=[ws], sb=ws)
        off = (ci % 4) * 128
        wc = PW if kind == "passp" else 128
        halo = kind in ("conv", "glu")
        pr = pa if (kind == "glu" and idx % 2 == 0) else nextbuf()
        for pi, (lo, hi) in enumerate(pieces):
            if pi == 0 and not halo:
                continue
            n = hi - lo
            bk = bank[nb["n"] % 4]
            nb["n"] += 1
            for j in range(16):
                P.op("pe", lambda e: e.matmul(bk[0:wc, 0:n], ws[:, j, off:off + wc], hT[j][:, lo:hi],
                                              start=(j == 0), stop=(j == 15)),
                     reads=[ws, hT[j]], writes=[bk], defer=(j != 15))
            if pi == 0:
                P.op("act", lambda e: e.activation(pr[0:wc, lo:hi], bk[0:wc, 0:n], AF.Copy, scale=hm[0:wc, 0:1]),
                     reads=[bk, hm], writes=[pr])
            else:
                P.op("act", lambda e: e.activation(pr[0:wc, lo:hi], bk[0:wc, 0:n], AF.Copy), reads=[bk], writes=[pr])
        if kind == "conv":
            acc = nextbuf()
            P.op("dve", lambda e: e.tensor_scalar(acc[:, 0:NT], pr[:, 29:29 + NT], cw[:, idx:idx + 1], None, ALU.mult),
                 reads=[pr, cw], writes=[acc])
            for k in range(1, 4):
                P.op("dve", lambda e: e.scalar_tensor_tensor(acc[:, 0:NT], pr[:, 29 + k:29 + k + NT],
                                                             cw[:, k * NCONV + idx:k * NCONV + idx + 1], acc[:, 0:NT],
                                                             ALU.mult, ALU.add),
                     reads=[pr, cw, acc], writes=[acc])
            P.op("act", lambda e: e.activation(acc[:, 0:NT], acc[:, 0:NT], AF.Silu, bias=cb[:, idx:idx + 1], scale=1.0),
                 reads=[acc, cb], writes=[acc])
            if layer == 0 and idx < 16:
                qs = 128.0 ** -0.5 if idx < 8 else 1.0
                for hh in range(2):
                    sl = slice(hh * 512, (hh + 1) * 512)
                    q = sqb[hh]
                    P.op("act", lambda e: e.activation(q[:], acc[:, sl], AF.Square), reads=[acc], writes=[q])
                    P.op("pe", lambda e: e.matmul(bank[6][:], onesb[:], q[:], start=True, stop=True),
                         reads=[onesb, q], writes=[bank[6]])
                    t = tsm[hh]
                    P.op("act", lambda e: e.activation(t[:], bank[6][:], AF.Sqrt, bias=EPS, scale=1.0),
                         reads=[bank[6]], writes=[t])
                    P.op("dve", lambda e: e.reciprocal(t[:], t[:]), reads=[t], writes=[t])
                    P.op("dve", lambda e: e.scalar_tensor_tensor(acc[:, sl], acc[:, sl], qs, t[:], ALU.mult, ALU.mult),
                         reads=[acc, t], writes=[acc])
            r0 = out_row["conv"] + idx * 128
            P.dma("sp" if ci % 2 else "act", out_d[r0:r0 + 128, :], acc[:, 0:NT], reads=[acc], sb=acc)
        elif kind == "glu":
            if idx % 2 == 0:
                continue
            c = idx // 2
            P.op("act", lambda e: e.activation(pr[:], pr[:], AF.Sigmoid), reads=[pr], writes=[pr])
            P.op("dve", lambda e: e.tensor_tensor(pr[:], pr[:], pa[:], ALU.mult), reads=[pr, pa], writes=[pr])
            acc = cv[c]
            P.op("dve", lambda e: e.tensor_scalar(acc[:, 0:NT], pr[:, 2:2 + NT], dw[:, c:c + 1], None, ALU.mult),
                 reads=[pr, dw], writes=[acc])
            for k in range(1, 31):
                P.op("dve", lambda e: e.scalar_tensor_tensor(acc[:, 0:NT], pr[:, 2 + k:2 + k + NT],
                                                             dw[:, k * 8 + c:k * 8 + c + 1], acc[:, 0:NT],
                                                             ALU.mult, ALU.add),
                     reads=[pr, dw, acc], writes=[acc])
            P.op("dve", lambda e: e.tensor_scalar(acc[:, 0:NT], acc[:, 0:NT], dv[:, c:c + 1], None, ALU.add),
                 reads=[acc, dv], writes=[acc])
            if c == 7:
                for hh in range(2):
                    sl = slice(hh * 512, (hh + 1) * 512)
                    for c2 in range(8):
                        P.op("pe", lambda e: e.matmul(bank[6][:], ones[:], cv[c2][:, sl], start=(c2 == 0), stop=(c2 == 7)),
                             reads=[ones, cv[c2]], writes=[bank[6]], defer=(c2 != 7))
                    mb = tsm[0]
                    P.op("act", lambda e: e.activation(mb[:], bank[6][:], AF.Copy, scale=1.0 / 1024),
                         reads=[bank[6]], writes=[mb])
                    for c2 in range(8):
                        P.op("dve", lambda e: e.tensor_tensor(cv[c2][:, sl], cv[c2][:, sl], mb[:], ALU.subtract),
                             reads=[cv[c2], mb], writes=[cv[c2]])
                        q = sqb[c2 % 2]
                        P.op("act", lambda e: e.activation(q[:], cv[c2][:, sl], AF.Square), reads=[cv[c2]], writes=[q])
                        P.op("pe", lambda e: e.matmul(bank[7][:], onesb[:], q[:], start=(c2 == 0), stop=(c2 == 7)),
                             reads=[onesb, q], writes=[bank[7]])
                    t = tsm[1]
                    P.op("act", lambda e: e.activation(t[:], bank[7][:], AF.Sqrt, bias=EPS, scale=1.0 / 1024),
                         reads=[bank[7]], writes=[t])
                    P.op("dve", lambda e: e.reciprocal(t[:], t[:]), reads=[t], writes=[t])
                    for c2 in range(8):
                        P.op("dve", lambda e: e.tensor_tensor(cv[c2][:, sl], cv[c2][:, sl], t[:], ALU.mult),
                             reads=[cv[c2], t], writes=[cv[c2]])
                        P.op("act", lambda e: e.activation(cv[c2][:, sl], cv[c2][:, sl], AF.Silu,
                                                           bias=dv[:, 16 + c2:17 + c2], scale=dv[:, 8 + c2:9 + c2]),
                             reads=[cv[c2], dv], writes=[cv[c2]])
                for c2 in range(8):
                    r0 = out_row["glu"] + c2 * 128
                    P.dma("sp" if c2 % 2 else "act", out_d[r0:r0 + 128, :], cv[c2][:, 0:NT], reads=[cv[c2]], sb=cv[c2])
        else:
            r0 = out_row[kind] + idx * 128
            P.dma("sp" if ci % 2 else "act", out_d[r0:r0 + wc, :], pr[0:wc, HALO:NTK], reads=[pr], sb=pr)
    allb = big
    P.wait_all("sp", allb)
    P.wait_all("act", allb)
    P.close()
    return nc


def build_gdn():
    T, NTILE, GT = 8192, 64, 4
    nc = bass.Bass("TRN2", target_bir_lowering=False)
    P = Prog(nc)
    qT_d = P.dram("qT", [128, T], F32, "ExternalInput")
    kT_d = P.dram("kT", [128, T], F32, "ExternalInput")
    kk_d = P.dram("k_tok", [T, 128], F32, "ExternalInput")
    vk_d = P.dram("v_tok", [T, 128], F32, "ExternalInput")
    zk_d = P.dram("z_tok", [T, 128], F32, "ExternalInput")
    bl_d = P.dram("bl", [128, 64], F32, "ExternalInput")
    al_d = P.dram("al", [128, 64], F32, "ExternalInput")
    rv_d = P.dram("rowvec", [128, 2], F32, "ExternalInput")
    gn_d = P.dram("gn", [128, 128], F32, "ExternalInput")
    cm_d = P.dram("cmat", [128, 768], F32, "ExternalInput")
    out_d = P.dram("o_tok", [T, 128], F32, "ExternalOutput")

    cm = P.sbuf([128, 768], F32, "cm")
    TRI, BLK, SEL0, SEL1, IDENT, MASKS = (cm[:, i * 128:(i + 1) * 128] for i in range(6))
    rv = P.sbuf([128, 2], F32, "rv")
    gn = P.sbuf([128, 128], F32, "gn")
    names = ("bl", "al", "beta", "xb", "ax", "ex", "ln", "sp", "g", "gcs", "gtot", "eg", "kdsc", "beg",
             "dec0", "dec1", "aneg")
    S = {k: P.sbuf([128, 64], F32, "s_" + k) for k in names}
    St = [P.sbuf([128, 128], F32, f"St{i}") for i in range(2)]
    qTg = [P.sbuf([128, 512], F32, f"qTg{i}") for i in range(2)]
    kTg = [P.sbuf([128, 512], F32, f"kTg{i}") for i in range(2)]
    kkg = [P.sbuf([128, GT, 128], F32, f"kkg{i}") for i in range(2)]
    vkg = [P.sbuf([128, GT, 128], F32, f"vkg{i}") for i in range(2)]
    zkg = [P.sbuf([128, GT, 128], F32, f"zkg{i}") for i in range(2)]
    og = [P.sbuf([128, GT, 128], F32, f"og{i}") for i in range(2)]
    W = {}
    for gi in range(GT):
        for k in ("Y", "YT", "decn", "dect", "erb", "t", "N", "M", "Na", "Nb", "Ma", "Mb", "QKm", "qeg", "kd",
                  "kcdT", "u", "o", "sz", "rbs"):
            W[k, gi] = P.sbuf([128, 128], F32, f"w_{k}{gi}")
        for k in ("R0", "R1"):
            W[k, gi] = P.sbuf([128, 256], F32, f"w_{k}{gi}")
        for k in ("ss", "rs"):
            W[k, gi] = P.sbuf([128, 1], F32, f"w_{k}{gi}")
    bkA = [P.psum([128, 512], F32, f"bkA{i}") for i in range(GT)]
    bkB = [P.psum([128, 512], F32, f"bkB{i}") for i in range(GT)]
    def vw(b, lo, n):
        return View(b, b[:, lo:lo + n])
    A_q = [[vw(bkA[gi], q * 128, 128) for q in range(4)] for gi in range(GT)]
    A_h = [[vw(bkA[gi], h * 256, 256) for h in range(2)] for gi in range(GT)]
    B_q = [[vw(bkB[gi], q * 128, 128) for q in range(4)] for gi in range(GT)]
    pq = [B_q[0][0]]

    P.dma("sp", cm[:], cm_d[:], writes=[cm], sb=cm)
    P.dma("sp", rv[:], rv_d[:], writes=[rv], sb=rv)
    P.dma("sp", gn[:], gn_d[:], writes=[gn], sb=gn)
    P.dma("sp", S["bl"][:], bl_d[:], writes=[S["bl"]], sb=S["bl"])
    P.dma("sp", S["al"][:], al_d[:], writes=[S["al"]], sb=S["al"])

    D = lambda fn, r, w: P.op("dve", fn, reads=r, writes=w)
    A = lambda fn, r, w: P.op("act", fn, reads=r, writes=w)
    G = lambda fn, r, w: P.op("dve", fn, reads=r, writes=w)
    PE = lambda fn, r, w: P.op("pe", fn, reads=r, writes=w)

    A(lambda e: e.activation(S["beta"][:], S["bl"][:], AF.Sigmoid), [S["bl"]], [S["beta"]])
    D(lambda e: e.tensor_scalar(S["xb"][:], S["al"][:], rv[:, 1:2], None, ALU.add), [S["al"], rv], [S["xb"]])
    A(lambda e: e.activation(S["ax"][:], S["xb"][:], AF.Abs), [S["xb"]], [S["ax"]])
    A(lambda e: e.activation(S["ex"][:], S["ax"][:], AF.Exp, scale=-1.0), [S["ax"]], [S["ex"]])
    A(lambda e: e.activation(S["ln"][:], S["ex"][:], AF.Ln, bias=1.0, scale=1.0), [S["ex"]], [S["ln"]])
    D(lambda e: e.scalar_tensor_tensor(S["sp"][:], S["xb"][:], 0.0, S["ln"][:], ALU.max, ALU.add),
      [S["xb"], S["ln"]], [S["sp"]])
    A(lambda e: e.activation(S["aneg"][:, 0:1], rv[:, 0:1], AF.Exp), [rv], [S["aneg"]])
    D(lambda e: e.tensor_scalar(S["g"][:], S["sp"][:], S["aneg"][:, 0:1], -1.0, ALU.mult, ALU.mult),
      [S["sp"], S["aneg"]], [S["g"]])
    for (dst, mat, fn) in (("gcs", TRI, AF.Copy), ("gtot", BLK, AF.Copy), ("dec0", SEL0, AF.Exp), ("dec1", SEL1, AF.Exp)):
        PE(lambda e: e.matmul(pq[0][:, 0:64], mat, S["g"][:], start=True, stop=True), [cm, S["g"]], [pq[0]])
        A(lambda e: e.activation(S[dst][:], pq[0][:, 0:64], fn), [pq[0]], [S[dst]])
    A(lambda e: e.activation(S["eg"][:], S["gcs"][:], AF.Exp), [S["gcs"]], [S["eg"]])
    D(lambda e: e.tensor_tensor(S["kdsc"][:], S["gtot"][:], S["gcs"][:], ALU.subtract), [S["gtot"], S["gcs"]], [S["kdsc"]])
    A(lambda e: e.activation(S["kdsc"][:], S["kdsc"][:], AF.Exp), [S["kdsc"]], [S["kdsc"]])
    D(lambda e: e.tensor_tensor(S["beg"][:], S["beta"][:], S["eg"][:], ALU.mult), [S["beta"], S["eg"]], [S["beg"]])
    D(lambda e: e.memset(St[0][:], 0.0), [], [St[0]])

    def load_group(g):
        i = g % 2
        cs = slice(g * 512, (g + 1) * 512)
        P.dma("sp", qTg[i][:], qT_d[:, cs], writes=[qTg[i]], sb=qTg[i])
        P.dma("act", kTg[i][:], kT_d[:, cs], writes=[kTg[i]], sb=kTg[i])
        P.dma("sp", kkg[i][:], kk_d[cs, :].rearrange("(n p) d -> p n d", p=128), writes=[kkg[i]], sb=kkg[i])
        P.dma("act", vkg[i][:], vk_d[cs, :].rearrange("(n p) d -> p n d", p=128), writes=[vkg[i]], sb=vkg[i])
        P.dma("sp", zkg[i][:], zk_d[cs, :].rearrange("(n p) d -> p n d", p=128), writes=[zkg[i]], sb=zkg[i])

    NG = NTILE // GT
    load_group(0)
    cur = 0
    for g in range(NG):
        if g + 1 < NG:
            load_group(g + 1)
        i = g % 2
        tiles = range(GT)
        col = lambda gi: slice(g * GT + gi, g * GT + gi + 1)
        kT = lambda gi: kTg[i][:, gi * 128:(gi + 1) * 128]
        qT = lambda gi: qTg[i][:, gi * 128:(gi + 1) * 128]
        qa = lambda gi: A_q[gi][0]
        qb = lambda gi: A_q[gi][1]
        qc = lambda gi: A_q[gi][2]
        qd = lambda gi: B_q[gi][3]
        for gi in tiles:
            PE(lambda e: e.matmul(qa(gi)[:], S["g"][:, col(gi)].broadcast_to([128, 128]), TRI, start=True, stop=True),
               [S["g"], cm], [qa(gi)])
            PE(lambda e: e.matmul(qb(gi)[:], kT(gi), kT(gi), start=True, stop=True), [kTg[i]], [qb(gi)])
            PE(lambda e: e.matmul(qc(gi)[:], kT(gi), qT(gi), start=True, stop=True), [kTg[i], qTg[i]], [qc(gi)])
        for gi in tiles:
            w = lambda k: W[k, gi]
            gc_ = S["gcs"][:, col(gi)]
            D(lambda e: e.tensor_copy(w("rbs")[:], qa(gi)[:]), [qa(gi)], [w("rbs")])
            D(lambda e: e.tensor_scalar(w("Y")[:], w("rbs")[:], gc_, 0.0, ALU.subtract, ALU.max), [w("rbs"), S["gcs"]], [w("Y")])
            D(lambda e: e.tensor_scalar(w("YT")[:], w("rbs")[:], gc_, 0.0, ALU.subtract, ALU.min), [w("rbs"), S["gcs"]], [w("YT")])
        for gi in tiles:
            w = lambda k: W[k, gi]
            A(lambda e: e.activation(w("decn")[:], w("Y")[:], AF.Exp, scale=-1.0), [w("Y")], [w("decn")])
            A(lambda e: e.activation(w("dect")[:], w("YT")[:], AF.Exp), [w("YT")], [w("dect")])
            A(lambda e: e.activation(w("erb")[:], w("rbs")[:], AF.Exp), [w("rbs")], [w("erb")])
        for gi in tiles:
            w = lambda k: W[k, gi]
            G(lambda e: e.tensor_scalar(w("R0")[:, 0:128], vkg[i][:, gi, :], S["beta"][:, col(gi)], None, ALU.mult),
              [vkg[i], S["beta"]], [w("R0")])
            G(lambda e: e.tensor_scalar(w("R0")[:, 128:256], kkg[i][:, gi, :], S["beg"][:, col(gi)], None, ALU.mult),
              [kkg[i], S["beg"]], [w("R0")])
        for gi in tiles:
            w = lambda k: W[k, gi]
            D(lambda e: e.tensor_tensor(w("t")[:], qb(gi)[:], w("decn")[:], ALU.mult), [qb(gi), w("decn")], [w("t")])
            D(lambda e: e.scalar_tensor_tensor(w("N")[:], w("t")[:], S["beta"][:, col(gi)], MASKS, ALU.mult, ALU.mult),
              [w("t"), S["beta"], cm], [w("N")])
        for gi in tiles:
            PE(lambda e: e.matmul(qd(gi)[:], W["N", gi][:], IDENT, start=True, stop=True), [W["N", gi], cm], [qd(gi)])
        for gi in tiles:
            w = lambda k: W[k, gi]
            G(lambda e: e.tensor_tensor(w("dect")[:], w("dect")[:], TRI, ALU.mult), [w("dect"), cm], [w("dect")])
            D(lambda e: e.tensor_tensor(w("QKm")[:], qc(gi)[:], w("dect")[:], ALU.mult), [qc(gi), w("dect")], [w("QKm")])
            G(lambda e: e.tensor_tensor(w("qeg")[:], qT(gi), w("erb")[:], ALU.mult), [qTg[i], w("erb")], [w("qeg")])
            G(lambda e: e.tensor_scalar(w("kd")[:], kkg[i][:, gi, :], S["kdsc"][:, col(gi)], None, ALU.mult),
              [kkg[i], S["kdsc"]], [w("kd")])
        for gi in tiles:
            A(lambda e: e.activation(W["M", gi][:], qd(gi)[:], AF.Copy), [qd(gi)], [W["M", gi]])
        Mc = {gi: W["M", gi] for gi in tiles}
        Nc = {gi: W["N", gi] for gi in tiles}
        Rc = {gi: W["R0", gi] for gi in tiles}
        for lvl in range(6):
            sign = ALU.subtract if lvl == 0 else ALU.add
            apb = (lambda gi: A_h[gi][0]) if lvl % 2 == 0 else (lambda gi: A_h[gi][1])
            pm = (lambda gi: B_q[gi][0]) if lvl % 2 == 0 else (lambda gi: B_q[gi][2])
            pn = (lambda gi: B_q[gi][1]) if lvl % 2 == 0 else (lambda gi: B_q[gi][3])
            for gi in tiles:
                PE(lambda e: e.matmul(apb(gi)[:], Mc[gi][:], Rc[gi][:], start=True, stop=True), [Mc[gi], Rc[gi]], [apb(gi)])
                if lvl < 5:
                    PE(lambda e: e.matmul(pm(gi)[:], Nc[gi][:], Mc[gi][:], start=True, stop=True), [Nc[gi], Mc[gi]], [pm(gi)])
                if lvl < 4:
                    PE(lambda e: e.matmul(pn(gi)[:], Mc[gi][:], Nc[gi][:], start=True, stop=True), [Nc[gi], Mc[gi]], [pn(gi)])
            for gi in tiles:
                Rn = W["R1", gi] if Rc[gi] is W["R0", gi] else W["R0", gi]
                D(lambda e: e.tensor_tensor(Rn[:], Rc[gi][:], apb(gi)[:], sign), [Rc[gi], apb(gi)], [Rn])
                Rc[gi] = Rn
                if lvl < 5:
                    Mn = W["Ma", gi] if lvl % 2 == 0 else W["Mb", gi]
                    A(lambda e: e.activation(Mn[:], pm(gi)[:], AF.Copy), [pm(gi)], [Mn])
                if lvl < 4:
                    Nn = W["Na", gi] if lvl % 2 == 0 else W["Nb", gi]
                    A(lambda e: e.activation(Nn[:], pn(gi)[:], AF.Copy), [pn(gi)], [Nn])
                if lvl < 5:
                    Mc[gi] = Mn
                if lvl < 4:
                    Nc[gi] = Nn
        for gi in tiles:
            PE(lambda e: e.matmul(B_q[gi][0][:], Rc[gi][:, 128:256], IDENT, start=True, stop=True), [Rc[gi], cm], [B_q[gi][0]])
        for gi in tiles:
            A(lambda e: e.activation(W["kcdT", gi][:], B_q[gi][0][:], AF.Copy), [B_q[gi][0]], [W["kcdT", gi]])
        for gi in tiles:
            w = lambda k: W[k, gi]
            for c in range(2):
                lo, hi = c * 64, (c + 1) * 64
                s_old, s_new = St[cur], St[1 - cur]
                PE(lambda e: e.matmul(A_q[gi][0][:], w("kcdT")[:], s_old[:], start=True, stop=True), [w("kcdT"), s_old], [A_q[gi][0]])
                D(lambda e: e.tensor_tensor(w("u")[lo:hi, :], Rc[gi][lo:hi, 0:128], A_q[gi][0][lo:hi, :], ALU.subtract),
                  [Rc[gi], A_q[gi][0]], [w("u")])
                PE(lambda e: e.matmul(A_q[gi][1][:], w("kd")[lo:hi, :], w("u")[lo:hi, :], start=True, stop=True),
                   [w("kd"), w("u")], [A_q[gi][1]])
                P.op("pe", lambda e: e.matmul(B_q[gi][1][:], w("qeg")[:], s_old[:], start=True, stop=False),
                     reads=[w("qeg"), s_old], writes=[B_q[gi][1]], defer=True)
                PE(lambda e: e.matmul(B_q[gi][1][:], w("QKm")[lo:hi, :], w("u")[lo:hi, :], start=False, stop=True),
                   [w("QKm"), w("u")], [B_q[gi][1]])
                dec = S["dec0"] if c == 0 else S["dec1"]
                D(lambda e: e.scalar_tensor_tensor(s_new[:], s_old[:], dec[:, col(gi)], A_q[gi][1][:], ALU.mult, ALU.add),
                  [s_old, dec, A_q[gi][1]], [s_new])
                A(lambda e: e.activation(w("o")[lo:hi, :], B_q[gi][1][lo:hi, :], AF.Copy), [B_q[gi][1]], [w("o")])
                cur = 1 - cur
            A(lambda e: e.activation(w("sz")[:], w("o")[:], AF.Square, accum_out=w("ss")[:]), [w("o")], [w("sz"), w("ss")])
            A(lambda e: e.activation(w("rs")[:], w("ss")[:], AF.Sqrt, bias=EPS, scale=1.0 / 128), [w("ss")], [w("rs")])
            D(lambda e: e.reciprocal(w("rs")[:], w("rs")[:]), [w("rs")], [w("rs")])
            D(lambda e: e.scalar_tensor_tensor(w("o")[:], w("o")[:], w("rs")[:, 0:1], gn[:], ALU.mult, ALU.mult),
              [w("o"), w("rs"), gn], [w("o")])
            A(lambda e: e.activation(w("sz")[:], zkg[i][:, gi, :], AF.Silu), [zkg[i]], [w("sz")])
            D(lambda e: e.tensor_tensor(og[i][:, gi, :], w("o")[:], w("sz")[:], ALU.mult), [w("o"), w("sz")], [og[i]])
        P.dma("sp", out_d[g * 512:(g + 1) * 512, :].rearrange("(n p) d -> p n d", p=128), og[i][:],
              reads=[og[i]], sb=og[i])
    P.wait_all("sp", og)
    P.close()
    return nc


def build_ssd():
    T, NTILE, H, PD, NS = 8192, 64, 8, 64, 128
    nc = bass.Bass("TRN2", target_bir_lowering=False)
    P = Prog(nc)
    x_d = P.dram("x_tok", [T, 512], F32, "ExternalInput")
    z_d = P.dram("z_tok", [T, 512], F32, "ExternalInput")
    bt_d = P.dram("BT", [128, T], F32, "ExternalInput")
    ct_d = P.dram("CT", [128, T], F32, "ExternalInput")
    bk_d = P.dram("B_tok", [T, 128], F32, "ExternalInput")
    dtr_d = P.dram("dtr", [128, 512], F32, "ExternalInput")
    rv_d = P.dram("rowvec", [128, 24], F32, "ExternalInput")
    cm_d = P.dram("cmat", [128, 512], F32, "ExternalInput")
    out_d = P.dram("yz_tok", [T, 512], F32, "ExternalOutput")

    cm = P.sbuf([128, 512], F32, "cm")
    TRI, BLK, SEL0, SEL1 = (cm[:, i * 128:(i + 1) * 128] for i in range(4))
    rv = P.sbuf([128, 24], F32, "rv")
    dtr = P.sbuf([128, 512], F32, "dtr")
    names = ("xb", "ax", "ex", "ln", "dt", "da", "acs", "atot", "eacs", "dte", "dec0", "dec1", "aneg", "nacs")
    S = {k: P.sbuf([128, 512], F32, "s_" + k) for k in names}
    ST = [P.sbuf([128, 512], F32, f"ST{i}") for i in range(2)]
    NB = 3
    xt = [P.sbuf([128, 512], F32, f"xt{i}") for i in range(NB)]
    zt = [P.sbuf([128, 512], F32, f"zt{i}") for i in range(NB)]
    btt = [P.sbuf([128, 128], BF16, f"bt{i}") for i in range(NB)]
    ctt = [P.sbuf([128, 128], BF16, f"ct{i}") for i in range(NB)]
    bkt = [P.sbuf([128, 128], BF16, f"bk{i}") for i in range(NB)]
    xdd = [P.sbuf([128, 512], BF16, f"xdd{i}") for i in range(2)]
    xdt = [P.sbuf([128, 512], BF16, f"xdt{i}") for i in range(2)]
    cbm = [P.sbuf([128, 128], F32, f"cbm{i}") for i in range(2)]
    xx = [P.sbuf([128, 128], F32, f"xx{i}") for i in range(8)]
    mt = [P.sbuf([128, 128], BF16, f"mt{i}") for i in range(8)]
    STb = [P.sbuf([128, 512], BF16, f"STb{i}") for i in range(2)]
    yt = [P.sbuf([128, 512], F32, f"yt{i}") for i in range(2)]
    uo = [P.sbuf([128, 512], F32, f"uo{i}") for i in range(2)]
    bank = [P.psum([128, 512], F32, f"bank{i}") for i in range(8)]

    rbv = [View(bank[4 + h // 4], bank[4 + h // 4][:, (h % 4) * 128:(h % 4 + 1) * 128]) for h in range(8)]
    csbk = [bank[7], bank[1]]
    P.dma("sp", cm[:], cm_d[:], writes=[cm], sb=cm)
    P.dma("sp", rv[:], rv_d[:], writes=[rv], sb=rv)
    P.dma("sp", dtr[:], dtr_d[:], writes=[dtr], sb=dtr)

    def bc8(ap):
        return ap.unsqueeze(2).broadcast_to([ap.shape[0], 8, 64])

    def v3(ap):
        return ap.rearrange("p (h d) -> p h d", d=64)

    def rep(ap):
        return ap.unsqueeze(1).broadcast_to([128, 64, 8])

    def t3(ap):
        return ap.rearrange("p (n h) -> p n h", h=8)

    D = lambda fn, r, w: P.op("dve", fn, reads=r, writes=w)
    A = lambda fn, r, w: P.op("act", fn, reads=r, writes=w)
    D(lambda e: e.tensor_tensor(t3(S["xb"][:]), t3(dtr[:]), rep(rv[:, 0:8]), ALU.add), [dtr, rv], [S["xb"]])
    A(lambda e: e.activation(S["ax"][:], S["xb"][:], AF.Abs), [S["xb"]], [S["ax"]])
    A(lambda e: e.activation(S["ex"][:], S["ax"][:], AF.Exp, scale=-1.0), [S["ax"]], [S["ex"]])
    A(lambda e: e.activation(S["ln"][:], S["ex"][:], AF.Ln, bias=1.0, scale=1.0), [S["ex"]], [S["ln"]])
    D(lambda e: e.scalar_tensor_tensor(S["dt"][:], S["xb"][:], 0.0, S["ln"][:], ALU.max, ALU.add),
      [S["xb"], S["ln"]], [S["dt"]])
    A(lambda e: e.activation(S["aneg"][:, 0:8], rv[:, 8:16], AF.Exp), [rv], [S["aneg"]])
    D(lambda e: e.tensor_tensor(t3(S["da"][:]), t3(S["dt"][:]), rep(S["aneg"][:, 0:8]), ALU.mult),
      [S["dt"], S["aneg"]], [S["da"]])
    D(lambda e: e.tensor_scalar(S["da"][:], S["da"][:], -1.0, None, ALU.mult), [S["da"]], [S["da"]])
    for (dst, mat) in (("acs", TRI), ("atot", BLK), ("dec0", SEL0), ("dec1", SEL1)):
        P.op("pe", lambda e: e.matmul(bank[0][:], mat, S["da"][:], start=True, stop=True),
             reads=[cm, S["da"]], writes=[bank[0]])
        if dst in ("dec0", "dec1"):
            A(lambda e: e.activation(S[dst][:], bank[0][:], AF.Exp), [bank[0]], [S[dst]])
        else:
            A(lambda e: e.activation(S[dst][:], bank[0][:], AF.Copy), [bank[0]], [S[dst]])
    A(lambda e: e.activation(S["eacs"][:], S["acs"][:], AF.Exp), [S["acs"]], [S["eacs"]])
    A(lambda e: e.activation(S["nacs"][:], S["acs"][:], AF.Copy, scale=-1.0), [S["acs"]], [S["nacs"]])
    D(lambda e: e.tensor_tensor(S["dte"][:], S["atot"][:], S["acs"][:], ALU.subtract), [S["atot"], S["acs"]], [S["dte"]])
    A(lambda e: e.activation(S["dte"][:], S["dte"][:], AF.Exp), [S["dte"]], [S["dte"]])
    D(lambda e: e.tensor_tensor(S["dte"][:], S["dte"][:], S["dt"][:], ALU.mult), [S["dte"], S["dt"]], [S["dte"]])
    D(lambda e: e.memset(ST[0][:], 0.0), [], [ST[0]])
    D(lambda e: e.memset(STb[0][:], 0.0), [], [STb[0]])

    def load_tile(n):
        i = n % NB
        r = slice(n * 128, (n + 1) * 128)
        P.dma("sp", xt[i][:], x_d[r, :], writes=[xt[i]], sb=xt[i])
        P.dma("act", zt[i][:], z_d[r, :], writes=[zt[i]], sb=zt[i])
        P.dma("pool", btt[i][:], bt_d[:, r], writes=[btt[i]], sb=btt[i])
        P.dma("pool", ctt[i][:], ct_d[:, r], writes=[ctt[i]], sb=ctt[i])
        P.dma("pool", bkt[i][:], bk_d[r, :], writes=[bkt[i]], sb=bkt[i])

    G = lambda fn, r, w: P.op("pool", fn, reads=r, writes=w)
    load_tile(0)
    load_tile(1)
    state = {"cur": 0}

    def front(n):
        i = n % NB
        X, BT, CT = xt[i], btt[i], ctt[i]
        sc = slice(n * 8, (n + 1) * 8)
        xd, xe, y = xdt[n % 2], xdd[n % 2], yt[n % 2]
        G(lambda e: e.tensor_tensor(v3(xd[:]), v3(X[:]), bc8(S["dt"][:, sc]), ALU.mult), [X, S["dt"]], [xd])
        G(lambda e: e.tensor_tensor(v3(xe[:]), v3(X[:]), bc8(S["dte"][:, sc]), ALU.mult), [X, S["dte"]], [xe])
        G(lambda e: e.tensor_tensor(v3(y[:]), v3(X[:]), bc8(rv[:, 16:24]), ALU.mult), [X, rv], [y])
        P.op("pe", lambda e: e.matmul(bank[0][:, 0:128], BT[:], CT[:], start=True, stop=True),
             reads=[BT, CT], writes=[bank[0]])
        cb = cbm[n % 2]
        D(lambda e: e.tensor_tensor(cb[:], bank[0][:, 0:128], TRI, ALU.mult), [bank[0], cm], [cb])
        for h in range(H):
            col = n * 8 + h
            rb = rbv[h]
            P.op("pe", lambda e: e.matmul(rb[:], S["da"][:, col:col + 1].broadcast_to([128, 128]), TRI,
                                          start=True, stop=True),
                 reads=[S["da"], cm], writes=[rb])

    def middle(n):
        i = n % NB
        BK = bkt[i]
        xd, xe, cb = xdt[n % 2], xdd[n % 2], cbm[n % 2]
        yb = bank[2 + n % 2]
        for c in range(2):
            lo, hi = c * 64, (c + 1) * 64
            P.op("pe", lambda e: e.matmul(csbk[c][:], BK[lo:hi, :], xe[lo:hi, :], start=True, stop=True),
                 reads=[BK, xe], writes=[csbk[c]])
        for h in range(H):
            col = n * 8 + h
            rb = rbv[h]
            x_ = xx[h]
            A(lambda e: e.activation(x_[:], rb[:], AF.Exp, bias=S["nacs"][:, col:col + 1], scale=1.0),
              [rb, S["nacs"]], [x_])
        for h in range(H):
            x_ = xx[h]
            m_ = mt[h]
            D(lambda e: e.scalar_tensor_tensor(m_[:], x_[:], 1.0, cb[:], ALU.min, ALU.mult), [x_, cb], [m_])
        for h in range(H):
            m_ = mt[h]
            P.op("pe", lambda e: e.matmul(yb[:, h * 64:(h + 1) * 64], m_[:], xd[:, h * 64:(h + 1) * 64],
                                          start=True, stop=True),
                 reads=[m_, xd], writes=[yb])

    def scan(n):
        i = n % NB
        Z, CT = zt[i], ctt[i]
        sc = slice(n * 8, (n + 1) * 8)
        y = yt[n % 2]
        yb = bank[2 + n % 2]
        D(lambda e: e.tensor_tensor(y[:], y[:], yb[:], ALU.add), [y, yb], [y])
        for c in range(2):
            lo, hi = c * 64, (c + 1) * 64
            s_old, s_new = ST[state["cur"]], ST[1 - state["cur"]]
            yo = bank[6]
            sb_old, sb_new = STb[state["cur"]], STb[1 - state["cur"]]
            P.op("pe", lambda e: e.matmul(yo[:], CT[:], sb_old[:], start=True, stop=True),
                 reads=[CT, sb_old], writes=[yo])
            csb = csbk[c]
            u = uo[c]
            dec = S["dec0"] if c == 0 else S["dec1"]
            D(lambda e: e.tensor_tensor(v3(s_new[:]), v3(s_old[:]), bc8(dec[:, sc]), ALU.mult), [s_old, dec], [s_new])
            D(lambda e: e.tensor_tensor(s_new[:], s_new[:], csb[:], ALU.add), [s_new, csb], [s_new])
            A(lambda e: e.activation(sb_new[:], s_new[:], AF.Copy), [s_new], [sb_new])
            D(lambda e: e.tensor_tensor(v3(u[lo:hi, :]), v3(yo[lo:hi, :]), bc8(S["eacs"][lo:hi, sc]), ALU.mult),
              [yo, S["eacs"]], [u])
            G(lambda e: e.tensor_tensor(y[lo:hi, :], y[lo:hi, :], u[lo:hi, :], ALU.add), [y, u], [y])
            state["cur"] = 1 - state["cur"]
        A(lambda e: e.activation(Z[:], Z[:], AF.Silu), [Z], [Z])
        D(lambda e: e.tensor_tensor(y[:], y[:], Z[:], ALU.mult), [y, Z], [y])
        P.dma("sp", out_d[n * 128:(n + 1) * 128, :], y[:], reads=[y], sb=y)

    front(0)
    for n in range(NTILE):
        if n + 2 < NTILE:
            load_tile(n + 2)
        middle(n)
        if n + 1 < NTILE:
            front(n + 1)
        scan(n)
    P.wait_all("sp", yt)
    P.close()
    return nc


def build_tail(layer):
    rms = layer == 1
    final = layer == 1
    C = 4096 if layer == 1 else 2048
    CC = C // 128
    NT = 1024
    nc = bass.Bass("TRN2", target_bir_lowering=False)
    P = Prog(nc)
    xT_d = P.dram("xT", [2048, NT], F32, "ExternalInput")
    cat_d = P.dram("catT", [C, NT], F32, "ExternalInput")
    vec_d = P.dram("vec", [128, 6 * 16], F32, "ExternalInput")
    gc_d = P.dram("gcat", [128, CC], F32, "ExternalInput")
    wout_d = P.dram("wout", [C, 2048], F32, "ExternalInput")
    wr_d = P.dram("wr", [2048, 36], F32, "ExternalInput")
    br_d = P.dram("br", [128, 36], F32, "ExternalInput")
    w1_d = P.dram("w1", [32, 2048, 512], F32, "ExternalInput")
    w3_d = P.dram("w3", [32, 2048, 512], F32, "ExternalInput")
    w2_d = P.dram("w2", [32, 512, 2048], F32, "ExternalInput")
    id_d = P.dram("ident", [128, 128], F32, "ExternalInput")
    out_d = P.dram("outT", [2048, NT], F32, "ExternalOutput")

    xT = [[P.sbuf([128, 512], F32, f"x{j}_{h}") for h in range(2)] for j in range(16)]
    hT = [P.sbuf([128, 512], BF16, f"h{i}") for i in range(32)]
    wsl = [P.sbuf([128, 8192], BF16, f"wsl{i}") for i in range(4)]
    aT = [[P.sbuf([128, 512], BF16, f"a{j}_{h}") for h in range(2)] for j in range(4)]
    G = [P.sbuf([128, 1024], F32, f"G{i}") for i in range(2)]
    tmp1 = [P.sbuf([128, 512], F32, f"t1_{i}") for i in range(2)]
    tmp2 = [P.sbuf([128, 512], F32, f"t2_{i}") for i in range(2)]
    stg = [P.sbuf([128, 512], F32, f"stg{i}") for i in range(2)]
    sqb = [P.sbuf([128, 512], BF16, f"sq{i}") for i in range(2)]
    rstd = [P.sbuf([128, 512], F32, f"rstd{i}") for i in range(2)]
    ones = P.sbuf([128, 128], BF16, "ones")
    ident = P.sbuf([128, 128], F32, "ident")
    wr = P.sbuf([128, 16, 36], F32, "wr")
    br = P.sbuf([128, 36], F32, "br")
    vec = P.sbuf([128, 96], F32, "vec")
    gc = P.sbuf([128, CC], F32, "gc")
    gs2 = P.sbuf([128, 16], F32, "gs2")
    gd = [P.sbuf([128, 32], F32, f"gd{i}") for i in range(8)]
    sm = {k: P.sbuf([128, 36], F32, "sm_" + k) for k in
          ("lg", "ge", "ohg", "pen", "em", "oh1", "em2", "oh2")}
    sc = {k: P.sbuf([128, 1], F32, "sc_" + k) for k in
          ("gmax", "ngmax", "gsum", "grp", "m1", "m2", "d", "ed", "den", "p1", "p2", "p1g", "p2g")}
    bank = [P.psum([128, 512], F32, f"bank{i}") for i in range(8)]

    def V(i):
        return vec[:, i * 16:(i + 1) * 16]
    GATE_MIX, SHIFT2, SCALE2, GATE_FFN, NORMG2, FING = range(6)

    P.dma("sp", vec[:], vec_d[:], writes=[vec], sb=vec)
    P.dma("sp", gc[:], gc_d[:], writes=[gc], sb=gc)
    P.dma("sp", ident[:], id_d[:], writes=[ident], sb=ident)
    P.dma("sp", wr[:], wr_d.t.rearrange("(j p) n -> p j n", p=128), writes=[wr], sb=wr)
    P.dma("sp", br[:], br_d[:], writes=[br], sb=br)
    P.op("dve", lambda e: e.memset(ones[:], 1.0), writes=[ones])
    for j in range(16):
        for h in range(2):
            b = xT[j][h]
            P.dma("act" if (j + h) % 2 else "sp", b[:], xT_d[j * 128:(j + 1) * 128, h * 512:(h + 1) * 512],
                  writes=[b], sb=b)

    ring = {"n": 0}

    def wload(src_ap, kind):
        s = wsl[ring["n"] % 4]
        ring["n"] += 1
        if kind == "k16":
            dst = s[:].rearrange("p (j n) -> p j n", n=512)
            src = src_ap.rearrange("(j p) n -> p j n", p=128)
        else:
            dst = s[:].rearrange("p (j n) -> p j n", n=2048)
            src = src_ap.rearrange("(j p) n -> p j n", p=128)
        P.dma("pool", dst, src, writes=[s], sb=s)
        return s, dst

    def sum_sq_rstd(srcs, h, n_feat):
        n = len(srcs)
        for i, (sbuf_, ap) in enumerate(srcs):
            q = sqb[i % 2]
            P.op("act", lambda e: e.activation(q[:], ap, AF.Square), reads=[sbuf_], writes=[q])
            P.op("pe", lambda e: e.matmul(bank[6][:], ones[:], q[:], start=(i == 0), stop=(i == n - 1)),
                 reads=[ones, q], writes=[bank[6]])
        t = tmp1[0]
        P.op("act", lambda e: e.activation(t[:], bank[6][:], AF.Sqrt, bias=EPS, scale=1.0 / n_feat),
             reads=[bank[6]], writes=[t])
        P.op("dve", lambda e: e.reciprocal(rstd[h][:], t[:]), reads=[t], writes=[rstd[h]])

    nmm = 0
    for h in range(2):
        cbufs = []
        for cc in range(CC):
            s = stg[cc % 2]
            P.dma("sp" if cc % 2 else "act", s[:], cat_d[cc * 128:(cc + 1) * 128, h * 512:(h + 1) * 512],
                  writes=[s], sb=s)
            if rms:
                q = sqb[cc % 2]
                P.op("act", lambda e: e.activation(q[:], s[:], AF.Square), reads=[s], writes=[q])
                P.op("pe", lambda e: e.matmul(bank[6][:], ones[:], q[:], start=(cc == 0), stop=(cc == CC - 1)),
                     reads=[ones, q], writes=[bank[6]])
            dst = hT[cc] if CC == 32 else hT[h * 16 + cc]
            P.op("dve", lambda e: e.tensor_scalar(dst[:], s[:], gc[:, cc:cc + 1], None, ALU.mult),
                 reads=[s, gc], writes=[dst])
            cbufs.append(dst)
        if rms:
            t = tmp1[0]
            P.op("act", lambda e: e.activation(t[:], bank[6][:], AF.Sqrt, bias=EPS, scale=1.0 / C),
                 reads=[bank[6]], writes=[t])
            P.op("dve", lambda e: e.reciprocal(rstd[h][:], t[:]), reads=[t], writes=[rstd[h]])
        for cb in range(4):
            slots = []
            for rb in range(C // 2048):
                slots.append(wload(wout_d[rb * 2048:(rb + 1) * 2048, cb * 512:(cb + 1) * 512], "k16"))
            for fc in range(4):
                j = cb * 4 + fc
                bk = bank[nmm % 2]
                nmm += 1
                for cc in range(CC):
                    sb_, view = slots[cc // 16]
                    P.op("pe", lambda e: e.matmul(bk[:], view[:, cc % 16, fc * 128:(fc + 1) * 128], cbufs[cc][:],
                                                  start=(cc == 0), stop=(cc == CC - 1)),
                         reads=[sb_, cbufs[cc]], writes=[bk], defer=(cc != CC - 1))
                xb = xT[j][h]
                if rms:
                    t = tmp2[j % 2]
                    P.op("dve", lambda e: e.tensor_tensor(t[:], bk[:], rstd[h][:], ALU.mult),
                         reads=[bk, rstd[h]], writes=[t])
                    P.op("dve", lambda e: e.scalar_tensor_tensor(xb[:], t[:], V(GATE_MIX)[:, j:j + 1], xb[:],
                                                                 ALU.mult, ALU.add),
                         reads=[t, vec, xb], writes=[xb])
                else:
                    P.op("dve", lambda e: e.scalar_tensor_tensor(xb[:], bk[:], V(GATE_MIX)[:, j:j + 1], xb[:],
                                                                 ALU.mult, ALU.add),
                         reads=[bk, vec, xb], writes=[xb])

    P.op("dve", lambda e: e.tensor_scalar(gs2[:], V(SCALE2), 1.0, None, ALU.add), reads=[vec], writes=[gs2])
    P.op("dve", lambda e: e.tensor_tensor(gs2[:], gs2[:], V(NORMG2), ALU.mult), reads=[gs2, vec], writes=[gs2])
    for h in range(2):
        sum_sq_rstd([(xT[j][h], xT[j][h][:]) for j in range(16)], h, 2048)
        for j in range(16):
            hf = stg[j % 2]
            P.op("dve", lambda e: e.tensor_tensor(hf[:], xT[j][h][:], rstd[h][:], ALU.mult),
                 reads=[xT[j][h], rstd[h]], writes=[hf])
            P.op("dve", lambda e: e.tensor_scalar(hf[:], hf[:], gs2[:, j:j + 1], V(SHIFT2)[:, j:j + 1],
                                                  ALU.mult, ALU.add),
                 reads=[hf, gs2, vec], writes=[hf])
            hb = hT[j * 2 + h]
            P.op("act", lambda e: e.activation(hb[:], hf[:], AF.Copy), reads=[hf], writes=[hb])
            for tt in range(4):
                bk = bank[2 + tt]
                P.op("pe", lambda e: e.matmul(bk[:, 0:36], hf[:, tt * 128:(tt + 1) * 128], wr[:, j, :],
                                              start=(j == 0), stop=(j == 15)),
                     reads=[hf, wr], writes=[bk], defer=not (j == 15 or tt == 3))
        for tt in range(4):
            bk = bank[2 + tt]
            g = gd[h * 4 + tt]
            lg, ge, ohg, pen, em, oh1, em2, oh2 = (sm[k] for k in ("lg", "ge", "ohg", "pen", "em", "oh1", "em2", "oh2"))
            D = lambda fn, r, w: P.op("dve", fn, reads=r, writes=w)
            A = lambda fn, r, w: P.op("act", fn, reads=r, writes=w)
            D(lambda e: e.tensor_tensor(lg[:], bk[:, 0:36], br[:], ALU.add), [bk, br], [lg])
            D(lambda e: e.tensor_reduce(sc["gmax"][:], lg[:, 0:4], AX.X, ALU.max), [lg], [sc["gmax"]])
            D(lambda e: e.tensor_scalar(sc["ngmax"][:], sc["gmax"][:], -1.0, None, ALU.mult), [sc["gmax"]], [sc["ngmax"]])
            A(lambda e: e.activation(ge[:, 0:4], lg[:, 0:4], AF.Exp, bias=sc["ngmax"][:, 0:1], scale=1.0),
              [lg, sc["ngmax"]], [ge])
            D(lambda e: e.tensor_reduce(sc["gsum"][:], ge[:, 0:4], AX.X, ALU.add), [ge], [sc["gsum"]])
            D(lambda e: e.reciprocal(sc["grp"][:], sc["gsum"][:]), [sc["gsum"]], [sc["grp"]])
            D(lambda e: e.tensor_scalar(ohg[:, 0:4], lg[:, 0:4], sc["gmax"][:, 0:1], None, ALU.is_equal),
              [lg, sc["gmax"]], [ohg])
            D(lambda e: e.tensor_scalar(pen[:, 0:4], ohg[:, 0:4], 1e30, -1e30, ALU.mult, ALU.add), [ohg], [pen])
            D(lambda e: e.tensor_tensor(em[:, 0:32].rearrange("p (g k) -> p g k", k=8),
                                        lg[:, 4:36].rearrange("p (g k) -> p g k", k=8),
                                        pen[:, 0:4].unsqueeze(2).broadcast_to([128, 4, 8]), ALU.add),
              [lg, pen], [em])
            D(lambda e: e.tensor_reduce(sc["m1"][:], em[:, 0:32], AX.X, ALU.max), [em], [sc["m1"]])
            D(lambda e: e.tensor_scalar(oh1[:, 0:32], em[:, 0:32], sc["m1"][:, 0:1], None, ALU.is_equal),
              [em, sc["m1"]], [oh1])
            D(lambda e: e.scalar_tensor_tensor(em2[:, 0:32], oh1[:, 0:32], -1e30, em[:, 0:32], ALU.mult, ALU.add),
              [oh1, em], [em2])
            D(lambda e: e.tensor_reduce(sc["m2"][:], em2[:, 0:32], AX.X, ALU.max), [em2], [sc["m2"]])
            D(lambda e: e.tensor_scalar(oh2[:, 0:32], em2[:, 0:32], sc["m2"][:, 0:1], None, ALU.is_equal),
              [em2, sc["m2"]], [oh2])
            D(lambda e: e.tensor_tensor(sc["d"][:], sc["m2"][:], sc["m1"][:], ALU.subtract),
              [sc["m2"], sc["m1"]], [sc["d"]])
            A(lambda e: e.activation(sc["ed"][:], sc["d"][:], AF.Exp), [sc["d"]], [sc["ed"]])
            D(lambda e: e.tensor_scalar(sc["den"][:], sc["ed"][:], 1.0, None, ALU.add), [sc["ed"]], [sc["den"]])
            D(lambda e: e.reciprocal(sc["p1"][:], sc["den"][:]), [sc["den"]], [sc["p1"]])
            D(lambda e: e.tensor_tensor(sc["p2"][:], sc["ed"][:], sc["p1"][:], ALU.mult),
              [sc["ed"], sc["p1"]], [sc["p2"]])
            D(lambda e: e.tensor_tensor(sc["p1g"][:], sc["p1"][:], sc["grp"][:], ALU.mult),
              [sc["p1"], sc["grp"]], [sc["p1g"]])
            D(lambda e: e.tensor_tensor(sc["p2g"][:], sc["p2"][:], sc["grp"][:], ALU.mult),
              [sc["p2"], sc["grp"]], [sc["p2g"]])
            D(lambda e: e.tensor_scalar(g[:], oh1[:, 0:32], sc["p1g"][:, 0:1], None, ALU.mult),
              [oh1, sc["p1g"]], [g])
            D(lambda e: e.scalar_tensor_tensor(g[:], oh2[:, 0:32], sc["p2g"][:, 0:1], g[:], ALU.mult, ALU.add),
              [oh2, sc["p2g"], g], [g])

    def emit_G(e_):
        for tt in range(8):
            bk = bank[6 + tt // 4]
            P.op("pe", lambda e: e.matmul(bk[:, (tt % 4) * 128:(tt % 4 + 1) * 128],
                                          gd[tt][:, e_:e_ + 1].broadcast_to([128, 128]), ident[:],
                                          start=True, stop=True),
                 reads=[gd[tt], ident], writes=[bk], defer=(tt % 4 != 3))
        gb = G[e_ % 2]
        for hh in range(2):
            P.op("act", lambda e: e.activation(gb[:, hh * 512:(hh + 1) * 512], bank[6 + hh][:], AF.Copy),
                 reads=[bank[6 + hh]], writes=[gb])

    NE = 32
    pend = []
    for m in range(2):
        pass
    loads = {}

    def issue_loads(e_):
        loads[e_] = (wload(w1_d[e_], "k16"), wload(w3_d[e_], "k16"), wload(w2_d[e_], "k4"))

    issue_loads(0)
    emit_G(0)
    k = 0
    for e_ in range(NE):
        (s1, v1), (s3, v3), (s2, v2) = loads[e_]
        gb = G[e_ % 2]
        for h in range(2):
            for jc in range(4):
                b1 = bank[k % 2]
                b3 = bank[2 + k % 2]
                for j in range(16):
                    P.op("pe", lambda e: e.matmul(b1[:], v1[:, j, jc * 128:(jc + 1) * 128], hT[j * 2 + h][:],
                                                  start=(j == 0), stop=(j == 15)),
                         reads=[s1, hT[j * 2 + h]], writes=[b1], defer=(j != 15))
                for j in range(16):
                    P.op("pe", lambda e: e.matmul(b3[:], v3[:, j, jc * 128:(jc + 1) * 128], hT[j * 2 + h][:],
                                                  start=(j == 0), stop=(j == 15)),
                         reads=[s3, hT[j * 2 + h]], writes=[b3], defer=(j != 15))
                t1 = tmp1[k % 2]
                t2 = tmp2[k % 2]
                P.op("act", lambda e: e.activation(t1[:], b1[:], AF.Silu), reads=[b1], writes=[t1])
                P.op("dve", lambda e: e.tensor_tensor(t2[:], t1[:], b3[:], ALU.mult), reads=[t1, b3], writes=[t2])
                P.op("dve", lambda e: e.tensor_tensor(aT[jc][h][:], t2[:], gb[:, h * 512:(h + 1) * 512], ALU.mult),
                     reads=[t2, gb], writes=[aT[jc][h]])
                k += 1
            if h == 0 and e_ + 1 < NE:
                pass
        if e_ + 1 < NE:
            emit_G(e_ + 1)
        for h in range(2):
            for fc in range(16):
                by = bank[4 + fc % 2]
                for jc in range(4):
                    P.op("pe", lambda e: e.matmul(by[:], v2[:, jc, fc * 128:(fc + 1) * 128], aT[jc][h][:],
                                                  start=(jc == 0), stop=(jc == 3)),
                         reads=[s2, aT[jc][h]], writes=[by], defer=(jc != 3))
                xb = xT[fc][h]
                P.op("dve", lambda e: e.scalar_tensor_tensor(xb[:], by[:], V(GATE_FFN)[:, fc:fc + 1], xb[:],
                                                             ALU.mult, ALU.add),
                     reads=[by, vec, xb], writes=[xb])
        if e_ + 1 < NE:
            issue_loads(e_ + 1)

    outs = []
    if final:
        for h in range(2):
            sum_sq_rstd([(xT[j][h], xT[j][h][:]) for j in range(16)], h, 2048)
            for j in range(16):
                xb = xT[j][h]
                P.op("dve", lambda e: e.tensor_tensor(xb[:], xb[:], rstd[h][:], ALU.mult),
                     reads=[xb, rstd[h]], writes=[xb])
                P.op("dve", lambda e: e.tensor_scalar(xb[:], xb[:], V(FING)[:, j:j + 1], None, ALU.mult),
                     reads=[xb, vec], writes=[xb])
    for j in range(16):
        for h in range(2):
            xb = xT[j][h]
            P.dma("sp" if (j + h) % 2 else "act", out_d[j * 128:(j + 1) * 128, h * 512:(h + 1) * 512], xb[:],
                  reads=[xb], sb=xb)
            outs.append(xb)
    P.wait_all("sp", outs)
    P.wait_all("act", outs)
    P.close()
    return nc

from concourse.bass_utils import run_bass_kernel_spmd

_CORES = list(range(8))


def _run(nc, maps):
    return run_bass_kernel_spmd(nc, maps, core_ids=_CORES).results


def tail_inputs(layer, xfull, cat, mod, inp):
    m_mix = mod[2 * layer]
    m_ffn = mod[2 * layer + 1]
    vec = np.concatenate([fm(m_mix[4096:]), fm(m_ffn[:2048]), fm(m_ffn[2048:4096]), fm(m_ffn[4096:]),
                          fm(inp["norm_g"][layer, 1]), fm(inp["final_norm_g"])], axis=1)
    vec = np.ascontiguousarray(vec.astype(np.float32))
    if layer == 0:
        gcat = np.ones(2048, np.float32)
        wout = inp["e_w_out"][0]
    else:
        gcat = inp["o_norm_g"][0]
        wout = inp["o_w_out"][0]
    wr = np.ascontiguousarray(np.concatenate([inp["moe_w_group"][layer], inp["moe_w_expert"][layer]], axis=1))
    br = np.concatenate([inp["moe_b_group"][layer], inp["moe_b_expert"][layer]])[None, :]
    br = np.ascontiguousarray(np.tile(br, (128, 1)).astype(np.float32))
    ident = np.eye(128, dtype=np.float32)
    w1 = np.ascontiguousarray(inp["moe_w1"][layer])
    w3 = np.ascontiguousarray(inp["moe_w3"][layer])
    w2 = np.ascontiguousarray(inp["moe_w2"][layer])
    maps = []
    for i in range(8):
        sl = slice(i * 1024, (i + 1) * 1024)
        maps.append({"xT": np.ascontiguousarray(xfull[sl].T), "catT": np.ascontiguousarray(cat[sl].T), "vec": vec,
                     "gcat": fm(gcat), "wout": np.ascontiguousarray(wout), "wr": wr, "br": br,
                     "w1": w1, "w3": w3, "w2": w2, "ident": ident})
    return maps


def kernel(**inputs):
    inp = {k: np.asarray(v, dtype=np.float32) for k, v in inputs.items()}
    x0 = inp["x"][0]
    c = inp["c"][0]
    cT = np.ascontiguousarray(c.reshape(16, 128).T)
    maps = [{"cT": cT, "wmod": np.ascontiguousarray(inp["w_mod"][:, :, i * 768:(i + 1) * 768]),
             "bmod": np.ascontiguousarray(inp["b_mod"][:, i * 768:(i + 1) * 768])} for i in range(8)]
    res = _run(build_mod(), maps)
    mod = np.concatenate([res[i]["mod"] for i in range(8)], axis=1)
    res = _run(build_pre(0), pre_inputs(0, x0, mod, inp))
    pout0 = np.concatenate([res[i]["pout"] for i in range(8)], axis=1)
    res = _run(build_gdn(), gdn_inputs(pout0, inp))
    a_out = np.concatenate([res[h]["o_tok"] for h in range(8)], axis=1)
    cat0 = np.concatenate([a_out, pout0[3072:4096].T], axis=1)
    res = _run(build_tail(0), tail_inputs(0, x0, cat0, mod, inp))
    x1 = np.concatenate([res[i]["outT"].T for i in range(8)], axis=0)
    res = _run(build_pre(1), pre_inputs(1, x1, mod, inp))
    pout1 = np.concatenate([res[i]["pout"] for i in range(8)], axis=1)
    res = _run(build_ssd(), ssd_inputs(pout1, inp))
    yz = np.concatenate([res[g]["yz_tok"] for g in range(8)], axis=1)
    res = _run(build_tail(1), tail_inputs(1, x1, yz, mod, inp))
    out = np.concatenate([res[i]["outT"].T for i in range(8)], axis=0)
    return np.ascontiguousarray(out[None].astype(np.float32))
```

```python
import numpy as np

from contextlib import ExitStack
import concourse.bass as bass
import concourse.mybir as mybir

F32 = mybir.dt.float32
BF16 = mybir.dt.bfloat16
I32 = mybir.dt.int32
AF = mybir.ActivationFunctionType
ALU = mybir.AluOpType
AX = mybir.AxisListType


class Buf:
    __slots__ = ("t", "name", "w", "r", "dsem")

    def __init__(self, t, name):
        self.t = t
        self.name = name
        self.w = None
        self.r = {}
        self.dsem = None

    def __getitem__(self, idx):
        return self.t[idx]


class View:
    __slots__ = ("buf", "ap")

    def __init__(self, buf, ap):
        self.buf = buf
        self.ap = ap

    def __getitem__(self, idx):
        return self.ap[idx]


class Prog:
    def __init__(self, nc):
        self.nc = nc
        self.st = ExitStack()
        self.eng = {"pe": nc.tensor, "act": nc.scalar, "dve": nc.vector,
                    "pool": nc.gpsimd, "sp": nc.sync}
        self.sems = {}
        self.cnt = {}
        self.waited = {e: {} for e in self.eng}
        for e in ("pe", "act", "dve", "pool"):
            self.sems[e] = self.st.enter_context(nc.semaphore("s_" + e))
            self.cnt[e] = 0
        self.nbuf = 0
        self.pending = {}
        self.dma_sem_free = []
        self.ndsem = 0

    def sbuf(self, shape, dt, name=None):
        self.nbuf += 1
        name = name or f"sb{self.nbuf}"
        t = self.st.enter_context(self.nc.sbuf_tensor("S_" + name, list(shape), dt))
        return Buf(t, name)

    def psum(self, shape, dt, name=None):
        self.nbuf += 1
        name = name or f"ps{self.nbuf}"
        t = self.st.enter_context(self.nc.psum_tensor("P_" + name, list(shape), dt))
        return Buf(t, name)

    def dram(self, name, shape, dt, kind):
        t = self.nc.dram_tensor(name, list(shape), dt, kind=kind)
        return Buf(t.ap(), name)

    def new_dma_sem(self):
        self.ndsem += 1
        k = f"d{self.ndsem}"
        self.sems[k] = self.st.enter_context(self.nc.semaphore("s_" + k))
        self.cnt[k] = 0
        return k

    def _wait(self, e, deps):
        eng = self.eng[e]
        need = {}
        for d in deps:
            if d is None:
                continue
            k, v = d
            if e == "pe" and k == "pe":
                continue
            if v > need.get(k, 0):
                need[k] = v
        for k, v in need.items():
            if self.waited[e].get(k, 0) < v:
                eng.wait_ge(self.sems[k], v)
                self.waited[e][k] = v

    def _deps(self, reads, writes):
        reads = [getattr(b, "buf", b) for b in reads]
        writes = [getattr(b, "buf", b) for b in writes]
        deps = []
        for b in reads:
            deps.append(b.w)
        for b in writes:
            deps.append(b.w)
            deps.extend(b.r.items())
        return deps

    def _mark(self, key, val, reads, writes):
        reads = [getattr(b, "buf", b) for b in reads]
        writes = [getattr(b, "buf", b) for b in writes]
        for b in reads:
            if b.r.get(key, 0) < val:
                b.r[key] = val
        for b in writes:
            b.w = (key, val)
            b.r = {}

    def op(self, e, fn, reads=(), writes=(), defer=False):
        self._wait(e, self._deps(reads, writes))
        inst = fn(self.eng[e])
        if defer:
            pr, pw = self.pending.setdefault(e, ([], []))
            pr.extend(reads)
            pw.extend(writes)
            self._mark(e, self.cnt[e] + 1, reads, writes)
            return inst
        self.cnt[e] += 1
        inst.then_inc(self.sems[e], 1)
        if e in self.pending:
            self.pending.pop(e)
        self._mark(e, self.cnt[e], reads, writes)
        return inst

    def dma(self, q, out, in_, reads=(), writes=(), sb=None, **kw):
        if sb.dsem is None:
            sb.dsem = self.new_dma_sem()
        sem = sb.dsem
        self._wait(q, self._deps(reads, writes))
        inst = self.eng[q].dma_start(out=out, in_=in_, **kw)
        self.cnt[sem] += 16
        inst.then_inc(self.sems[sem], 16)
        self._mark(sem, self.cnt[sem], reads, writes)
        return inst

    def wait_all(self, e, bufs):
        self._wait(e, self._deps(bufs, bufs))

    def close(self):
        self.st.close()

EPS = 1e-6


def fm(v):
    return np.ascontiguousarray(np.asarray(v, np.float32).reshape(-1, 128).T)

def fmk(w):
    K, n = w.shape[0], w.shape[1] // 128
    return np.ascontiguousarray(np.asarray(w, np.float32).reshape(K, n, 128).transpose(2, 0, 1).reshape(128, K * n))

def pre_inputs(layer, xfull, mod, inp):
    m = mod[2 * layer]
    vec = np.concatenate([fm(m[:2048]), fm(m[2048:4096]), fm(inp["norm_g"][layer, 0])], axis=1)
    if layer == 0:
        w = inp["e_w_in"][0]
        glu_a = w[:, 4112:5136].reshape(2048, 8, 128); glu_b = w[:, 5136:6160].reshape(2048, 8, 128)
        glu = np.stack([glu_a, glu_b], axis=2).reshape(2048, 2048)
        win = np.concatenate([w[:, 0:3072], glu, w[:, 3072:4096], w[:, 4096:4112]], axis=1)
        convw = fmk(inp["e_conv_qkv"][0]); convb = np.zeros((128, 24), np.float32)
        extra = {"dw": fmk(inp["e_conf_dw"][0]),
                 "dvec": np.concatenate([fm(inp["e_conf_dw_b"][0]), fm(inp["e_conf_ln_g"][0]), fm(inp["e_conf_ln_b"][0])], axis=1),
                 "ident": np.eye(128, dtype=np.float32)}
    else:
        w = inp["o_w_in"][0]
        win = np.concatenate([w[:, 4096:10240], w[:, 0:4096], w[:, 10240:10304]], axis=1)
        convw = fmk(inp["o_conv_w"][0]); convb = fm(inp["o_conv_b"][0])
        extra = {}
    win = np.ascontiguousarray(win)
    xT = np.ascontiguousarray(xfull.T)
    maps = []
    for i in range(8):
        xs = np.zeros((2048, 1056), np.float32)
        lo = i * 1024 - 32
        if i == 0:
            xs[:, 32:] = xT[:, 0:1024]
        else:
            xs[:] = xT[:, lo:lo + 1056]
        hmask = np.full((128, 1), 0.0 if i == 0 else 1.0, np.float32)
        maps.append({"xT": xs, "vec": vec, "win": win, "convw": convw, "convb": convb, "hmask": hmask, **extra})
    return maps

def cmat():
    j = np.arange(128)[:, None]; i = np.arange(128)[None, :]
    same = (j // 64) == (i // 64)
    tri = ((j <= i) & same).astype(np.float32)
    blk = same.astype(np.float32)
    sel0 = np.broadcast_to(j < 64, (128, 128)).astype(np.float32)
    sel1 = np.broadcast_to(j >= 64, (128, 128)).astype(np.float32)
    return np.ascontiguousarray(np.concatenate([tri, blk, sel0, sel1], axis=1))

def ssd_inputs(pout, inp):
    maps = []
    cm = cmat()
    for g in range(8):
        xT = pout[g * 512:(g + 1) * 512]; zT = pout[6144 + g * 512:6144 + (g + 1) * 512]
        BT = np.ascontiguousarray(pout[4096 + g * 128:4096 + (g + 1) * 128]); CT = np.ascontiguousarray(pout[5120 + g * 128:5120 + (g + 1) * 128])
        dtr = pout[10240 + g * 8:10240 + (g + 1) * 8]
        dtr = np.ascontiguousarray(dtr.reshape(8, 64, 128).transpose(2, 1, 0).reshape(128, 512))
        hs = slice(g * 8, (g + 1) * 8)
        rv = np.concatenate([inp["o_dt_bias"][0][hs], inp["o_a_log"][0][hs], inp["o_d_skip"][0][hs]])[None, :]
        rv = np.ascontiguousarray(np.tile(rv, (128, 1)).astype(np.float32))
        maps.append({"x_tok": np.ascontiguousarray(xT.T), "z_tok": np.ascontiguousarray(zT.T), "BT": BT, "CT": CT,
                     "B_tok": np.ascontiguousarray(BT.T), "dtr": dtr, "rowvec": rv, "cmat": cm})
    return maps

def cmat_gdn():
    c = cmat()
    j = np.arange(128)[:, None]; i = np.arange(128)[None, :]
    masks = ((j > i) & ((j // 64) == (i // 64))).astype(np.float32)
    return np.ascontiguousarray(np.concatenate([c, np.eye(128, dtype=np.float32), masks], axis=1))

def gdn_inputs(pout, inp):
    cm = cmat_gdn()
    gn = np.ascontiguousarray(np.tile(inp["e_head_norm_g"][0][None, :], (128, 1)).astype(np.float32))
    maps = []
    for hd in range(8):
        r = slice(hd * 128, (hd + 1) * 128)
        qT = np.ascontiguousarray(pout[0:1024][r]); kT = np.ascontiguousarray(pout[1024:2048][r])
        vT = pout[2048:3072][r]; zT = pout[4096:5120][r]
        bl = np.ascontiguousarray(pout[5120 + hd].reshape(64, 128).T); al = np.ascontiguousarray(pout[5128 + hd].reshape(64, 128).T)
        rv = np.tile(np.array([[inp["e_a_log"][0][hd], inp["e_dt_bias"][0][hd]]], np.float32), (128, 1))
        maps.append({"qT": qT, "kT": kT, "k_tok": np.ascontiguousarray(kT.T), "v_tok": np.ascontiguousarray(vT.T),
                     "z_tok": np.ascontiguousarray(zT.T), "bl": bl, "al": al, "rowvec": np.ascontiguousarray(rv),
                     "gn": gn, "cmat": cm})
    return maps


def build_mod():
    nc = bass.Bass("TRN2", target_bir_lowering=False)
    P = Prog(nc)
    NCOL = 768
    c_d = P.dram("cT", [128, 16], F32, "ExternalInput")
    w_d = P.dram("wmod", [4, 2048, NCOL], F32, "ExternalInput")
    b_d = P.dram("bmod", [4, NCOL], F32, "ExternalInput")
    o_d = P.dram("mod", [4, NCOL], F32, "ExternalOutput")
    cs = P.sbuf([128, 16], F32, "cs")
    sc = P.sbuf([128, 16], F32, "sc")
    wb = [P.sbuf([128, 16, NCOL], F32, f"w{i}") for i in range(2)]
    bs = P.sbuf([1, 4 * NCOL], F32, "bs")
    os_ = P.sbuf([1, 4 * NCOL], F32, "os")
    bank = [P.psum([128, 512], F32, f"bank{i}") for i in range(2)]
    P.dma("sp", cs[:], c_d[:], writes=[cs], sb=cs)
    P.dma("sp", bs[:], b_d.t.rearrange("(o l) n -> o (l n)", o=1), writes=[bs], sb=bs)
    P.op("act", lambda e: e.activation(sc[:], cs[:], AF.Silu), reads=[cs], writes=[sc])
    k = 0
    for l in range(4):
        w = wb[l % 2]
        P.dma("sp" if l % 2 else "act", w[:], w_d[l].rearrange("(j p) n -> p j n", p=128), writes=[w], sb=w)
        for nh in range(2):
            bk = bank[k % 2]
            k += 1
            for j in range(16):
                P.op("pe", lambda e: e.matmul(bk[0:1, 0:384], sc[:, j:j + 1], w[:, j, nh * 384:(nh + 1) * 384],
                                              start=(j == 0), stop=(j == 15)),
                     reads=[sc, w], writes=[bk], defer=(j != 15))
            o = l * NCOL + nh * 384
            P.op("dve", lambda e: e.tensor_tensor(os_[0:1, o:o + 384], bk[0:1, 0:384], bs[0:1, o:o + 384], ALU.add),
                 reads=[bk, bs], writes=[os_])
    P.dma("sp", o_d.t.rearrange("(o l) n -> o (l n)", o=1), os_[:], reads=[os_], sb=os_)
    P.wait_all("sp", [os_])
    P.close()
    return nc


def build_pre(layer):
    NTK, HALO, NT = 1056, 32, 1024
    pieces = [(0, 32), (32, 544), (544, 1056)]
    if layer == 0:
        plan = [("conv", i) for i in range(24)] + [("glu", i) for i in range(16)] + \
               [("pass", i) for i in range(8)] + [("passp", 0)]
        NW, NOUT, NCONV, PW = 6160, 5136, 24, 16
        out_row = {"conv": 0, "glu": 3072, "pass": 4096, "passp": 5120}
    else:
        plan = [("conv", i) for i in range(48)] + [("pass", i) for i in range(32)] + [("passp", 0)]
        NW, NOUT, NCONV, PW = 10304, 10304, 48, 64
        out_row = {"conv": 0, "pass": 6144, "passp": 10240}
    nc = bass.Bass("TRN2", target_bir_lowering=False)
    P = Prog(nc)
    xT_d = P.dram("xT", [2048, NTK], F32, "ExternalInput")
    vec_d = P.dram("vec", [128, 48], F32, "ExternalInput")
    w_d = P.dram("win", [2048, NW], F32, "ExternalInput")
    cw_d = P.dram("convw", [128, 4 * NCONV], F32, "ExternalInput")
    cb_d = P.dram("convb", [128, NCONV], F32, "ExternalInput")
    hm_d = P.dram("hmask", [128, 1], F32, "ExternalInput")
    if layer == 0:
        dw_d = P.dram("dw", [128, 31 * 8], F32, "ExternalInput")
        dv_d = P.dram("dvec", [128, 24], F32, "ExternalInput")
        id_d = P.dram("ident", [128, 128], F32, "ExternalInput")
    out_d = P.dram("pout", [NOUT, NT], F32, "ExternalOutput")

    big = [P.sbuf([128, NTK], F32, f"big{i}") for i in range(16)]
    hT = [P.sbuf([128, NTK], BF16, f"h{j}") for j in range(16)]
    wsl = [P.sbuf([128, 16, 512], BF16, f"wsl{i}") for i in range(3)]
    rstd = P.sbuf([128, NTK], F32, "rstd")
    sqb = [P.sbuf([128, 512], BF16, f"sq{i}") for i in range(2)]
    tsm = [P.sbuf([128, 512], F32, f"tsm{i}") for i in range(2)]
    ones = P.sbuf([128, 128], F32, "ones")
    onesb = P.sbuf([128, 128], BF16, "onesb")
    vec = P.sbuf([128, 48], F32, "vec")
    gs = P.sbuf([128, 16], F32, "gs")
    cw = P.sbuf([128, 4 * NCONV], F32, "cw")
    cb = P.sbuf([128, NCONV], F32, "cb")
    hm = P.sbuf([128, 1], F32, "hm")
    bank = [P.psum([128, 512], F32, f"bank{i}") for i in range(8)]
    if layer == 0:
        dw = P.sbuf([128, 31 * 8], F32, "dw")
        dv = P.sbuf([128, 24], F32, "dv")
        identf = P.sbuf([128, 128], F32, "identf")
        identb = P.sbuf([128, 128], BF16, "identb")
        ub = P.sbuf([128, NTK], BF16, "ub")
        dg = [P.sbuf([128, 128], BF16, f"dg{i}") for i in range(4)]
        P.dma("sp", identf[:], id_d[:], writes=[identf], sb=identf)
        P.op("dve", lambda e: e.tensor_copy(identb[:], identf[:]), reads=[identf], writes=[identb])
        P.dma("sp", dw[:], dw_d[:], writes=[dw], sb=dw)
        P.dma("sp", dv[:], dv_d[:], writes=[dv], sb=dv)
    P.dma("sp", vec[:], vec_d[:], writes=[vec], sb=vec)
    P.dma("sp", cw[:], cw_d[:], writes=[cw], sb=cw)
    P.dma("sp", cb[:], cb_d[:], writes=[cb], sb=cb)
    P.dma("sp", hm[:], hm_d[:], writes=[hm], sb=hm)
    P.op("dve", lambda e: e.memset(ones[:], 1.0), writes=[ones])
    P.op("dve", lambda e: e.memset(onesb[:], 1.0), writes=[onesb])
    for j in range(16):
        P.dma("act" if j % 2 else "sp", big[j][:], xT_d[j * 128:(j + 1) * 128, :], writes=[big[j]], sb=big[j])

    P.op("dve", lambda e: e.tensor_scalar(gs[:], vec[:, 16:32], 1.0, None, ALU.add), reads=[vec], writes=[gs])
    P.op("dve", lambda e: e.tensor_tensor(gs[:], gs[:], vec[:, 32:48], ALU.mult), reads=[gs, vec], writes=[gs])
    for (lo, hi) in pieces:
        n = hi - lo
        for j in range(16):
            q = sqb[j % 2]
            P.op("act", lambda e: e.activation(q[:, 0:n], big[j][:, lo:hi], AF.Square), reads=[big[j]], writes=[q])
            P.op("pe", lambda e: e.matmul(bank[6][:, 0:n], onesb[:], q[:, 0:n], start=(j == 0), stop=(j == 15)),
                 reads=[onesb, q], writes=[bank[6]])
        t = tsm[0]
        P.op("act", lambda e: e.activation(t[:, 0:n], bank[6][:, 0:n], AF.Sqrt, bias=EPS, scale=1.0 / 2048),
             reads=[bank[6]], writes=[t])
        P.op("dve", lambda e: e.reciprocal(rstd[:, lo:hi], t[:, 0:n]), reads=[t], writes=[rstd])
    for j in range(16):
        P.op("dve", lambda e: e.tensor_tensor(big[j][:], big[j][:], rstd[:], ALU.mult),
             reads=[big[j], rstd], writes=[big[j]])
        P.op("dve", lambda e: e.tensor_scalar(hT[j][:], big[j][:], gs[:, j:j + 1], vec[:, j:j + 1], ALU.mult, ALU.add),
             reads=[big[j], gs, vec], writes=[hT[j]])

    rot = {"n": 0}
    if layer == 0:
        cv = big[0:8]
        pa = big[8]
        pool = big[9:16]
    else:
        pool = big

    def nextbuf():
        b = pool[rot["n"] % len(pool)]
        rot["n"] += 1
        return b

    nb = {"n": 0}
    wv = None
    for ci, (kind, idx) in enumerate(plan):
        blk = ci // 4
        if ci % 4 == 0:
            width = min(512, NW - blk * 512)
            ws = wsl[blk % 3]
            P.dma("pool", ws[:, :, 0:width], w_d[:, blk * 512:blk * 512 + width].rearrange("(j p) n -> p j n", p=128),
                  writes=[ws], sb=ws)
        off = (ci % 4) * 128
        wc = PW if kind == "passp" else 128
        halo = kind in ("conv", "glu")
        pr = pa if (kind == "glu" and idx % 2 == 0) else nextbuf()
        for pi, (lo, hi) in enumerate(pieces):
            if pi == 0 and not halo:
                continue
            n = hi - lo
            bk = bank[nb["n"] % 4]
            nb["n"] += 1
            for j in range(16):
                P.op("pe", lambda e: e.matmul(bk[0:wc, 0:n], ws[:, j, off:off + wc], hT[j][:, lo:hi],
                                              start=(j == 0), stop=(j == 15)),
                     reads=[ws, hT[j]], writes=[bk], defer=(j != 15))
            if pi == 0:
                P.op("act", lambda e: e.activation(pr[0:wc, lo:hi], bk[0:wc, 0:n], AF.Copy, scale=hm[0:wc, 0:1]),
                     reads=[bk, hm], writes=[pr])
            else:
                P.op("act", lambda e: e.activation(pr[0:wc, lo:hi], bk[0:wc, 0:n], AF.Copy), reads=[bk], writes=[pr])
        if kind == "conv":
            acc = nextbuf()
            P.op("dve", lambda e: e.tensor_scalar(acc[:, 0:NT], pr[:, 29:29 + NT], cw[:, idx:idx + 1], None, ALU.mult),
                 reads=[pr, cw], writes=[acc])
            for k in range(1, 4):
                P.op("dve", lambda e: e.scalar_tensor_tensor(acc[:, 0:NT], pr[:, 29 + k:29 + k + NT],
                                                             cw[:, k * NCONV + idx:k * NCONV + idx + 1], acc[:, 0:NT],
                                                             ALU.mult, ALU.add),
                     reads=[pr, cw, acc], writes=[acc])
            P.op("act", lambda e: e.activation(acc[:, 0:NT], acc[:, 0:NT], AF.Silu, bias=cb[:, idx:idx + 1], scale=1.0),
                 reads=[acc, cb], writes=[acc])
            if layer == 0 and idx < 16:
                qs = 128.0 ** -0.5 if idx < 8 else 1.0
                for hh in range(2):
                    sl = slice(hh * 512, (hh + 1) * 512)
                    q = sqb[hh]
                    P.op("act", lambda e: e.activation(q[:], acc[:, sl], AF.Square), reads=[acc], writes=[q])
                    P.op("pe", lambda e: e.matmul(bank[6][:], onesb[:], q[:], start=True, stop=True),
                         reads=[onesb, q], writes=[bank[6]])
                    t = tsm[hh]
                    P.op("act", lambda e: e.activation(t[:], bank[6][:], AF.Sqrt, bias=EPS, scale=1.0),
                         reads=[bank[6]], writes=[t])
                    P.op("dve", lambda e: e.reciprocal(t[:], t[:]), reads=[t], writes=[t])
                    P.op("dve", lambda e: e.scalar_tensor_tensor(acc[:, sl], acc[:, sl], qs, t[:], ALU.mult, ALU.mult),
                         reads=[acc, t], writes=[acc])
            r0 = out_row["conv"] + idx * 128
            P.dma("sp" if ci % 2 else "act", out_d[r0:r0 + 128, :], acc[:, 0:NT], reads=[acc], sb=acc)
        elif kind == "glu":
            if idx % 2 == 0:
                continue
            c = idx // 2
            P.op("act", lambda e: e.activation(pr[:], pr[:], AF.Sigmoid), reads=[pr], writes=[pr])
            P.op("dve", lambda e: e.tensor_tensor(ub[:], pr[:], pa[:], ALU.mult), reads=[pr, pa], writes=[ub])
            acc = cv[c]
            for k in range(31):
                d_ = dg[k % 4]
                P.op("dve", lambda e: e.tensor_scalar(d_[:], identb[:], dw[:, k * 8 + c:k * 8 + c + 1], None, ALU.mult),
                     reads=[identb, dw], writes=[d_])
                for hh in range(2):
                    o = 2 + k + hh * 512
                    P.op("pe", lambda e: e.matmul(bank[4 + hh][:], d_[:], ub[:, o:o + 512], start=(k == 0), stop=(k == 30)),
                         reads=[d_, ub], writes=[bank[4 + hh]])
            for hh in range(2):
                P.op("act", lambda e: e.activation(acc[:, hh * 512:(hh + 1) * 512], bank[4 + hh][:], AF.Identity,
                                                   bias=dv[:, c:c + 1], scale=1.0),
                     reads=[bank[4 + hh], dv], writes=[acc])
            if c == 7:
                for hh in range(2):
                    sl = slice(hh * 512, (hh + 1) * 512)
                    for c2 in range(8):
                        P.op("pe", lambda e: e.matmul(bank[6][:], ones[:], cv[c2][:, sl], start=(c2 == 0), stop=(c2 == 7)),
                             reads=[ones, cv[c2]], writes=[bank[6]], defer=(c2 != 7))
                    mb = tsm[0]
                    P.op("act", lambda e: e.activation(mb[:], bank[6][:], AF.Copy, scale=1.0 / 1024),
                         reads=[bank[6]], writes=[mb])
                    for c2 in range(8):
                        P.op("dve", lambda e: e.tensor_tensor(cv[c2][:, sl], cv[c2][:, sl], mb[:], ALU.subtract),
                             reads=[cv[c2], mb], writes=[cv[c2]])
                        q = sqb[c2 % 2]
                        P.op("act", lambda e: e.activation(q[:], cv[c2][:, sl], AF.Square), reads=[cv[c2]], writes=[q])
                        P.op("pe", lambda e: e.matmul(bank[7][:], onesb[:], q[:], start=(c2 == 0), stop=(c2 == 7)),
                             reads=[onesb, q], writes=[bank[7]])
                    t = tsm[1]
                    P.op("act", lambda e: e.activation(t[:], bank[7][:], AF.Sqrt, bias=EPS, scale=1.0 / 1024),
                         reads=[bank[7]], writes=[t])
                    P.op("dve", lambda e: e.reciprocal(t[:], t[:]), reads=[t], writes=[t])
                    for c2 in range(8):
                        P.op("dve", lambda e: e.tensor_tensor(cv[c2][:, sl], cv[c2][:, sl], t[:], ALU.mult),
                             reads=[cv[c2], t], writes=[cv[c2]])
                        P.op("act", lambda e: e.activation(cv[c2][:, sl], cv[c2][:, sl], AF.Silu,
                                                           bias=dv[:, 16 + c2:17 + c2], scale=dv[:, 8 + c2:9 + c2]),
                             reads=[cv[c2], dv], writes=[cv[c2]])
                for c2 in range(8):
                    r0 = out_row["glu"] + c2 * 128
                    P.dma("sp" if c2 % 2 else "act", out_d[r0:r0 + 128, :], cv[c2][:, 0:NT], reads=[cv[c2]], sb=cv[c2])
        else:
            r0 = out_row[kind] + idx * 128
            P.dma("sp" if ci % 2 else "act", out_d[r0:r0 + wc, :], pr[0:wc, HALO:NTK], reads=[pr], sb=pr)
    allb = big
    P.wait_all("sp", allb)
    P.wait_all("act", allb)
    P.close()
    return nc


def build_gdn():
    T, NTILE, GT = 8192, 64, 4
    nc = bass.Bass("TRN2", target_bir_lowering=False)
    P = Prog(nc)
    qT_d = P.dram("qT", [128, T], F32, "ExternalInput")
    kT_d = P.dram("kT", [128, T], F32, "ExternalInput")
    kk_d = P.dram("k_tok", [T, 128], F32, "ExternalInput")
    vk_d = P.dram("v_tok", [T, 128], F32, "ExternalInput")
    zk_d = P.dram("z_tok", [T, 128], F32, "ExternalInput")
    bl_d = P.dram("bl", [128, 64], F32, "ExternalInput")
    al_d = P.dram("al", [128, 64], F32, "ExternalInput")
    rv_d = P.dram("rowvec", [128, 2], F32, "ExternalInput")
    gn_d = P.dram("gn", [128, 128], F32, "ExternalInput")
    cm_d = P.dram("cmat", [128, 768], F32, "ExternalInput")
    out_d = P.dram("o_tok", [T, 128], F32, "ExternalOutput")

    cm = P.sbuf([128, 768], F32, "cm")
    TRI, BLK, SEL0, SEL1, IDENT, MASKS = (cm[:, i * 128:(i + 1) * 128] for i in range(6))
    rv = P.sbuf([128, 2], F32, "rv")
    gn = P.sbuf([128, 128], F32, "gn")
    names = ("bl", "al", "beta", "xb", "ax", "ex", "ln", "sp", "g", "gcs", "gtot", "eg", "kdsc", "beg",
             "dec0", "dec1", "aneg")
    S = {k: P.sbuf([128, 64], F32, "s_" + k) for k in names}
    St = [P.sbuf([128, 128], F32, f"St{i}") for i in range(2)]
    qTg = [P.sbuf([128, 512], F32, f"qTg{i}") for i in range(2)]
    kTg = [P.sbuf([128, 512], F32, f"kTg{i}") for i in range(2)]
    kkg = [P.sbuf([128, GT, 128], F32, f"kkg{i}") for i in range(2)]
    vkg = [P.sbuf([128, GT, 128], F32, f"vkg{i}") for i in range(2)]
    zkg = [P.sbuf([128, GT, 128], F32, f"zkg{i}") for i in range(2)]
    og = [P.sbuf([128, GT, 128], F32, f"og{i}") for i in range(2)]
    W = {}
    for gi in range(GT):
        for k in ("Y", "YT", "decn", "dect", "erb", "t", "N", "M", "Na", "Nb", "Ma", "Mb", "QKm", "qeg", "kd",
                  "kcdT", "u", "o", "sz", "rbs"):
            W[k, gi] = P.sbuf([128, 128], F32, f"w_{k}{gi}")
        for k in ("R0", "R1"):
            W[k, gi] = P.sbuf([128, 256], F32, f"w_{k}{gi}")
        for k in ("ss", "rs"):
            W[k, gi] = P.sbuf([128, 1], F32, f"w_{k}{gi}")
    bkA = [P.psum([128, 512], F32, f"bkA{i}") for i in range(GT)]
    bkB = [P.psum([128, 512], F32, f"bkB{i}") for i in range(GT)]
    def vw(b, lo, n):
        return View(b, b[:, lo:lo + n])
    A_q = [[vw(bkA[gi], q * 128, 128) for q in range(4)] for gi in range(GT)]
    A_h = [[vw(bkA[gi], h * 256, 256) for h in range(2)] for gi in range(GT)]
    B_q = [[vw(bkB[gi], q * 128, 128) for q in range(4)] for gi in range(GT)]
    pq = [B_q[0][0]]

    P.dma("sp", cm[:], cm_d[:], writes=[cm], sb=cm)
    P.dma("sp", rv[:], rv_d[:], writes=[rv], sb=rv)
    P.dma("sp", gn[:], gn_d[:], writes=[gn], sb=gn)
    P.dma("sp", S["bl"][:], bl_d[:], writes=[S["bl"]], sb=S["bl"])
    P.dma("sp", S["al"][:], al_d[:], writes=[S["al"]], sb=S["al"])

    D = lambda fn, r, w: P.op("dve", fn, reads=r, writes=w)
    A = lambda fn, r, w: P.op("act", fn, reads=r, writes=w)
    G = lambda fn, r, w: P.op("dve", fn, reads=r, writes=w)
    PE = lambda fn, r, w: P.op("pe", fn, reads=r, writes=w)

    A(lambda e: e.activation(S["beta"][:], S["bl"][:], AF.Sigmoid), [S["bl"]], [S["beta"]])
    D(lambda e: e.tensor_scalar(S["xb"][:], S["al"][:], rv[:, 1:2], None, ALU.add), [S["al"], rv], [S["xb"]])
    A(lambda e: e.activation(S["ax"][:], S["xb"][:], AF.Abs), [S["xb"]], [S["ax"]])
    A(lambda e: e.activation(S["ex"][:], S["ax"][:], AF.Exp, scale=-1.0), [S["ax"]], [S["ex"]])
    A(lambda e: e.activation(S["ln"][:], S["ex"][:], AF.Ln, bias=1.0, scale=1.0), [S["ex"]], [S["ln"]])
    D(lambda e: e.scalar_tensor_tensor(S["sp"][:], S["xb"][:], 0.0, S["ln"][:], ALU.max, ALU.add),
      [S["xb"], S["ln"]], [S["sp"]])
    A(lambda e: e.activation(S["aneg"][:, 0:1], rv[:, 0:1], AF.Exp), [rv], [S["aneg"]])
    D(lambda e: e.tensor_scalar(S["g"][:], S["sp"][:], S["aneg"][:, 0:1], -1.0, ALU.mult, ALU.mult),
      [S["sp"], S["aneg"]], [S["g"]])
    for (dst, mat, fn) in (("gcs", TRI, AF.Copy), ("gtot", BLK, AF.Copy), ("dec0", SEL0, AF.Exp), ("dec1", SEL1, AF.Exp)):
        PE(lambda e: e.matmul(pq[0][:, 0:64], mat, S["g"][:], start=True, stop=True), [cm, S["g"]], [pq[0]])
        A(lambda e: e.activation(S[dst][:], pq[0][:, 0:64], fn), [pq[0]], [S[dst]])
    A(lambda e: e.activation(S["eg"][:], S["gcs"][:], AF.Exp), [S["gcs"]], [S["eg"]])
    D(lambda e: e.tensor_tensor(S["kdsc"][:], S["gtot"][:], S["gcs"][:], ALU.subtract), [S["gtot"], S["gcs"]], [S["kdsc"]])
    A(lambda e: e.activation(S["kdsc"][:], S["kdsc"][:], AF.Exp), [S["kdsc"]], [S["kdsc"]])
    D(lambda e: e.tensor_tensor(S["beg"][:], S["beta"][:], S["eg"][:], ALU.mult), [S["beta"], S["eg"]], [S["beg"]])
    D(lambda e: e.memset(St[0][:], 0.0), [], [St[0]])

    def load_group(g):
        i = g % 2
        cs = slice(g * 512, (g + 1) * 512)
        P.dma("sp", qTg[i][:], qT_d[:, cs], writes=[qTg[i]], sb=qTg[i])
        P.dma("act", kTg[i][:], kT_d[:, cs], writes=[kTg[i]], sb=kTg[i])
        P.dma("sp", kkg[i][:], kk_d[cs, :].rearrange("(n p) d -> p n d", p=128), writes=[kkg[i]], sb=kkg[i])
        P.dma("act", vkg[i][:], vk_d[cs, :].rearrange("(n p) d -> p n d", p=128), writes=[vkg[i]], sb=vkg[i])
        P.dma("sp", zkg[i][:], zk_d[cs, :].rearrange("(n p) d -> p n d", p=128), writes=[zkg[i]], sb=zkg[i])

    NG = NTILE // GT
    load_group(0)
    cur = 0
    for g in range(NG):
        if g + 1 < NG:
            load_group(g + 1)
        i = g % 2
        tiles = range(GT)
        col = lambda gi: slice(g * GT + gi, g * GT + gi + 1)
        kT = lambda gi: kTg[i][:, gi * 128:(gi + 1) * 128]
        qT = lambda gi: qTg[i][:, gi * 128:(gi + 1) * 128]
        qa = lambda gi: A_q[gi][0]
        qb = lambda gi: A_q[gi][1]
        qc = lambda gi: A_q[gi][2]
        qd = lambda gi: B_q[gi][3]
        for gi in tiles:
            PE(lambda e: e.matmul(qa(gi)[:], S["g"][:, col(gi)].broadcast_to([128, 128]), TRI, start=True, stop=True),
               [S["g"], cm], [qa(gi)])
            PE(lambda e: e.matmul(qb(gi)[:], kT(gi), kT(gi), start=True, stop=True), [kTg[i]], [qb(gi)])
            PE(lambda e: e.matmul(qc(gi)[:], kT(gi), qT(gi), start=True, stop=True), [kTg[i], qTg[i]], [qc(gi)])
        for gi in tiles:
            w = lambda k: W[k, gi]
            gc_ = S["gcs"][:, col(gi)]
            D(lambda e: e.tensor_copy(w("rbs")[:], qa(gi)[:]), [qa(gi)], [w("rbs")])
            D(lambda e: e.tensor_scalar(w("Y")[:], w("rbs")[:], gc_, 0.0, ALU.subtract, ALU.max), [w("rbs"), S["gcs"]], [w("Y")])
            D(lambda e: e.tensor_scalar(w("YT")[:], w("rbs")[:], gc_, 0.0, ALU.subtract, ALU.min), [w("rbs"), S["gcs"]], [w("YT")])
        for gi in tiles:
            w = lambda k: W[k, gi]
            A(lambda e: e.activation(w("decn")[:], w("Y")[:], AF.Exp, scale=-1.0), [w("Y")], [w("decn")])
            A(lambda e: e.activation(w("dect")[:], w("YT")[:], AF.Exp), [w("YT")], [w("dect")])
            A(lambda e: e.activation(w("erb")[:], w("rbs")[:], AF.Exp), [w("rbs")], [w("erb")])
        for gi in tiles:
            w = lambda k: W[k, gi]
            G(lambda e: e.tensor_scalar(w("R0")[:, 0:128], vkg[i][:, gi, :], S["beta"][:, col(gi)], None, ALU.mult),
              [vkg[i], S["beta"]], [w("R0")])
            G(lambda e: e.tensor_scalar(w("R0")[:, 128:256], kkg[i][:, gi, :], S["beg"][:, col(gi)], None, ALU.mult),
              [kkg[i], S["beg"]], [w("R0")])
        for gi in tiles:
            w = lambda k: W[k, gi]
            D(lambda e: e.tensor_tensor(w("t")[:], qb(gi)[:], w("decn")[:], ALU.mult), [qb(gi), w("decn")], [w("t")])
            D(lambda e: e.scalar_tensor_tensor(w("N")[:], w("t")[:], S["beta"][:, col(gi)], MASKS, ALU.mult, ALU.mult),
              [w("t"), S["beta"], cm], [w("N")])
        for gi in tiles:
            PE(lambda e: e.matmul(qd(gi)[:], W["N", gi][:], IDENT, start=True, stop=True), [W["N", gi], cm], [qd(gi)])
        for gi in tiles:
            w = lambda k: W[k, gi]
            G(lambda e: e.tensor_tensor(w("dect")[:], w("dect")[:], TRI, ALU.mult), [w("dect"), cm], [w("dect")])
            D(lambda e: e.tensor_tensor(w("QKm")[:], qc(gi)[:], w("dect")[:], ALU.mult), [qc(gi), w("dect")], [w("QKm")])
            G(lambda e: e.tensor_tensor(w("qeg")[:], qT(gi), w("erb")[:], ALU.mult), [qTg[i], w("erb")], [w("qeg")])
            G(lambda e: e.tensor_scalar(w("kd")[:], kkg[i][:, gi, :], S["kdsc"][:, col(gi)], None, ALU.mult),
              [kkg[i], S["kdsc"]], [w("kd")])
        for gi in tiles:
            A(lambda e: e.activation(W["M", gi][:], qd(gi)[:], AF.Copy), [qd(gi)], [W["M", gi]])
        Mc = {gi: W["M", gi] for gi in tiles}
        Nc = {gi: W["N", gi] for gi in tiles}
        Rc = {gi: W["R0", gi] for gi in tiles}
        for lvl in range(6):
            sign = ALU.subtract if lvl == 0 else ALU.add
            apb = (lambda gi: A_h[gi][0]) if lvl % 2 == 0 else (lambda gi: A_h[gi][1])
            pm = (lambda gi: B_q[gi][0]) if lvl % 2 == 0 else (lambda gi: B_q[gi][2])
            pn = (lambda gi: B_q[gi][1]) if lvl % 2 == 0 else (lambda gi: B_q[gi][3])
            for gi in tiles:
                PE(lambda e: e.matmul(apb(gi)[:], Mc[gi][:], Rc[gi][:], start=True, stop=True), [Mc[gi], Rc[gi]], [apb(gi)])
                if lvl < 5:
                    PE(lambda e: e.matmul(pm(gi)[:], Nc[gi][:], Mc[gi][:], start=True, stop=True), [Nc[gi], Mc[gi]], [pm(gi)])
                if lvl < 4:
                    PE(lambda e: e.matmul(pn(gi)[:], Mc[gi][:], Nc[gi][:], start=True, stop=True), [Nc[gi], Mc[gi]], [pn(gi)])
            for gi in tiles:
                Rn = W["R1", gi] if Rc[gi] is W["R0", gi] else W["R0", gi]
                D(lambda e: e.tensor_tensor(Rn[:], Rc[gi][:], apb(gi)[:], sign), [Rc[gi], apb(gi)], [Rn])
                Rc[gi] = Rn
                if lvl < 5:
                    Mn = W["Ma", gi] if lvl % 2 == 0 else W["Mb", gi]
                    A(lambda e: e.activation(Mn[:], pm(gi)[:], AF.Copy), [pm(gi)], [Mn])
                if lvl < 4:
                    Nn = W["Na", gi] if lvl % 2 == 0 else W["Nb", gi]
                    A(lambda e: e.activation(Nn[:], pn(gi)[:], AF.Copy), [pn(gi)], [Nn])
                if lvl < 5:
                    Mc[gi] = Mn
                if lvl < 4:
                    Nc[gi] = Nn
        for gi in tiles:
            PE(lambda e: e.matmul(B_q[gi][0][:], Rc[gi][:, 128:256], IDENT, start=True, stop=True), [Rc[gi], cm], [B_q[gi][0]])
        for gi in tiles:
            A(lambda e: e.activation(W["kcdT", gi][:], B_q[gi][0][:], AF.Copy), [B_q[gi][0]], [W["kcdT", gi]])
        for gi in tiles:
            w = lambda k: W[k, gi]
            for c in range(2):
                lo, hi = c * 64, (c + 1) * 64
                s_old, s_new = St[cur], St[1 - cur]
                PE(lambda e: e.matmul(A_q[gi][0][:], w("kcdT")[:], s_old[:], start=True, stop=True), [w("kcdT"), s_old], [A_q[gi][0]])
                D(lambda e: e.tensor_tensor(w("u")[lo:hi, :], Rc[gi][lo:hi, 0:128], A_q[gi][0][lo:hi, :], ALU.subtract),
                  [Rc[gi], A_q[gi][0]], [w("u")])
                PE(lambda e: e.matmul(A_q[gi][1][:], w("kd")[lo:hi, :], w("u")[lo:hi, :], start=True, stop=True),
                   [w("kd"), w("u")], [A_q[gi][1]])
                P.op("pe", lambda e: e.matmul(B_q[gi][1][:], w("qeg")[:], s_old[:], start=True, stop=False),
                     reads=[w("qeg"), s_old], writes=[B_q[gi][1]], defer=True)
                PE(lambda e: e.matmul(B_q[gi][1][:], w("QKm")[lo:hi, :], w("u")[lo:hi, :], start=False, stop=True),
                   [w("QKm"), w("u")], [B_q[gi][1]])
                dec = S["dec0"] if c == 0 else S["dec1"]
                D(lambda e: e.scalar_tensor_tensor(s_new[:], s_old[:], dec[:, col(gi)], A_q[gi][1][:], ALU.mult, ALU.add),
                  [s_old, dec, A_q[gi][1]], [s_new])
                A(lambda e: e.activation(w("o")[lo:hi, :], B_q[gi][1][lo:hi, :], AF.Copy), [B_q[gi][1]], [w("o")])
                cur = 1 - cur
            A(lambda e: e.activation(w("sz")[:], w("o")[:], AF.Square, accum_out=w("ss")[:]), [w("o")], [w("sz"), w("ss")])
            A(lambda e: e.activation(w("rs")[:], w("ss")[:], AF.Sqrt, bias=EPS, scale=1.0 / 128), [w("ss")], [w("rs")])
            D(lambda e: e.reciprocal(w("rs")[:], w("rs")[:]), [w("rs")], [w("rs")])
            D(lambda e: e.scalar_tensor_tensor(w("o")[:], w("o")[:], w("rs")[:, 0:1], gn[:], ALU.mult, ALU.mult),
              [w("o"), w("rs"), gn], [w("o")])
            A(lambda e: e.activation(w("sz")[:], zkg[i][:, gi, :], AF.Silu), [zkg[i]], [w("sz")])
            D(lambda e: e.tensor_tensor(og[i][:, gi, :], w("o")[:], w("sz")[:], ALU.mult), [w("o"), w("sz")], [og[i]])
        P.dma("sp", out_d[g * 512:(g + 1) * 512, :].rearrange("(n p) d -> p n d", p=128), og[i][:],
              reads=[og[i]], sb=og[i])
    P.wait_all("sp", og)
    P.close()
    return nc


def build_ssd():
    T, NTILE, H, PD, NS = 8192, 64, 8, 64, 128
    nc = bass.Bass("TRN2", target_bir_lowering=False)
    P = Prog(nc)
    x_d = P.dram("x_tok", [T, 512], F32, "ExternalInput")
    z_d = P.dram("z_tok", [T, 512], F32, "ExternalInput")
    bt_d = P.dram("BT", [128, T], F32, "ExternalInput")
    ct_d = P.dram("CT", [128, T], F32, "ExternalInput")
    bk_d = P.dram("B_tok", [T, 128], F32, "ExternalInput")
    dtr_d = P.dram("dtr", [128, 512], F32, "ExternalInput")
    rv_d = P.dram("rowvec", [128, 24], F32, "ExternalInput")
    cm_d = P.dram("cmat", [128, 512], F32, "ExternalInput")
    out_d = P.dram("yz_tok", [T, 512], F32, "ExternalOutput")

    cm = P.sbuf([128, 512], F32, "cm")
    TRI, BLK, SEL0, SEL1 = (cm[:, i * 128:(i + 1) * 128] for i in range(4))
    rv = P.sbuf([128, 24], F32, "rv")
    dtr = P.sbuf([128, 512], F32, "dtr")
    names = ("xb", "ax", "ex", "ln", "dt", "da", "acs", "atot", "eacs", "dte", "dec0", "dec1", "aneg", "nacs")
    S = {k: P.sbuf([128, 512], F32, "s_" + k) for k in names}
    ST = [P.sbuf([128, 512], F32, f"ST{i}") for i in range(2)]
    NB = 3
    xt = [P.sbuf([128, 512], F32, f"xt{i}") for i in range(NB)]
    zt = [P.sbuf([128, 512], F32, f"zt{i}") for i in range(NB)]
    btt = [P.sbuf([128, 128], BF16, f"bt{i}") for i in range(NB)]
    ctt = [P.sbuf([128, 128], BF16, f"ct{i}") for i in range(NB)]
    bkt = [P.sbuf([128, 128], BF16, f"bk{i}") for i in range(NB)]
    xdd = [P.sbuf([128, 512], BF16, f"xdd{i}") for i in range(2)]
    xdt = [P.sbuf([128, 512], BF16, f"xdt{i}") for i in range(2)]
    cbm = [P.sbuf([128, 128], F32, f"cbm{i}") for i in range(2)]
    xx = [P.sbuf([128, 128], F32, f"xx{i}") for i in range(8)]
    mt = [P.sbuf([128, 128], BF16, f"mt{i}") for i in range(8)]
    STb = [P.sbuf([128, 512], BF16, f"STb{i}") for i in range(2)]
    yt = [P.sbuf([128, 512], F32, f"yt{i}") for i in range(2)]
    uo = [P.sbuf([128, 512], F32, f"uo{i}") for i in range(2)]
    bank = [P.psum([128, 512], F32, f"bank{i}") for i in range(8)]

    rbv = [View(bank[4 + h // 4], bank[4 + h // 4][:, (h % 4) * 128:(h % 4 + 1) * 128]) for h in range(8)]
    csbk = [bank[7], bank[1]]
    P.dma("sp", cm[:], cm_d[:], writes=[cm], sb=cm)
    P.dma("sp", rv[:], rv_d[:], writes=[rv], sb=rv)
    P.dma("sp", dtr[:], dtr_d[:], writes=[dtr], sb=dtr)

    def bc8(ap):
        return ap.unsqueeze(2).broadcast_to([ap.shape[0], 8, 64])

    def v3(ap):
        return ap.rearrange("p (h d) -> p h d", d=64)

    def rep(ap):
        return ap.unsqueeze(1).broadcast_to([128, 64, 8])

    def t3(ap):
        return ap.rearrange("p (n h) -> p n h", h=8)

    D = lambda fn, r, w: P.op("dve", fn, reads=r, writes=w)
    A = lambda fn, r, w: P.op("act", fn, reads=r, writes=w)
    D(lambda e: e.tensor_tensor(t3(S["xb"][:]), t3(dtr[:]), rep(rv[:, 0:8]), ALU.add), [dtr, rv], [S["xb"]])
    A(lambda e: e.activation(S["ax"][:], S["xb"][:], AF.Abs), [S["xb"]], [S["ax"]])
    A(lambda e: e.activation(S["ex"][:], S["ax"][:], AF.Exp, scale=-1.0), [S["ax"]], [S["ex"]])
    A(lambda e: e.activation(S["ln"][:], S["ex"][:], AF.Ln, bias=1.0, scale=1.0), [S["ex"]], [S["ln"]])
    D(lambda e: e.scalar_tensor_tensor(S["dt"][:], S["xb"][:], 0.0, S["ln"][:], ALU.max, ALU.add),
      [S["xb"], S["ln"]], [S["dt"]])
    A(lambda e: e.activation(S["aneg"][:, 0:8], rv[:, 8:16], AF.Exp), [rv], [S["aneg"]])
    D(lambda e: e.tensor_tensor(t3(S["da"][:]), t3(S["dt"][:]), rep(S["aneg"][:, 0:8]), ALU.mult),
      [S["dt"], S["aneg"]], [S["da"]])
    D(lambda e: e.tensor_scalar(S["da"][:], S["da"][:], -1.0, None, ALU.mult), [S["da"]], [S["da"]])
    for (dst, mat) in (("acs", TRI), ("atot", BLK), ("dec0", SEL0), ("dec1", SEL1)):
        P.op("pe", lambda e: e.matmul(bank[0][:], mat, S["da"][:], start=True, stop=True),
             reads=[cm, S["da"]], writes=[bank[0]])
        if dst in ("dec0", "dec1"):
            A(lambda e: e.activation(S[dst][:], bank[0][:], AF.Exp), [bank[0]], [S[dst]])
        else:
            A(lambda e: e.activation(S[dst][:], bank[0][:], AF.Copy), [bank[0]], [S[dst]])
    A(lambda e: e.activation(S["eacs"][:], S["acs"][:], AF.Exp), [S["acs"]], [S["eacs"]])
    A(lambda e: e.activation(S["nacs"][:], S["acs"][:], AF.Copy, scale=-1.0), [S["acs"]], [S["nacs"]])
    D(lambda e: e.tensor_tensor(S["dte"][:], S["atot"][:], S["acs"][:], ALU.subtract), [S["atot"], S["acs"]], [S["dte"]])
    A(lambda e: e.activation(S["dte"][:], S["dte"][:], AF.Exp), [S["dte"]], [S["dte"]])
    D(lambda e: e.tensor_tensor(S["dte"][:], S["dte"][:], S["dt"][:], ALU.mult), [S["dte"], S["dt"]], [S["dte"]])
    D(lambda e: e.memset(ST[0][:], 0.0), [], [ST[0]])
    D(lambda e: e.memset(STb[0][:], 0.0), [], [STb[0]])

    def load_tile(n):
        i = n % NB
        r = slice(n * 128, (n + 1) * 128)
        P.dma("sp", xt[i][:], x_d[r, :], writes=[xt[i]], sb=xt[i])
        P.dma("act", zt[i][:], z_d[r, :], writes=[zt[i]], sb=zt[i])
        P.dma("pool", btt[i][:], bt_d[:, r], writes=[btt[i]], sb=btt[i])
        P.dma("pool", ctt[i][:], ct_d[:, r], writes=[ctt[i]], sb=ctt[i])
        P.dma("pool", bkt[i][:], bk_d[r, :], writes=[bkt[i]], sb=bkt[i])

    G = lambda fn, r, w: P.op("pool", fn, reads=r, writes=w)
    load_tile(0)
    load_tile(1)
    state = {"cur": 0}

    def front(n):
        i = n % NB
        X, BT, CT = xt[i], btt[i], ctt[i]
        sc = slice(n * 8, (n + 1) * 8)
        xd, xe, y = xdt[n % 2], xdd[n % 2], yt[n % 2]
        G(lambda e: e.tensor_tensor(v3(xd[:]), v3(X[:]), bc8(S["dt"][:, sc]), ALU.mult), [X, S["dt"]], [xd])
        G(lambda e: e.tensor_tensor(v3(xe[:]), v3(X[:]), bc8(S["dte"][:, sc]), ALU.mult), [X, S["dte"]], [xe])
        G(lambda e: e.tensor_tensor(v3(y[:]), v3(X[:]), bc8(rv[:, 16:24]), ALU.mult), [X, rv], [y])
        P.op("pe", lambda e: e.matmul(bank[0][:, 0:128], BT[:], CT[:], start=True, stop=True),
             reads=[BT, CT], writes=[bank[0]])
        cb = cbm[n % 2]
        D(lambda e: e.tensor_tensor(cb[:], bank[0][:, 0:128], TRI, ALU.mult), [bank[0], cm], [cb])
        for h in range(H):
            col = n * 8 + h
            rb = rbv[h]
            P.op("pe", lambda e: e.matmul(rb[:], S["da"][:, col:col + 1].broadcast_to([128, 128]), TRI,
                                          start=True, stop=True),
                 reads=[S["da"], cm], writes=[rb])

    def middle(n):
        i = n % NB
        BK = bkt[i]
        xd, xe, cb = xdt[n % 2], xdd[n % 2], cbm[n % 2]
        yb = bank[2 + n % 2]
        for c in range(2):
            lo, hi = c * 64, (c + 1) * 64
            P.op("pe", lambda e: e.matmul(csbk[c][:], BK[lo:hi, :], xe[lo:hi, :], start=True, stop=True),
                 reads=[BK, xe], writes=[csbk[c]])
        for h in range(H):
            col = n * 8 + h
            rb = rbv[h]
            x_ = xx[h]
            A(lambda e: e.activation(x_[:], rb[:], AF.Exp, bias=S["nacs"][:, col:col + 1], scale=1.0),
              [rb, S["nacs"]], [x_])
        for h in range(H):
            x_ = xx[h]
            m_ = mt[h]
            D(lambda e: e.scalar_tensor_tensor(m_[:], x_[:], 1.0, cb[:], ALU.min, ALU.mult), [x_, cb], [m_])
        for h in range(H):
            m_ = mt[h]
            P.op("pe", lambda e: e.matmul(yb[:, h * 64:(h + 1) * 64], m_[:], xd[:, h * 64:(h + 1) * 64],
                                          start=True, stop=True),
                 reads=[m_, xd], writes=[yb])

    def scan(n):
        i = n % NB
        Z, CT = zt[i], ctt[i]
        sc = slice(n * 8, (n + 1) * 8)
        y = yt[n % 2]
        yb = bank[2 + n % 2]
        D(lambda e: e.tensor_tensor(y[:], y[:], yb[:], ALU.add), [y, yb], [y])
        for c in range(2):
            lo, hi = c * 64, (c + 1) * 64
            s_old, s_new = ST[state["cur"]], ST[1 - state["cur"]]
            yo = bank[6]
            sb_old, sb_new = STb[state["cur"]], STb[1 - state["cur"]]
            P.op("pe", lambda e: e.matmul(yo[:], CT[:], sb_old[:], start=True, stop=True),
                 reads=[CT, sb_old], writes=[yo])
            csb = csbk[c]
            u = uo[c]
            dec = S["dec0"] if c == 0 else S["dec1"]
            D(lambda e: e.tensor_tensor(v3(s_new[:]), v3(s_old[:]), bc8(dec[:, sc]), ALU.mult), [s_old, dec], [s_new])
            D(lambda e: e.tensor_tensor(s_new[:], s_new[:], csb[:], ALU.add), [s_new, csb], [s_new])
            A(lambda e: e.activation(sb_new[:], s_new[:], AF.Copy), [s_new], [sb_new])
            D(lambda e: e.tensor_tensor(v3(u[lo:hi, :]), v3(yo[lo:hi, :]), bc8(S["eacs"][lo:hi, sc]), ALU.mult),
              [yo, S["eacs"]], [u])
            G(lambda e: e.tensor_tensor(y[lo:hi, :], y[lo:hi, :], u[lo:hi, :], ALU.add), [y, u], [y])
            state["cur"] = 1 - state["cur"]
        A(lambda e: e.activation(Z[:], Z[:], AF.Silu), [Z], [Z])
        D(lambda e: e.tensor_tensor(y[:], y[:], Z[:], ALU.mult), [y, Z], [y])
        P.dma("sp", out_d[n * 128:(n + 1) * 128, :], y[:], reads=[y], sb=y)

    front(0)
    for n in range(NTILE):
        if n + 2 < NTILE:
            load_tile(n + 2)
        middle(n)
        if n + 1 < NTILE:
            front(n + 1)
        scan(n)
    P.wait_all("sp", yt)
    P.close()
    return nc


def build_tail(layer):
    rms = layer == 1
    final = layer == 1
    C = 4096 if layer == 1 else 2048
    CC = C // 128
    NT = 1024
    nc = bass.Bass("TRN2", target_bir_lowering=False)
    P = Prog(nc)
    xT_d = P.dram("xT", [2048, NT], F32, "ExternalInput")
    cat_d = P.dram("catT", [C, NT], F32, "ExternalInput")
    vec_d = P.dram("vec", [128, 6 * 16], F32, "ExternalInput")
    gc_d = P.dram("gcat", [128, CC], F32, "ExternalInput")
    wout_d = P.dram("wout", [C, 2048], F32, "ExternalInput")
    wr_d = P.dram("wr", [2048, 36], F32, "ExternalInput")
    br_d = P.dram("br", [128, 36], F32, "ExternalInput")
    w1_d = P.dram("w1", [32, 2048, 512], F32, "ExternalInput")
    w3_d = P.dram("w3", [32, 2048, 512], F32, "ExternalInput")
    w2_d = P.dram("w2", [32, 512, 2048], F32, "ExternalInput")
    id_d = P.dram("ident", [128, 128], F32, "ExternalInput")
    out_d = P.dram("outT", [2048, NT], F32, "ExternalOutput")

    xT = [[P.sbuf([128, 512], F32, f"x{j}_{h}") for h in range(2)] for j in range(16)]
    hT = [P.sbuf([128, 512], BF16, f"h{i}") for i in range(32)]
    wsl = [P.sbuf([128, 8192], BF16, f"wsl{i}") for i in range(4)]
    aT = [[P.sbuf([128, 512], BF16, f"a{j}_{h}") for h in range(2)] for j in range(4)]
    G = [P.sbuf([128, 1024], F32, f"G{i}") for i in range(2)]
    tmp1 = [P.sbuf([128, 512], F32, f"t1_{i}") for i in range(2)]
    tmp2 = [P.sbuf([128, 512], F32, f"t2_{i}") for i in range(2)]
    stg = [P.sbuf([128, 512], F32, f"stg{i}") for i in range(2)]
    sqb = [P.sbuf([128, 512], BF16, f"sq{i}") for i in range(2)]
    rstd = [P.sbuf([128, 512], F32, f"rstd{i}") for i in range(2)]
    ones = P.sbuf([128, 128], BF16, "ones")
    ident = P.sbuf([128, 128], F32, "ident")
    wr = P.sbuf([128, 16, 36], F32, "wr")
    br = P.sbuf([128, 36], F32, "br")
    vec = P.sbuf([128, 96], F32, "vec")
    gc = P.sbuf([128, CC], F32, "gc")
    gs2 = P.sbuf([128, 16], F32, "gs2")
    gd = [P.sbuf([128, 32], F32, f"gd{i}") for i in range(8)]
    sm = {k: P.sbuf([128, 36], F32, "sm_" + k) for k in
          ("lg", "ge", "ohg", "pen", "em", "oh1", "em2", "oh2")}
    sc = {k: P.sbuf([128, 1], F32, "sc_" + k) for k in
          ("gmax", "ngmax", "gsum", "grp", "m1", "m2", "d", "ed", "den", "p1", "p2", "p1g", "p2g")}
    bank = [P.psum([128, 512], F32, f"bank{i}") for i in range(8)]

    def V(i):
        return vec[:, i * 16:(i + 1) * 16]
    GATE_MIX, SHIFT2, SCALE2, GATE_FFN, NORMG2, FING = range(6)

    P.dma("sp", vec[:], vec_d[:], writes=[vec], sb=vec)
    P.dma("sp", gc[:], gc_d[:], writes=[gc], sb=gc)
    P.dma("sp", ident[:], id_d[:], writes=[ident], sb=ident)
    P.dma("sp", wr[:], wr_d.t.rearrange("(j p) n -> p j n", p=128), writes=[wr], sb=wr)
    P.dma("sp", br[:], br_d[:], writes=[br], sb=br)
    P.op("dve", lambda e: e.memset(ones[:], 1.0), writes=[ones])
    for j in range(16):
        for h in range(2):
            b = xT[j][h]
            P.dma("act" if (j + h) % 2 else "sp", b[:], xT_d[j * 128:(j + 1) * 128, h * 512:(h + 1) * 512],
                  writes=[b], sb=b)

    ring = {"n": 0}

    def wload(src_ap, kind):
        s = wsl[ring["n"] % 4]
        ring["n"] += 1
        if kind == "k16":
            dst = s[:].rearrange("p (j n) -> p j n", n=512)
            src = src_ap.rearrange("(j p) n -> p j n", p=128)
        else:
            dst = s[:].rearrange("p (j n) -> p j n", n=2048)
            src = src_ap.rearrange("(j p) n -> p j n", p=128)
        P.dma("pool", dst, src, writes=[s], sb=s)
        return s, dst

    def sum_sq_rstd(srcs, h, n_feat):
        n = len(srcs)
        for i, (sbuf_, ap) in enumerate(srcs):
            q = sqb[i % 2]
            P.op("act", lambda e: e.activation(q[:], ap, AF.Square), reads=[sbuf_], writes=[q])
            P.op("pe", lambda e: e.matmul(bank[6][:], ones[:], q[:], start=(i == 0), stop=(i == n - 1)),
                 reads=[ones, q], writes=[bank[6]])
        t = tmp1[0]
        P.op("act", lambda e: e.activation(t[:], bank[6][:], AF.Sqrt, bias=EPS, scale=1.0 / n_feat),
             reads=[bank[6]], writes=[t])
        P.op("dve", lambda e: e.reciprocal(rstd[h][:], t[:]), reads=[t], writes=[rstd[h]])

    nmm = 0
    for h in range(2):
        cbufs = []
        for cc in range(CC):
            s = stg[cc % 2]
            P.dma("sp" if cc % 2 else "act", s[:], cat_d[cc * 128:(cc + 1) * 128, h * 512:(h + 1) * 512],
                  writes=[s], sb=s)
            if rms:
                q = sqb[cc % 2]
                P.op("act", lambda e: e.activation(q[:], s[:], AF.Square), reads=[s], writes=[q])
                P.op("pe", lambda e: e.matmul(bank[6][:], ones[:], q[:], start=(cc == 0), stop=(cc == CC - 1)),
                     reads=[ones, q], writes=[bank[6]])
            dst = hT[cc] if CC == 32 else hT[h * 16 + cc]
            P.op("dve", lambda e: e.tensor_scalar(dst[:], s[:], gc[:, cc:cc + 1], None, ALU.mult),
                 reads=[s, gc], writes=[dst])
            cbufs.append(dst)
        if rms:
            t = tmp1[0]
            P.op("act", lambda e: e.activation(t[:], bank[6][:], AF.Sqrt, bias=EPS, scale=1.0 / C),
                 reads=[bank[6]], writes=[t])
            P.op("dve", lambda e: e.reciprocal(rstd[h][:], t[:]), reads=[t], writes=[rstd[h]])
        for cb in range(4):
            slots = []
            for rb in range(C // 2048):
                slots.append(wload(wout_d[rb * 2048:(rb + 1) * 2048, cb * 512:(cb + 1) * 512], "k16"))
            for fc in range(4):
                j = cb * 4 + fc
                bk = bank[nmm % 2]
                nmm += 1
                for cc in range(CC):
                    sb_, view = slots[cc // 16]
                    P.op("pe", lambda e: e.matmul(bk[:], view[:, cc % 16, fc * 128:(fc + 1) * 128], cbufs[cc][:],
                                                  start=(cc == 0), stop=(cc == CC - 1)),
                         reads=[sb_, cbufs[cc]], writes=[bk], defer=(cc != CC - 1))
                xb = xT[j][h]
                if rms:
                    t = tmp2[j % 2]
                    P.op("dve", lambda e: e.tensor_tensor(t[:], bk[:], rstd[h][:], ALU.mult),
                         reads=[bk, rstd[h]], writes=[t])
                    P.op("dve", lambda e: e.scalar_tensor_tensor(xb[:], t[:], V(GATE_MIX)[:, j:j + 1], xb[:],
                                                                 ALU.mult, ALU.add),
                         reads=[t, vec, xb], writes=[xb])
                else:
                    P.op("dve", lambda e: e.scalar_tensor_tensor(xb[:], bk[:], V(GATE_MIX)[:, j:j + 1], xb[:],
                                                                 ALU.mult, ALU.add),
                         reads=[bk, vec, xb], writes=[xb])

    P.op("dve", lambda e: e.tensor_scalar(gs2[:], V(SCALE2), 1.0, None, ALU.add), reads=[vec], writes=[gs2])
    P.op("dve", lambda e: e.tensor_tensor(gs2[:], gs2[:], V(NORMG2), ALU.mult), reads=[gs2, vec], writes=[gs2])
    for h in range(2):
        sum_sq_rstd([(xT[j][h], xT[j][h][:]) for j in range(16)], h, 2048)
        for j in range(16):
            hf = stg[j % 2]
            P.op("dve", lambda e: e.tensor_tensor(hf[:], xT[j][h][:], rstd[h][:], ALU.mult),
                 reads=[xT[j][h], rstd[h]], writes=[hf])
            P.op("dve", lambda e: e.tensor_scalar(hf[:], hf[:], gs2[:, j:j + 1], V(SHIFT2)[:, j:j + 1],
                                                  ALU.mult, ALU.add),
                 reads=[hf, gs2, vec], writes=[hf])
            hb = hT[j * 2 + h]
            P.op("act", lambda e: e.activation(hb[:], hf[:], AF.Copy), reads=[hf], writes=[hb])
            for tt in range(4):
                bk = bank[2 + tt]
                P.op("pe", lambda e: e.matmul(bk[:, 0:36], hf[:, tt * 128:(tt + 1) * 128], wr[:, j, :],
                                              start=(j == 0), stop=(j == 15)),
                     reads=[hf, wr], writes=[bk], defer=not (j == 15 or tt == 3))
        for tt in range(4):
            bk = bank[2 + tt]
            g = gd[h * 4 + tt]
            lg, ge, ohg, pen, em, oh1, em2, oh2 = (sm[k] for k in ("lg", "ge", "ohg", "pen", "em", "oh1", "em2", "oh2"))
            D = lambda fn, r, w: P.op("dve", fn, reads=r, writes=w)
            A = lambda fn, r, w: P.op("act", fn, reads=r, writes=w)
            D(lambda e: e.tensor_tensor(lg[:], bk[:, 0:36], br[:], ALU.add), [bk, br], [lg])
            D(lambda e: e.tensor_reduce(sc["gmax"][:], lg[:, 0:4], AX.X, ALU.max), [lg], [sc["gmax"]])
            D(lambda e: e.tensor_scalar(sc["ngmax"][:], sc["gmax"][:], -1.0, None, ALU.mult), [sc["gmax"]], [sc["ngmax"]])
            A(lambda e: e.activation(ge[:, 0:4], lg[:, 0:4], AF.Exp, bias=sc["ngmax"][:, 0:1], scale=1.0),
              [lg, sc["ngmax"]], [ge])
            D(lambda e: e.tensor_reduce(sc["gsum"][:], ge[:, 0:4], AX.X, ALU.add), [ge], [sc["gsum"]])
            D(lambda e: e.reciprocal(sc["grp"][:], sc["gsum"][:]), [sc["gsum"]], [sc["grp"]])
            D(lambda e: e.tensor_scalar(ohg[:, 0:4], lg[:, 0:4], sc["gmax"][:, 0:1], None, ALU.is_equal),
              [lg, sc["gmax"]], [ohg])
            D(lambda e: e.tensor_scalar(pen[:, 0:4], ohg[:, 0:4], 1e30, -1e30, ALU.mult, ALU.add), [ohg], [pen])
            D(lambda e: e.tensor_tensor(em[:, 0:32].rearrange("p (g k) -> p g k", k=8),
                                        lg[:, 4:36].rearrange("p (g k) -> p g k", k=8),
                                        pen[:, 0:4].unsqueeze(2).broadcast_to([128, 4, 8]), ALU.add),
              [lg, pen], [em])
            D(lambda e: e.tensor_reduce(sc["m1"][:], em[:, 0:32], AX.X, ALU.max), [em], [sc["m1"]])
            D(lambda e: e.tensor_scalar(oh1[:, 0:32], em[:, 0:32], sc["m1"][:, 0:1], None, ALU.is_equal),
              [em, sc["m1"]], [oh1])
            D(lambda e: e.scalar_tensor_tensor(em2[:, 0:32], oh1[:, 0:32], -1e30, em[:, 0:32], ALU.mult, ALU.add),
              [oh1, em], [em2])
            D(lambda e: e.tensor_reduce(sc["m2"][:], em2[:, 0:32], AX.X, ALU.max), [em2], [sc["m2"]])
            D(lambda e: e.tensor_scalar(oh2[:, 0:32], em2[:, 0:32], sc["m2"][:, 0:1], None, ALU.is_equal),
              [em2, sc["m2"]], [oh2])
            D(lambda e: e.tensor_tensor(sc["d"][:], sc["m2"][:], sc["m1"][:], ALU.subtract),
              [sc["m2"], sc["m1"]], [sc["d"]])
            A(lambda e: e.activation(sc["ed"][:], sc["d"][:], AF.Exp), [sc["d"]], [sc["ed"]])
            D(lambda e: e.tensor_scalar(sc["den"][:], sc["ed"][:], 1.0, None, ALU.add), [sc["ed"]], [sc["den"]])
            D(lambda e: e.reciprocal(sc["p1"][:], sc["den"][:]), [sc["den"]], [sc["p1"]])
            D(lambda e: e.tensor_tensor(sc["p2"][:], sc["ed"][:], sc["p1"][:], ALU.mult),
              [sc["ed"], sc["p1"]], [sc["p2"]])
            D(lambda e: e.tensor_tensor(sc["p1g"][:], sc["p1"][:], sc["grp"][:], ALU.mult),
              [sc["p1"], sc["grp"]], [sc["p1g"]])
            D(lambda e: e.tensor_tensor(sc["p2g"][:], sc["p2"][:], sc["grp"][:], ALU.mult),
              [sc["p2"], sc["grp"]], [sc["p2g"]])
            D(lambda e: e.tensor_scalar(g[:], oh1[:, 0:32], sc["p1g"][:, 0:1], None, ALU.mult),
              [oh1, sc["p1g"]], [g])
            D(lambda e: e.scalar_tensor_tensor(g[:], oh2[:, 0:32], sc["p2g"][:, 0:1], g[:], ALU.mult, ALU.add),
              [oh2, sc["p2g"], g], [g])

    def emit_G(e_):
        for tt in range(8):
            bk = bank[6 + tt // 4]
            P.op("pe", lambda e: e.matmul(bk[:, (tt % 4) * 128:(tt % 4 + 1) * 128],
                                          gd[tt][:, e_:e_ + 1].broadcast_to([128, 128]), ident[:],
                                          start=True, stop=True),
                 reads=[gd[tt], ident], writes=[bk], defer=(tt % 4 != 3))
        gb = G[e_ % 2]
        for hh in range(2):
            P.op("act", lambda e: e.activation(gb[:, hh * 512:(hh + 1) * 512], bank[6 + hh][:], AF.Copy),
                 reads=[bank[6 + hh]], writes=[gb])

    NE = 32
    pend = []
    for m in range(2):
        pass
    loads = {}

    def issue_loads(e_):
        loads[e_] = (wload(w1_d[e_], "k16"), wload(w3_d[e_], "k16"), wload(w2_d[e_], "k4"))

    issue_loads(0)
    emit_G(0)
    k = 0
    for e_ in range(NE):
        (s1, v1), (s3, v3), (s2, v2) = loads[e_]
        gb = G[e_ % 2]
        for h in range(2):
            for jc in range(4):
                b1 = bank[k % 2]
                b3 = bank[2 + k % 2]
                for j in range(16):
                    P.op("pe", lambda e: e.matmul(b1[:], v1[:, j, jc * 128:(jc + 1) * 128], hT[j * 2 + h][:],
                                                  start=(j == 0), stop=(j == 15)),
                         reads=[s1, hT[j * 2 + h]], writes=[b1], defer=(j != 15))
                for j in range(16):
                    P.op("pe", lambda e: e.matmul(b3[:], v3[:, j, jc * 128:(jc + 1) * 128], hT[j * 2 + h][:],
                                                  start=(j == 0), stop=(j == 15)),
                         reads=[s3, hT[j * 2 + h]], writes=[b3], defer=(j != 15))
                t1 = tmp1[k % 2]
                t2 = tmp2[k % 2]
                P.op("act", lambda e: e.activation(t1[:], b1[:], AF.Silu), reads=[b1], writes=[t1])
                P.op("dve", lambda e: e.tensor_tensor(t2[:], t1[:], b3[:], ALU.mult), reads=[t1, b3], writes=[t2])
                P.op("dve", lambda e: e.tensor_tensor(aT[jc][h][:], t2[:], gb[:, h * 512:(h + 1) * 512], ALU.mult),
                     reads=[t2, gb], writes=[aT[jc][h]])
                k += 1
            if h == 0 and e_ + 1 < NE:
                pass
        if e_ + 1 < NE:
            emit_G(e_ + 1)
        for h in range(2):
            for fc in range(16):
                by = bank[4 + fc % 2]
                for jc in range(4):
                    P.op("pe", lambda e: e.matmul(by[:], v2[:, jc, fc * 128:(fc + 1) * 128], aT[jc][h][:],
                                                  start=(jc == 0), stop=(jc == 3)),
                         reads=[s2, aT[jc][h]], writes=[by], defer=(jc != 3))
                xb = xT[fc][h]
                P.op("dve", lambda e: e.scalar_tensor_tensor(xb[:], by[:], V(GATE_FFN)[:, fc:fc + 1], xb[:],
                                                             ALU.mult, ALU.add),
                     reads=[by, vec, xb], writes=[xb])
        if e_ + 1 < NE:
            issue_loads(e_ + 1)

    outs = []
    if final:
        for h in range(2):
            sum_sq_rstd([(xT[j][h], xT[j][h][:]) for j in range(16)], h, 2048)
            for j in range(16):
                xb = xT[j][h]
                P.op("dve", lambda e: e.tensor_tensor(xb[:], xb[:], rstd[h][:], ALU.mult),
                     reads=[xb, rstd[h]], writes=[xb])
                P.op("dve", lambda e: e.tensor_scalar(xb[:], xb[:], V(FING)[:, j:j + 1], None, ALU.mult),
                     reads=[xb, vec], writes=[xb])
    for j in range(16):
        for h in range(2):
            xb = xT[j][h]
            P.dma("sp" if (j + h) % 2 else "act", out_d[j * 128:(j + 1) * 128, h * 512:(h + 1) * 512], xb[:],
                  reads=[xb], sb=xb)
            outs.append(xb)
    P.wait_all("sp", outs)
    P.wait_all("act", outs)
    P.close()
    return nc

from concourse.bass_utils import run_bass_kernel_spmd

_CORES = list(range(8))


def _run(nc, maps):
    return run_bass_kernel_spmd(nc, maps, core_ids=_CORES).results


def tail_inputs(layer, xfull, cat, mod, inp):
    m_mix = mod[2 * layer]
    m_ffn = mod[2 * layer + 1]
    vec = np.concatenate([fm(m_mix[4096:]), fm(m_ffn[:2048]), fm(m_ffn[2048:4096]), fm(m_ffn[4096:]),
                          fm(inp["norm_g"][layer, 1]), fm(inp["final_norm_g"])], axis=1)
    vec = np.ascontiguousarray(vec.astype(np.float32))
    if layer == 0:
        gcat = np.ones(2048, np.float32)
        wout = inp["e_w_out"][0]
    else:
        gcat = inp["o_norm_g"][0]
        wout = inp["o_w_out"][0]
    wr = np.ascontiguousarray(np.concatenate([inp["moe_w_group"][layer], inp["moe_w_expert"][layer]], axis=1))
    br = np.concatenate([inp["moe_b_group"][layer], inp["moe_b_expert"][layer]])[None, :]
    br = np.ascontiguousarray(np.tile(br, (128, 1)).astype(np.float32))
    ident = np.eye(128, dtype=np.float32)
    w1 = np.ascontiguousarray(inp["moe_w1"][layer])
    w3 = np.ascontiguousarray(inp["moe_w3"][layer])
    w2 = np.ascontiguousarray(inp["moe_w2"][layer])
    maps = []
    for i in range(8):
        sl = slice(i * 1024, (i + 1) * 1024)
        maps.append({"xT": np.ascontiguousarray(xfull[sl].T), "catT": np.ascontiguousarray(cat[sl].T), "vec": vec,
                     "gcat": fm(gcat), "wout": np.ascontiguousarray(wout), "wr": wr, "br": br,
                     "w1": w1, "w3": w3, "w2": w2, "ident": ident})
    return maps


def kernel(**inputs):
    inp = {k: np.asarray(v, dtype=np.float32) for k, v in inputs.items()}
    x0 = inp["x"][0]
    c = inp["c"][0]
    cT = np.ascontiguousarray(c.reshape(16, 128).T)
    maps = [{"cT": cT, "wmod": np.ascontiguousarray(inp["w_mod"][:, :, i * 768:(i + 1) * 768]),
             "bmod": np.ascontiguousarray(inp["b_mod"][:, i * 768:(i + 1) * 768])} for i in range(8)]
    res = _run(build_mod(), maps)
    mod = np.concatenate([res[i]["mod"] for i in range(8)], axis=1)
    res = _run(build_pre(0), pre_inputs(0, x0, mod, inp))
    pout0 = np.concatenate([res[i]["pout"] for i in range(8)], axis=1)
    res = _run(build_gdn(), gdn_inputs(pout0, inp))
    a_out = np.concatenate([res[h]["o_tok"] for h in range(8)], axis=1)
    cat0 = np.concatenate([a_out, pout0[3072:4096].T], axis=1)
    res = _run(build_tail(0), tail_inputs(0, x0, cat0, mod, inp))
    x1 = np.concatenate([res[i]["outT"].T for i in range(8)], axis=0)
    res = _run(build_pre(1), pre_inputs(1, x1, mod, inp))
    pout1 = np.concatenate([res[i]["pout"] for i in range(8)], axis=1)
    res = _run(build_ssd(), ssd_inputs(pout1, inp))
    yz = np.concatenate([res[g]["yz_tok"] for g in range(8)], axis=1)
    res = _run(build_tail(1), tail_inputs(1, x1, yz, mod, inp))
    out = np.concatenate([res[i]["outT"].T for i in range(8)], axis=0)
    return np.ascontiguousarray(out[None].astype(np.float32))
```
